# Optimizing a Trainium2 kernel written in Bass

```python
import math
import jax
import jax.numpy as jnp
from jax import lax
import numpy as np


D_MODEL = 1024
BATCH = 8
SEQ = 8192
DEPTH = 2

GRID_W = 64
CTX_LEN = 256
EPS = 1e-6
N_MOD = 6

M_HEADS = 4
M_HEAD_DIM = 128
M_WIDTH = M_HEADS * M_HEAD_DIM
M_GATES = 4 * M_HEADS
M_CHUNK = 128
M_CONV = 3
ROPE_BASE = 10000.0

NA_HEADS = 8
NA_HEAD_DIM = 64
NA_WIDTH = NA_HEADS * NA_HEAD_DIM
WIN_R = 8
WIN_C = 16
Q_COLS = 16
K_COLS = 32

S5_GROUP = 16
S5_WIDTH = 512
S5_GROUPS = S5_WIDTH // S5_GROUP
S5_STATE = 64
S5_MAX_RE = -1e-4

N_BRANCH = 3
IN_SPLIT = (M_WIDTH, M_WIDTH, M_WIDTH, M_WIDTH, M_GATES, NA_WIDTH, NA_WIDTH, NA_WIDTH, S5_WIDTH, N_BRANCH * D_MODEL)
IN_COLS = 4 * M_WIDTH + M_GATES + 3 * NA_WIDTH + S5_WIDTH + N_BRANCH * D_MODEL

FF_DIM = 2816
N_EXPERTS = 8
TOP_K = 2
EXPERT_FF = 3584
N_DENSE_LAYERS = (DEPTH + 1) // 2
N_MOE_LAYERS = DEPTH // 2

kernel_name = 'hybrid_mlstm_na_s5_moe_dit'


def rms_norm(x, g):
    xf = x.astype(jnp.float32)
    y = xf * lax.rsqrt(jnp.mean(xf * xf, axis=-1, keepdims=True) + EPS)
    return (y * g.astype(jnp.float32)).astype(x.dtype)


def modulate(h, shift, scale):
    return h * (1 + scale) + shift


def adaln(cvec, w, b):
    return jnp.split(jax.nn.silu(cvec) @ w + b, N_MOD, axis=-1)


def split_columns(w):
    points, acc = [], 0
    for size in IN_SPLIT[:-1]:
        acc += size
        points.append(acc)
    return jnp.split(w, points, axis=-1)


def short_conv(x, w, b):
    pad = w.shape[0] // 2
    y = lax.conv_general_dilated(x, w[:, None, :], window_strides=(1,), padding=[(pad, pad)],
                                 dimension_numbers=('NWC', 'WIO', 'NWC'), feature_group_count=x.shape[-1])
    return y + b


def axial_rope_angles(n):
    pos = jnp.arange(n, dtype=jnp.int32)
    row = (pos // GRID_W).astype(jnp.float32)
    col = (pos % GRID_W).astype(jnp.float32)
    n_freq = M_HEAD_DIM // 4
    inv = ROPE_BASE ** (-jnp.arange(n_freq, dtype=jnp.float32) / n_freq)
    return row[:, None] * inv, col[:, None] * inv


def rotate_half(x, ang):
    x1, x2 = jnp.split(x, 2, axis=-1)
    cos = jnp.cos(ang)[:, None, :]
    sin = jnp.sin(ang)[:, None, :]
    return jnp.concatenate([x1 * cos - x2 * sin, x1 * sin + x2 * cos], axis=-1)


def axial_rope(x, ang_row, ang_col):
    x_row, x_col = jnp.split(x, 2, axis=-1)
    return jnp.concatenate([rotate_half(x_row, ang_row), rotate_half(x_col, ang_col)], axis=-1)


def mlstm_qkv(q_pre, k_pre, v, conv_w, conv_b, ang):
    bsz, n, _ = q_pre.shape
    qk = jax.nn.silu(short_conv(jnp.concatenate([q_pre, k_pre], axis=-1), conv_w, conv_b))
    q, k = jnp.split(qk, 2, axis=-1)
    heads = lambda a: a.reshape(bsz, n, M_HEADS, M_HEAD_DIM).astype(jnp.float32)
    q, k, v = heads(q), heads(k), heads(v)
    if ang is not None:
        q = axial_rope(q, ang[0], ang[1])
        k = axial_rope(k, ang[0], ang[1])
    return q, k * (M_HEAD_DIM ** -0.5), v


def mlstm_zero_state(bsz):
    return (jnp.zeros((bsz, M_HEADS, M_HEAD_DIM, M_HEAD_DIM), jnp.float32),
            jnp.zeros((bsz, M_HEADS, M_HEAD_DIM), jnp.float32),
            jnp.zeros((bsz, M_HEADS), jnp.float32))


def mlstm_chunk_scan(q, k, v, i_pre, log_f, state):
    bsz, n, n_heads, dh = q.shape
    n_chunks = n // M_CHUNK

    def chunks(a):
        a = a.reshape((bsz, n_chunks, M_CHUNK) + a.shape[2:])
        return jnp.moveaxis(a, (1, 2), (0, 3))

    tril = jnp.tril(jnp.ones((M_CHUNK, M_CHUNK), dtype=bool))

    def step(carry, xs):
        c_mat, n_vec, m = carry
        qc, kc, vc, ic, fc = xs
        b = jnp.cumsum(fc, axis=-1)
        log_w = jnp.where(tril, b[..., :, None] - b[..., None, :] + ic[..., None, :], -jnp.inf)
        carry_log = b + m[..., None]
        m_t = jnp.maximum(carry_log, jnp.max(log_w, axis=-1))
        w = jnp.exp(log_w - m_t[..., None])
        s = jnp.einsum('bhtd,bhsd->bhts', qc, kc) * w
        c_scale = jnp.exp(carry_log - m_t)
        num = jnp.einsum('bhts,bhsd->bhtd', s, vc) + c_scale[..., None] * jnp.einsum('bhde,bhte->bhtd', c_mat, qc)
        den = jnp.sum(s, axis=-1) + c_scale * jnp.einsum('bhe,bhte->bht', n_vec, qc)
        h = num / jnp.maximum(jnp.abs(den), jnp.exp(-m_t))[..., None]
        b_end = b[..., -1]
        log_u = b_end[..., None] - b + ic
        m_new = jnp.maximum(b_end + m, jnp.max(log_u, axis=-1))
        u = jnp.exp(log_u - m_new[..., None])
        decay = jnp.exp(b_end + m - m_new)
        c_new = decay[..., None, None] * c_mat + jnp.einsum('bhs,bhsd,bhse->bhde', u, vc, kc)
        n_new = decay[..., None] * n_vec + jnp.einsum('bhs,bhse->bhe', u, kc)
        return (c_new, n_new, m_new), h

    state, h = lax.scan(step, state, (chunks(q), chunks(k), chunks(v), chunks(i_pre), chunks(log_f)))
    h = jnp.moveaxis(h, (0, 3), (1, 2)).reshape(bsz, n, n_heads, dh)
    return h, state


def mlstm_bidir(q, k, v, gates, state_f, state_b):
    i_f, f_f, i_b, f_b = jnp.split(gates.astype(jnp.float32), 4, axis=-1)
    h_f, fin_f = mlstm_chunk_scan(q, k, v, i_f, jax.nn.log_sigmoid(f_f), state_f)
    flip = lambda a: jnp.flip(a, axis=1)
    h_b, fin_b = mlstm_chunk_scan(flip(q), flip(k), flip(v), flip(i_b), flip(jax.nn.log_sigmoid(f_b)), state_b)
    return h_f + flip(h_b), fin_f, fin_b


def mlstm_out(h, o_pre, norm_w):
    bsz, n = h.shape[:2]
    mu = jnp.mean(h, axis=-1, keepdims=True)
    var = jnp.mean(jnp.square(h - mu), axis=-1, keepdims=True)
    hn = ((h - mu) * lax.rsqrt(var + EPS)).reshape(bsz, n, M_WIDTH)
    return (hn * norm_w.astype(jnp.float32) * jax.nn.sigmoid(o_pre.astype(jnp.float32))).astype(o_pre.dtype)


def neighbourhood_attention(q, k, v, k_ctx, v_ctx, rpb):
    bsz, n, n_heads, dh = q.shape
    rows = n // GRID_W
    win_r = min(WIN_R, rows)
    n_cb = GRID_W // Q_COLS
    q_col = np.arange(GRID_W).reshape(n_cb, Q_COLS)
    col_start = np.clip(q_col - WIN_C // 2, 0, GRID_W - WIN_C)
    k_start = np.clip(np.arange(n_cb) * Q_COLS - WIN_C // 2, 0, GRID_W - K_COLS)
    k_col = k_start[:, None] + np.arange(K_COLS)
    col_ok = (k_col[:, None, :] >= col_start[:, :, None]) & (k_col[:, None, :] < col_start[:, :, None] + WIN_C)
    dc_idx = np.clip(k_col[:, None, :] - q_col[:, :, None] + WIN_C - 1, 0, 2 * WIN_C - 2)
    scale = dh ** -0.5
    n_loc = win_r * K_COLS
    q_g = q.reshape(bsz, rows, n_cb, Q_COLS, n_heads, dh)
    k_g = k.reshape(bsz, rows, GRID_W, n_heads, dh)[:, :, k_col]
    v_g = v.reshape(bsz, rows, GRID_W, n_heads, dh)[:, :, k_col]

    def row_block(r):
        r0 = jnp.clip(r - win_r // 2, 0, rows - win_r)
        k_r = lax.dynamic_slice_in_dim(k_g, r0, win_r, axis=1)
        v_r = lax.dynamic_slice_in_dim(v_g, r0, win_r, axis=1)
        q_r = lax.dynamic_index_in_dim(q_g, r, axis=1, keepdims=False)
        dr_idx = r0 + jnp.arange(win_r) - r + WIN_R - 1
        bias = rpb[:, dr_idx[None, None, :, None], dc_idx[:, :, None, :]].astype(jnp.float32)
        s_loc = jnp.einsum('bjqhd,bwjkhd->bhjqwk', q_r, k_r).astype(jnp.float32) * scale + bias
        s_loc = jnp.where(col_ok[:, :, None, :], s_loc, -jnp.inf).reshape(bsz, n_heads, n_cb, Q_COLS, n_loc)
        s_ctx = jnp.einsum('bjqhd,bchd->bhjqc', q_r, k_ctx).astype(jnp.float32) * scale
        p = jax.nn.softmax(jnp.concatenate([s_loc, s_ctx], axis=-1), axis=-1).astype(v.dtype)
        p_loc = p[..., :n_loc].reshape(bsz, n_heads, n_cb, Q_COLS, win_r, K_COLS)
        o = jnp.einsum('bhjqwk,bwjkhd->bjqhd', p_loc, v_r) + jnp.einsum('bhjqc,bchd->bjqhd', p[..., n_loc:], v_ctx)
        return o.reshape(bsz, GRID_W, n_heads, dh)

    out = lax.map(row_block, jnp.arange(rows))
    return jnp.moveaxis(out, 0, 1).reshape(bsz, n, n_heads, dh)


def context_attention(q, k, v):
    s = jnp.einsum('bqhd,bkhd->bhqk', q, k).astype(jnp.float32) * (q.shape[-1] ** -0.5)
    p = jax.nn.softmax(s, axis=-1).astype(v.dtype)
    return jnp.einsum('bhqk,bkhd->bqhd', p, v)


def s5_discretise(lam_re, lam_im, log_dt, b_re, b_im):
    lam = lax.complex(jnp.minimum(lam_re.astype(jnp.float32), S5_MAX_RE), lam_im.astype(jnp.float32))
    lam_bar = jnp.exp(lam * jnp.exp(log_dt.astype(jnp.float32))[:, None])
    b_bar = ((lam_bar - 1.0) / lam)[..., None] * lax.complex(b_re.astype(jnp.float32), b_im.astype(jnp.float32))
    return lam_bar, b_bar


def linear_recurrence_combine(left, right):
    a_l, b_l = left
    a_r, b_r = right
    return a_r * a_l, a_r * b_l + b_r


def s5_scan(u, lam_bar, b_bar, h0):
    bu = jnp.einsum('gph,bngh->bngp', b_bar, u.astype(jnp.complex64))
    bu = bu.at[:, 0].add(lam_bar * h0)
    a = jnp.broadcast_to(lam_bar, (1, u.shape[1]) + lam_bar.shape)
    _, states = lax.associative_scan(linear_recurrence_combine, (a, bu), axis=1)
    return states


def s5_states(u, disc, h0_f, h0_b):
    bsz, n, _ = u.shape
    ug = u.astype(jnp.float32).reshape(bsz, n, S5_GROUPS, S5_GROUP)
    st_f = s5_scan(ug, disc[0][0], disc[0][1], h0_f)
    st_b = jnp.flip(s5_scan(jnp.flip(ug, axis=1), disc[1][0], disc[1][1], h0_b), axis=1)
    return st_f, st_b


def s5_readout(st_f, st_b, c_f, c_b):
    y = jnp.real(jnp.einsum('ghp,bngp->bngh', c_f, st_f)) + jnp.real(jnp.einsum('ghp,bngp->bngh', c_b, st_b))
    return y.reshape(y.shape[0], y.shape[1], S5_WIDTH)


def s5_out(y_ssm, u, d, glu_w, glu_b):
    y = (y_ssm + d.astype(jnp.float32) * u.astype(jnp.float32)).astype(u.dtype)
    g = jax.nn.gelu(y)
    return g * jax.nn.sigmoid(g @ glu_w + glu_b)


def merge_branches(y_m, y_na, y_s5, gate_pre, lp):
    g = jax.nn.sigmoid(gate_pre.astype(jnp.float32)).astype(gate_pre.dtype)
    g_m, g_na, g_s5 = jnp.split(g, N_BRANCH, axis=-1)
    y = g_m * (y_m @ lp['w_branch_m']) + g_na * (y_na @ lp['w_branch_na']) + g_s5 * (y_s5 @ lp['w_branch_s5'])
    return y @ lp['w_out']


def token_mixers(hx, hc, lp, ang, ctx_out):
    bsz, n, _ = hx.shape
    n_ctx = hc.shape[1]
    w_parts = split_columns(lp['w_in'])
    px = [hx @ w for w in w_parts]
    pc = lambda i: hc @ w_parts[i]
    qx, kx, vx = mlstm_qkv(px[0], px[1], px[2], lp['m_conv_w'], lp['m_conv_b'], ang)
    qc, kc, vc = mlstm_qkv(pc(0), pc(1), pc(2), lp['m_conv_w'], lp['m_conv_b'], None)
    zero = mlstm_zero_state(bsz)
    hm_c, fin_f, fin_b = mlstm_bidir(qc, kc, vc, pc(4) + lp['m_gate_b'], zero, zero)
    hm_x, _, _ = mlstm_bidir(qx, kx, vx, px[4] + lp['m_gate_b'], fin_f, fin_b)
    ym_x = mlstm_out(hm_x, px[3], lp['m_norm'])
    heads = lambda a: a.reshape(a.shape[0], a.shape[1], NA_HEADS, NA_HEAD_DIM)
    k_ctx, v_ctx = heads(pc(6)), heads(pc(7))
    yn_x = neighbourhood_attention(heads(px[5]), heads(px[6]), heads(px[7]), k_ctx, v_ctx, lp['na_rpb']).reshape(bsz, n, NA_WIDTH)
    disc = [s5_discretise(lp['s5_lam_re'][d], lp['s5_lam_im'][d], lp['s5_log_dt'][d], lp['s5_b_re'][d], lp['s5_b_im'][d]) for d in range(2)]
    c_f, c_b = [lax.complex(lp['s5_c_re'][d].astype(jnp.float32), lp['s5_c_im'][d].astype(jnp.float32)) for d in range(2)]
    zero_s = jnp.zeros((bsz, S5_GROUPS, S5_STATE), jnp.complex64)
    u_c = pc(8)
    sc_f, sc_b = s5_states(u_c, disc, zero_s, zero_s)
    sx_f, sx_b = s5_states(px[8], disc, sc_f[:, -1], sc_b[:, 0])
    ys_x = s5_out(s5_readout(sx_f, sx_b, c_f, c_b), px[8], lp['s5_d'], lp['s5_glu_w'], lp['s5_glu_b'])
    out_x = merge_branches(ym_x, yn_x, ys_x, px[9], lp)
    if not ctx_out:
        return out_x, None
    ym_c = mlstm_out(hm_c, pc(3), lp['m_norm'])
    yn_c = context_attention(heads(pc(5)), k_ctx, v_ctx).reshape(bsz, n_ctx, NA_WIDTH)
    ys_c = s5_out(s5_readout(sc_f, sc_b, c_f, c_b), u_c, lp['s5_d'], lp['s5_glu_w'], lp['s5_glu_b'])
    out_c = merge_branches(ym_c, yn_c, ys_c, pc(9), lp)
    return out_x, out_c


def swiglu(h, w_gate, w_up, w_down):
    return (jax.nn.silu(h @ w_gate) * (h @ w_up)) @ w_down


def moe_swiglu(h, router, w_gate, w_up, w_down):
    shape = h.shape
    t = h.reshape(-1, shape[-1])
    logits = (t @ router).astype(jnp.float32)
    top_val, top_idx = lax.top_k(logits, TOP_K)
    top_p = jax.nn.softmax(top_val, axis=-1)
    combine = jnp.sum(jax.nn.one_hot(top_idx, N_EXPERTS, dtype=jnp.float32) * top_p[..., None], axis=1).astype(t.dtype)
    out = jnp.zeros_like(t)
    for e in range(N_EXPERTS):
        out = out + combine[:, e:e + 1] * swiglu(t, w_gate[e], w_up[e], w_down[e])
    return out.reshape(shape)


def setup_inputs(seed: int = 0) -> dict:
    key = jax.random.key(seed)
    ks = iter(jax.random.split(key, 40))

    def nrm(shape, scale):
        return jax.random.normal(next(ks), shape, jnp.float32) * scale

    def gain(shape):
        return 1.0 + nrm(shape, 0.05)

    d = D_MODEL
    forget_b = jnp.linspace(3.0, 6.0, M_HEADS, dtype=jnp.float32)
    zeros_h = jnp.zeros((M_HEADS,), jnp.float32)
    gate_b_base = jnp.concatenate([zeros_h, forget_b, zeros_h, forget_b])
    n_idx = jnp.arange(S5_STATE, dtype=jnp.float32)
    s5_shape = (DEPTH, 2, S5_GROUPS, S5_STATE)
    return {
        'x': nrm((BATCH, SEQ, d), 1.0),
        'c': nrm((BATCH, d), 1.0),
        'ctx': nrm((BATCH, CTX_LEN, d), 1.0),
        'c_ctx': nrm((d,), 1.0),
        'ada_w': nrm((DEPTH, d, N_MOD * d), 0.5 * d ** -0.5),
        'ada_b': nrm((DEPTH, N_MOD * d), 0.02),
        'norm_mix_pre': gain((DEPTH, d)),
        'norm_mix_post': gain((DEPTH, d)),
        'norm_ffn_pre': gain((DEPTH, d)),
        'norm_ffn_post': gain((DEPTH, d)),
        'w_in': nrm((DEPTH, d, IN_COLS), d ** -0.5),
        'm_gate_b': gate_b_base + nrm((DEPTH, M_GATES), 0.1),
        'm_conv_w': nrm((DEPTH, M_CONV, 2 * M_WIDTH), M_CONV ** -0.5),
        'm_conv_b': nrm((DEPTH, 2 * M_WIDTH), 0.02),
        'm_norm': gain((DEPTH, M_WIDTH)),
        'na_rpb': nrm((DEPTH, NA_HEADS, 2 * WIN_R - 1, 2 * WIN_C - 1), 0.02),
        's5_lam_re': -0.5 + nrm(s5_shape, 0.01),
        's5_lam_im': jnp.pi * n_idx + nrm(s5_shape, 0.01),
        's5_log_dt': jax.random.uniform(next(ks), (DEPTH, 2, S5_GROUPS), jnp.float32, minval=math.log(1e-3), maxval=math.log(1e-1)),
        's5_b_re': nrm((DEPTH, 2, S5_GROUPS, S5_STATE, S5_GROUP), (2 * S5_GROUP) ** -0.5),
        's5_b_im': nrm((DEPTH, 2, S5_GROUPS, S5_STATE, S5_GROUP), (2 * S5_GROUP) ** -0.5),
        's5_c_re': nrm((DEPTH, 2, S5_GROUPS, S5_GROUP, S5_STATE), S5_STATE ** -0.5),
        's5_c_im': nrm((DEPTH, 2, S5_GROUPS, S5_GROUP, S5_STATE), S5_STATE ** -0.5),
        's5_d': nrm((DEPTH, S5_WIDTH), 1.0),
        's5_glu_w': nrm((DEPTH, S5_WIDTH, S5_WIDTH), S5_WIDTH ** -0.5),
        's5_glu_b': nrm((DEPTH, S5_WIDTH), 0.02),
        'w_branch_m': nrm((DEPTH, M_WIDTH, d), M_WIDTH ** -0.5),
        'w_branch_na': nrm((DEPTH, NA_WIDTH, d), NA_WIDTH ** -0.5),
        'w_branch_s5': nrm((DEPTH, S5_WIDTH, d), S5_WIDTH ** -0.5),
        'w_out': nrm((DEPTH, d, d), d ** -0.5),
        'ffn_w_gate': nrm((N_DENSE_LAYERS, d, FF_DIM), d ** -0.5),
        'ffn_w_up': nrm((N_DENSE_LAYERS, d, FF_DIM), d ** -0.5),
        'ffn_w_down': nrm((N_DENSE_LAYERS, FF_DIM, d), FF_DIM ** -0.5),
        'moe_router': nrm((N_MOE_LAYERS, d, N_EXPERTS), d ** -0.5),
        'moe_w_gate': nrm((N_MOE_LAYERS, N_EXPERTS, d, EXPERT_FF), d ** -0.5),
        'moe_w_up': nrm((N_MOE_LAYERS, N_EXPERTS, d, EXPERT_FF), d ** -0.5),
        'moe_w_down': nrm((N_MOE_LAYERS, N_EXPERTS, EXPERT_FF, d), EXPERT_FF ** -0.5),
    }


def reference(x, c, ctx, c_ctx, ada_w, ada_b, norm_mix_pre, norm_mix_post, norm_ffn_pre, norm_ffn_post,
              w_in, m_gate_b, m_conv_w, m_conv_b, m_norm, na_rpb, s5_lam_re, s5_lam_im, s5_log_dt,
              s5_b_re, s5_b_im, s5_c_re, s5_c_im, s5_d, s5_glu_w, s5_glu_b, w_branch_m, w_branch_na,
              w_branch_s5, w_out, ffn_w_gate, ffn_w_up, ffn_w_down, moe_router, moe_w_gate, moe_w_up, moe_w_down):
    ang = axial_rope_angles(x.shape[1])
    xc = ctx
    for l in range(DEPTH):
        last = l == DEPTH - 1
        lp = {
            'w_in': w_in[l], 'm_gate_b': m_gate_b[l], 'm_conv_w': m_conv_w[l], 'm_conv_b': m_conv_b[l],
            'm_norm': m_norm[l], 'na_rpb': na_rpb[l], 's5_lam_re': s5_lam_re[l], 's5_lam_im': s5_lam_im[l],
            's5_log_dt': s5_log_dt[l], 's5_b_re': s5_b_re[l], 's5_b_im': s5_b_im[l], 's5_c_re': s5_c_re[l],
            's5_c_im': s5_c_im[l], 's5_d': s5_d[l], 's5_glu_w': s5_glu_w[l], 's5_glu_b': s5_glu_b[l],
            'w_branch_m': w_branch_m[l], 'w_branch_na': w_branch_na[l], 'w_branch_s5': w_branch_s5[l],
            'w_out': w_out[l],
        }
        mx = [m[:, None, :] for m in adaln(c, ada_w[l], ada_b[l])]
        mc = adaln(c_ctx, ada_w[l], ada_b[l])
        hx = modulate(rms_norm(x, norm_mix_pre[l]), mx[0], mx[1])
        hc = modulate(rms_norm(xc, norm_mix_pre[l]), mc[0], mc[1])
        out_x, out_c = token_mixers(hx, hc, lp, ang, not last)
        x = x + mx[2] * rms_norm(out_x, norm_mix_post[l])
        j = l // 2
        if l % 2 == 0:
            def ffn(h, j=j):
                return swiglu(h, ffn_w_gate[j], ffn_w_up[j], ffn_w_down[j])
        else:
            def ffn(h, j=j):
                return moe_swiglu(h, moe_router[j], moe_w_gate[j], moe_w_up[j], moe_w_down[j])
        x = x + mx[5] * rms_norm(ffn(modulate(rms_norm(x, norm_ffn_pre[l]), mx[3], mx[4])), norm_ffn_post[l])
        if not last:
            xc = xc + mc[2] * rms_norm(out_c, norm_mix_post[l])
            xc = xc + mc[5] * rms_norm(ffn(modulate(rms_norm(xc, norm_ffn_pre[l]), mc[3], mc[4])), norm_ffn_post[l])
    return x
```

```python
import numpy as np
from contextlib import ExitStack
import concourse.bass as bass
import concourse.mybir as mybir
from concourse.bass_utils import run_bass_kernel_spmd
from concourse.alu_op_type import AluOpType as ALU

F32 = mybir.dt.float32
BF16 = mybir.dt.bfloat16
I32 = mybir.dt.int32
AF = mybir.ActivationFunctionType
AX = mybir.AxisListType

D = 1024
KD = 8
CTX = 256
GRID_W = 64
EPS = 1e-6
FF = 2816
EFF = 3584
NEXP = 8


SEM_LIMIT = 30000


class Sem:
    def __init__(self, h):
        self.h = h
        self.cnt = 0


class Buf:
    def __init__(self, t, name):
        self.t = t
        self.name = name
        self.w = {}
        self.r = {}
        self.dsem = None
        self.multi = False

    def __getitem__(self, k):
        return self.t[k]


class Eng:
    def __init__(self, h, sem, name):
        self.h = h
        self.sem = sem
        self.name = name
        self.known = {}


class KB:
    def __init__(self, nc):
        self.nc = nc
        self.glob = ExitStack()
        self.stk = self.glob
        self.pe_sems = set()
        self.pe = Eng(nc.tensor, self._sem("pe"), "pe")
        self.dve = Eng(nc.vector, self._sem("dve"), "dve")
        self.act = Eng(nc.scalar, self._sem("act"), "act")
        self.pool = Eng(nc.gpsimd, self._sem("pool"), "pool")
        self.sp = Eng(nc.sync, self._sem("sp"), "sp")
        self.dsems = []
        self.retired = []
        self.ndma = 0
        self.uid = 0
        self.free_dsems = []
        self.freed = {}
        self.live = []

    def _sem(self, name):
        return Sem(self.glob.enter_context(self.nc.semaphore(name)))

    def _track(self, b):
        b.r = dict(self.freed)
        self.stk.callback(self._release, b)
        return b

    def _release(self, b):
        if b.dsem is not None:
            self.free_dsems.append(b.dsem)
            b.dsem = None
        for dd in (b.w, b.r):
            for s, v in dd.items():
                if self.freed.get(s, 0) < v:
                    self.freed[s] = v

    def sb(self, name, shape, dt):
        self.uid += 1
        t = self.stk.enter_context(self.nc.sbuf_tensor(f"{name}_{self.uid}", list(shape), dt))
        return self._track(Buf(t, name))

    def ps(self, name, shape, dt=F32):
        self.uid += 1
        t = self.stk.enter_context(self.nc.psum_tensor(f"{name}_{self.uid}", list(shape), dt))
        return self._track(Buf(t, name))

    def dram(self, name, shape, dt, kind="Internal"):
        t = self.nc.dram_tensor(name, list(shape), dt, kind=kind).ap()
        b = Buf(t, name)
        b.multi = True
        return b

    def _deps(self, e, r, w):
        deps = {}
        for b in r:
            for s, v in b.w.items():
                if deps.get(s, 0) < v:
                    deps[s] = v
        for b in w:
            for dd in ((b.r,) if b.multi else (b.w, b.r)):
                for s, v in dd.items():
                    if deps.get(s, 0) < v:
                        deps[s] = v
        for s, v in deps.items():
            if e is self.pe and (s is e.sem or s in self.pe_sems):
                continue
            if e.known.get(s, 0) < v:
                e.h.wait_ge(s.h, v)
                e.known[s] = v

    def _mark(self, s, v, r, w):
        for b in r:
            if b.r.get(s, 0) < v:
                b.r[s] = v
        for b in w:
            if b.multi:
                if b.w.get(s, 0) < v:
                    b.w[s] = v
            else:
                b.w = {s: v}
                b.r = {}

    def op(self, e, fn, r=(), w=(), sig=True):
        self._deps(e, r, w)
        ins = fn()
        sm = e.sem
        if sig:
            sm.cnt += 1
            ins.then_inc(sm.h, 1)
            v = sm.cnt
            if sm.cnt >= SEM_LIMIT:
                self.retired.append(sm)
                if e is self.pe:
                    self.pe_sems.add(sm)
                e.sem = self._sem(f"{e.name}{len(self.retired)}")
        else:
            v = sm.cnt + 1
        self._mark(sm, v, r, w)
        return ins

    def dma(self, q, out, in_, r=(), w=(), sb=None, **kw):
        self._deps(q, r, w)
        if sb.dsem is None:
            if self.free_dsems:
                sb.dsem = self.free_dsems.pop()
            else:
                sb.dsem = self._sem(f"d{len(self.dsems)}")
                self.dsems.append(sb.dsem)
        if sb.dsem.cnt + 16 > SEM_LIMIT:
            self.retired.append(sb.dsem)
            sb.dsem = self._sem(f"d{len(self.dsems)}")
            self.dsems.append(sb.dsem)
        s = sb.dsem
        ins = q.h.dma_start(out=out, in_=in_, **kw)
        s.cnt += 16
        ins.then_inc(s.h, 16)
        self._mark(s, s.cnt, r, w)
        self.ndma += 1
        return ins

    def wait_all(self, e, bufs):
        self._deps(e, bufs, ())


def mm(kb, out_ap, lhsT, rhs, start, stop, r, w, sig=None, **kw):
    if sig is None:
        sig = stop
    return kb.op(kb.pe, lambda: kb.nc.tensor.matmul(out_ap, lhsT=lhsT, rhs=rhs, start=start, stop=stop, **kw),
                 r=r, w=w, sig=sig)


def tok_blocks(NT, bs=512, t_lo=0):
    out = []
    t = t_lo
    while t < NT:
        n = min(bs, NT - t)
        out.append((t, n))
        t += n
    return out


def gemm_fm(kb, NT, srcs, groups, epilogue, wbufs=2, tag="g", t_lo=0):
    nc = kb.nc
    old = kb.stk
    with ExitStack() as st:
        kb.stk = st
        maxblk = max(g["nblk"] for g in groups)
        nstream = max(len(g["streams"]) for g in groups)
        wt = {}
        for si in range(nstream):
            kcs = max(srcs[g["streams"][si][0]][1] for g in groups if len(g["streams"]) > si)
            wt[si] = [kb.sb(f"{tag}w{si}", [128, kcs, maxblk * 128], BF16) for _ in range(wbufs)]
        abuf = {}
        for ai, (A, Kc) in enumerate(srcs):
            abuf[ai] = [kb.sb(f"{tag}a{ai}", [128, Kc, 512], BF16) for _ in range(2)]
        nps = 8 // nstream
        pss = [[kb.ps(f"{tag}p", [128, 512]) for _ in range(nstream)] for _ in range(min(nps, 4))]
        pi = 0
        tbs = tok_blocks(NT, t_lo=t_lo)
        seq = [(gi, ti) for gi in range(len(groups)) for ti in range(len(tbs))]
        wloaded = {}

        def load_w(gi):
            if gi in wloaded or gi >= len(groups):
                return
            g = groups[gi]
            ws = []
            for si, (src_idx, W_ap) in enumerate(g["streams"]):
                Kc = srcs[src_idx][1]
                wb = wt[si][gi % wbufs]
                kb.dma(kb.pool, wb[:, 0:Kc, 0:g["nblk"] * 128],
                       W_ap.rearrange("(kc p) m -> p kc m", p=128), w=[wb], sb=wb)
                ws.append(wb)
            wloaded[gi] = ws

        def load_a(idx):
            gi, ti = seq[idx]
            t0, tn = tbs[ti]
            ab = {}
            for ai in sorted(set(s_[0] for s_ in groups[gi]["streams"])):
                A, Kc = srcs[ai]
                b = abuf[ai][idx % 2]
                kb.dma(kb.sp, b[:, :, 0:tn], A.t.rearrange("(kc p) n -> p kc n", p=128)[:, :, t0:t0 + tn],
                       r=[A], w=[b], sb=b)
                ab[ai] = b
            return ab

        load_w(0)
        pending = load_a(0)
        for idx, (gi, ti) in enumerate(seq):
            g = groups[gi]
            t0, tn = tbs[ti]
            ws = wloaded[gi]
            if ti == 0 and wbufs > 1:
                load_w(gi + 1)
            ab = pending
            if idx + 1 < len(seq):
                if seq[idx + 1][0] != gi:
                    load_w(seq[idx + 1][0])
                pending = load_a(idx + 1)
            for j in range(g["nblk"]):
                pl = pss[pi % len(pss)]
                pi += 1
                for si, (src_idx, W_ap) in enumerate(g["streams"]):
                    Kc = srcs[src_idx][1]
                    for kc in range(Kc):
                        mm(kb, pl[si][:, 0:tn], ws[si][:, kc, j * 128:(j + 1) * 128], ab[src_idx][:, kc, 0:tn],
                           kc == 0, kc == Kc - 1, r=[ws[si], ab[src_idx]], w=[pl[si]])
                epilogue(kb, pl[:len(g["streams"])], g, j, t0, tn)
    kb.stk = old


def gemm_tm(kb, tiles, A, Kc, W_ap, M, epilogue, tag="t", wtile=None, npairs=None):
    old = kb.stk
    with ExitStack() as st:
        kb.stk = st
        nh = (M + 511) // 512
        if wtile is None:
            wb = kb.sb(f"{tag}w", [128, Kc, M], BF16)
            kb.dma(kb.pool, wb[:, :, :], W_ap.rearrange("(kc p) m -> p kc m", p=128), w=[wb], sb=wb)
        else:
            wb = wtile
        abufs = [kb.sb(f"{tag}a", [128, Kc, 512], BF16) for _ in range(2)]
        if npairs is None:
            npairs = 8 // nh if nh > 1 else 4
        pss = [[kb.ps(f"{tag}p", [128, 512]) for _ in range(nh)] for _ in range(npairs)]
        pi = 0
        runs = []
        for t0 in tiles:
            if runs and runs[-1][0] + 128 * len(runs[-1][1]) == t0 and len(runs[-1][1]) < 4:
                runs[-1][1].append(t0)
            else:
                runs.append((t0, [t0]))
        def load_run(ri):
            r0, ts = runs[ri]
            b = abufs[ri % 2]
            n = 128 * len(ts)
            kb.dma(kb.sp, b[:, :, 0:n], A.t.rearrange("(kc p) n -> p kc n", p=128)[:, :, r0:r0 + n],
                   r=[A], w=[b], sb=b)
            return b

        pending = load_run(0)
        for ri, (r0, ts) in enumerate(runs):
            b = pending
            if ri + 1 < len(runs):
                pending = load_run(ri + 1)
            for ti, t0 in enumerate(ts):
                pl = pss[pi % len(pss)]
                pi += 1
                for h in range(nh):
                    mw = min(512, M - h * 512)
                    for kc in range(Kc):
                        mm(kb, pl[h][:, 0:mw], b[:, kc, ti * 128:(ti + 1) * 128], wb[:, kc, h * 512:h * 512 + mw],
                           kc == 0, kc == Kc - 1, r=[wb, b], w=[pl[h]])
                epilogue(kb, pl, t0)
    kb.stk = old


class Prog:
    def __init__(self, N, depth=2, taps=()):
        self.N = N
        self.NT = N + CTX
        self.depth = depth
        self.taps = set(taps)
        self.dbg_in = {}
        self.nc = bass.Bass("TRN2", target_bir_lowering=False)
        self.kb = KB(self.nc)
        self.inp = {}
        self.out = {}

    def din(self, name, shape, dt=F32):
        b = self.kb.dram(name, shape, dt, kind="ExternalInput")
        self.inp[name] = b
        return b

    def dscr(self, name, shape, dt, tap=None):
        kind = "ExternalOutput" if name in self.taps else "Internal"
        b = self.kb.dram(name, shape, dt, kind=kind)
        if kind == "ExternalOutput":
            self.out[name] = b
        return b

    def consts(self):
        kb, nc = self.kb, self.nc
        self.ident = kb.sb("ident", [128, 128], F32)
        ones = kb.sb("ones", [128, 128], F32)
        kb.op(kb.pool, lambda: nc.gpsimd.memset(ones[:, :], 1.0), w=[ones])
        kb.op(kb.pool, lambda: nc.gpsimd.affine_select(out=self.ident[:, :], in_=ones[:, :], pattern=[[-1, 128]],
                                                      compare_op=ALU.is_equal, fill=0.0, base=0, channel_multiplier=1),
              r=[ones], w=[self.ident])
        self.ones = ones
        self.identb = kb.sb("identb", [128, 128], BF16)
        kb.op(kb.dve, lambda: nc.vector.tensor_copy(out=self.identb[:, :], in_=self.ident[:, :]),
              r=[self.ident], w=[self.identb])
        self.maskf = kb.sb("maskf", [128, 128], F32)
        self.maskb = kb.sb("maskb", [128, 128], F32)
        kb.op(kb.pool, lambda: nc.gpsimd.affine_select(out=self.maskf[:, :], in_=ones[:, :], pattern=[[1, 128]],
                                                      compare_op=ALU.is_ge, fill=0.0, base=0, channel_multiplier=-1),
              r=[ones], w=[self.maskf])
        kb.op(kb.pool, lambda: nc.gpsimd.affine_select(out=self.maskb[:, :], in_=ones[:, :], pattern=[[-1, 128]],
                                                      compare_op=ALU.is_ge, fill=0.0, base=0, channel_multiplier=1),
              r=[ones], w=[self.maskb])

    def adaln(self, l):
        kb, nc, I = self.kb, self.nc, self.inp
        res = {}
        modf = kb.sb("modf", [128, 48, 2], F32)
        ab = kb.sb("ab", [128, 4, 8, 2], F32)
        gm = kb.sb("gm", [128, 4, 1024], F32)
        gmrow = self.dscr(f"gmrow{l}", [4, 1024], F32)
        old = kb.stk
        with ExitStack() as st:
            kb.stk = st
            cc = kb.sb("cc", [128, 8, 2], F32)
            sc = kb.sb("sc", [128, 8, 2], F32)
            with nc.allow_non_contiguous_dma(reason="tiny conditioning vectors"):
                for s in range(2):
                    kb.dma(kb.sp, cc[:, :, s], I["cc"].t[s, :].rearrange("(kc p) -> p kc", p=128), w=[cc], sb=cc)
                bf = kb.sb("adab", [128, 48], F32)
                kb.dma(kb.sp, bf[:, :], I["ada_b"].t[l, :].rearrange("(j p) -> p j", p=128), w=[bf], sb=bf)
                gn = kb.sb("gn", [128, 2, 8], F32)
                kb.dma(kb.sp, gn[:, 0, :], I["norm_mix_pre"].t[l, :].rearrange("(j p) -> p j", p=128), w=[gn], sb=gn)
                kb.dma(kb.sp, gn[:, 1, :], I["norm_ffn_pre"].t[l, :].rearrange("(j p) -> p j", p=128), w=[gn], sb=gn)
            kb.op(kb.act, lambda: nc.scalar.activation(out=sc[:, :, :], in_=cc[:, :, :], func=AF.Silu), r=[cc], w=[sc])
            brow = kb.sb("brow", [1, 2, 1024], F32)
            grow = kb.sb("grow", [1, 2, 1024], F32)
            kb.dma(kb.sp, brow[0:1, 0, :], I["ada_b"].t[l:l + 1, 2 * 1024:3 * 1024], w=[brow], sb=brow)
            kb.dma(kb.sp, brow[0:1, 1, :], I["ada_b"].t[l:l + 1, 5 * 1024:6 * 1024], w=[brow], sb=brow)
            kb.dma(kb.sp, grow[0:1, 0, :], I["norm_mix_post"].t[l:l + 1, :], w=[grow], sb=grow)
            kb.dma(kb.sp, grow[0:1, 1, :], I["norm_ffn_post"].t[l:l + 1, :], w=[grow], sb=grow)
            rows = kb.sb("rows", [1, 4, 1024], F32)
            wts = [kb.sb("adaw", [128, 8, 1024], F32) for _ in range(2)]
            psf = [kb.ps("adap", [128, 512]) for _ in range(2)]
            psr = [kb.ps("adar", [128, 512]) for _ in range(2)]
            pi = 0
            for i in range(6):
                wb = wts[i % 2]
                kb.dma(kb.sp, wb[:, :, :],
                       I["ada_w"].t[l, :, i * 1024:(i + 1) * 1024].rearrange("(kc p) m -> p kc m", p=128), w=[wb], sb=wb)
                for jb in range(8):
                    p = psf[pi % 2]
                    pi += 1
                    for kc in range(8):
                        mm(kb, p[:, 0:2], wb[:, kc, jb * 128:(jb + 1) * 128], sc[:, kc, :], kc == 0, kc == 7,
                           r=[wb, sc], w=[p])
                    j = i * 8 + jb
                    kb.op(kb.dve, lambda p=p, j=j: nc.vector.tensor_scalar(out=modf[:, j, :], in0=p[:, 0:2], scalar1=bf[:, j:j + 1],
                                                                         scalar2=None, op0=ALU.add), r=[p, bf], w=[modf])
                if i in (2, 5):
                    gi = 0 if i == 2 else 1
                    for s in range(2):
                        for h in range(2):
                            p = psr[(s * 2 + h) % 2]
                            for kc in range(8):
                                mm(kb, p[0:1, 0:512], sc[:, kc, s:s + 1], wb[:, kc, h * 512:(h + 1) * 512], kc == 0, kc == 7,
                                   r=[wb, sc], w=[p])
                            ro = rows[0:1, gi * 2 + s, h * 512:(h + 1) * 512]
                            kb.op(kb.dve, lambda p=p, ro=ro, gi=gi, h=h: nc.vector.tensor_tensor(
                                out=ro, in0=p[0:1, 0:512], in1=brow[0:1, gi, h * 512:(h + 1) * 512], op=ALU.add),
                                r=[p, brow], w=[rows])
                            kb.op(kb.dve, lambda ro=ro, gi=gi, h=h: nc.vector.tensor_tensor(
                                out=ro, in0=ro, in1=grow[0:1, gi, h * 512:(h + 1) * 512], op=ALU.mult),
                                r=[rows, grow], w=[rows])
            kb.dma(kb.sp, gmrow.t[:, :], rows[0:1, :, :], r=[rows], w=[gmrow], sb=rows)
            for which, (sh_i, sc_i, gidx) in enumerate(((0, 1, 0), (3, 4, 1))):
                for s in range(2):
                    a_out = ab[:, 2 * which, :, s]
                    b_out = ab[:, 2 * which + 1, :, s]
                    kb.op(kb.dve, lambda a_out=a_out, sc_i=sc_i, s=s, gidx=gidx: nc.vector.scalar_tensor_tensor(
                        out=a_out, in0=modf[:, sc_i * 8:(sc_i + 1) * 8, s], scalar=1.0, in1=gn[:, gidx, :],
                        op0=ALU.add, op1=ALU.mult), r=[modf, gn], w=[ab])
                    kb.op(kb.dve, lambda b_out=b_out, sh_i=sh_i, s=s: nc.vector.tensor_copy(
                        out=b_out, in_=modf[:, sh_i * 8:(sh_i + 1) * 8, s]), r=[modf], w=[ab])
            kb.wait_all(kb.dve, [ab])
        kb.stk = old
        for k in range(4):
            kb.dma(kb.sp, gm[:, k, :], gmrow.t[k:k + 1, :].partition_broadcast(128), r=[gmrow], w=[gm], sb=gm)
        res["ab"] = ab
        res["gm"] = gm
        res["modf"] = modf
        return res

    def norm_stats(self, xt, rstd, sq):
        raise NotImplementedError

    def prenorm_to_hxt(self, l, mods, XU, HXT, which=0):
        kb, nc = self.kb, self.nc
        ab = mods["ab"]
        old = kb.stk
        with ExitStack() as st:
            kb.stk = st
            xts = [kb.sb("xt", [128, 1024], F32) for _ in range(3)]
            sq = kb.sb("sq", [128, 1024], BF16)
            stats = [kb.sb("st", [128, 4], F32) for _ in range(3)]
            pts = [kb.ps("ptr", [128, 512]) for _ in range(4)]
            hbl = [kb.sb("hbl", [128, 8, 512], BF16) for _ in range(2)]
            ntile = self.NT // 128
            for i in range(ntile):
                seg = 1 if i < 2 else 0
                xt = xts[i % 3]
                stt = stats[i % 3]
                kb.dma(kb.sp, xt[:, :], XU.t[i * 128:(i + 1) * 128, :], r=[XU], w=[xt], sb=xt)
                self.rstd(xt[:, :], [xt], stt, sq)
                hb = hbl[(i // 4) % 2]
                self.norm_T(xt, [xt], stt, ab, 2 * which, seg, pts, (i % 2) * 2, hb, (i % 4) * 128)
                if i % 4 == 3 or i == ntile - 1:
                    n = ((i % 4) + 1) * 128
                    t0 = (i // 4) * 512
                    kb.dma(kb.sp, HXT.t.rearrange("(kc p) n -> p kc n", p=128)[:, :, t0:t0 + n], hb[:, :, 0:n],
                           r=[hb], w=[HXT], sb=hb)
        kb.stk = old

    def rstd(self, x_ap, xbufs, stt, sq):
        kb, nc = self.kb, self.nc
        kb.op(kb.act, lambda: nc.scalar.activation(out=sq[:, :], in_=x_ap, func=AF.Square, accum_out=stt[:, 0:1]),
              r=xbufs, w=[sq, stt])
        kb.op(kb.dve, lambda: nc.vector.tensor_scalar(out=stt[:, 2:3], in0=stt[:, 0:1], scalar1=1.0 / D, scalar2=EPS,
                                                     op0=ALU.mult, op1=ALU.add), r=[stt], w=[stt])
        kb.op(kb.act, lambda: nc.scalar.activation(out=stt[:, 3:4], in_=stt[:, 2:3], func=AF.Sqrt), r=[stt], w=[stt])
        kb.op(kb.dve, lambda: nc.vector.reciprocal(out=stt[:, 1:2], in_=stt[:, 3:4]), r=[stt], w=[stt])

    def norm_T(self, xt, xbufs, stt, ab, abi, seg, pts, pbase, hb, hoff, hf=None):
        kb, nc = self.kb, self.nc
        if stt is not None:
            kb.op(kb.dve, lambda: nc.vector.tensor_scalar(out=xt[:, :], in0=xt[:, :], scalar1=stt[:, 1:2], scalar2=None,
                                                         op0=ALU.mult), r=xbufs + [stt], w=[xt])
        for half in range(2):
            p = pts[pbase + half]
            for q in range(4):
                kc = half * 4 + q
                kb.op(kb.pe, lambda p=p, q=q, kc=kc: nc.tensor.transpose(out=p[:, q * 128:(q + 1) * 128],
                                                                         in_=xt[:, kc * 128:(kc + 1) * 128],
                                                                         identity=self.ident[:, :]),
                      r=[xt, self.ident], w=[p], sig=(q == 3))
            for q in range(4):
                kc = half * 4 + q
                eng = kb.dve if q % 2 == 0 else kb.act
                if eng is kb.dve:
                    kb.op(kb.dve, lambda p=p, q=q, kc=kc: nc.vector.tensor_scalar(
                        out=hb[:, kc, hoff:hoff + 128], in0=p[:, q * 128:(q + 1) * 128],
                        scalar1=ab[:, abi, kc, seg:seg + 1], scalar2=ab[:, abi + 1, kc, seg:seg + 1],
                        op0=ALU.mult, op1=ALU.add), r=[p, ab], w=[hb])
                else:
                    kb.op(kb.act, lambda p=p, q=q, kc=kc: nc.scalar.activation(
                        out=hb[:, kc, hoff:hoff + 128], in_=p[:, q * 128:(q + 1) * 128], func=AF.Identity,
                        scale=ab[:, abi, kc, seg:seg + 1], bias=ab[:, abi + 1, kc, seg:seg + 1]), r=[p, ab], w=[hb])
                if hf is not None:
                    kb.op(kb.dve, lambda p=p, q=q, kc=kc: nc.vector.tensor_scalar(
                        out=hf[:, kc, :], in0=p[:, q * 128:(q + 1) * 128],
                        scalar1=ab[:, abi, kc, seg:seg + 1], scalar2=ab[:, abi + 1, kc, seg:seg + 1],
                        op0=ALU.mult, op1=ALU.add), r=[p, ab], w=[hf])

    def stager(self, name, shape, dt, n=3):
        bufs = [self.kb.sb(name, shape, dt) for _ in range(n)]
        state = {"i": 0}

        def nxt():
            b = bufs[state["i"] % n]
            state["i"] += 1
            return b
        return nxt

    def proj(self, l, HXT, S):
        kb, nc, I = self.kb, self.nc, self.inp
        NT = self.NT
        old = kb.stk
        with ExitStack() as st:
            kb.stk = st
            gb = kb.sb("gbias", [128, 4], F32)
            with nc.allow_non_contiguous_dma(reason="tiny"):
                kb.dma(kb.sp, gb[:, :], I["gate_b_sp"].t[l, :, :].rearrange("k p -> p k"), w=[gb], sb=gb)
            stg_b = self.stager("stgb", [128, 512], BF16, 4)
            stg_f = self.stager("stgf", [128, 512], F32, 2)
            blockmap = {}
            for b in range(8):
                blockmap[b] = (S["QP"], b * 128, "copy", None)
            for b in range(4):
                blockmap[8 + b] = (S["NAQ"], b * 128, "scale", 0.125)
                blockmap[12 + b] = (S["NAK"], b * 128, "copy", None)
                blockmap[16 + b] = (S["S5U"], b * 128, "copy", None)
                blockmap[44 + b] = (S["GR"], b * 128, "bias", b)
            for b in range(24):
                blockmap[20 + b] = (S["GT"], b * 128, "sigmoid", None)
            cnt = {"i": 0}

            def epi(kb, pl, g, j, t0, tn):
                blk = g["b0"] + j
                dest, row0, kind, prm = blockmap[blk]
                p = pl[0]
                if kind == "bias":
                    sg = stg_f()
                    kb.op(kb.dve, lambda: nc.vector.tensor_scalar(out=sg[:, 0:tn], in0=p[:, 0:tn], scalar1=gb[:, prm:prm + 1],
                                                                 scalar2=None, op0=ALU.add), r=[p, gb], w=[sg])
                else:
                    sg = stg_b()
                    if kind == "sigmoid":
                        kb.op(kb.act, lambda: nc.scalar.activation(out=sg[:, 0:tn], in_=p[:, 0:tn], func=AF.Sigmoid), r=[p], w=[sg])
                    elif kind == "scale":
                        kb.op(kb.dve, lambda: nc.vector.tensor_scalar(out=sg[:, 0:tn], in0=p[:, 0:tn], scalar1=prm, scalar2=None,
                                                                     op0=ALU.mult), r=[p], w=[sg])
                    else:
                        cnt["i"] += 1
                        if cnt["i"] % 2:
                            kb.op(kb.dve, lambda: nc.vector.tensor_copy(out=sg[:, 0:tn], in_=p[:, 0:tn]), r=[p], w=[sg])
                        else:
                            kb.op(kb.act, lambda: nc.scalar.copy(out=sg[:, 0:tn], in_=p[:, 0:tn]), r=[p], w=[sg])
                kb.dma(kb.sp, dest.t[row0:row0 + 128, t0:t0 + tn], sg[:, 0:tn], r=[sg], w=[dest], sb=sg)

            groups = []
            for gi in range(6):
                groups.append(dict(streams=[(0, I["w_fm"].t[l, :, gi * 1024:(gi + 1) * 1024])], nblk=8, b0=gi * 8))
            gemm_fm(kb, NT, [(HXT, 8)], groups, epi, tag="pj")

            stg_t = self.stager("stgt", [128, 1024], BF16, 3)
            tiles = list(range(0, NT, 128))

            def epi_vo(kb, pl, t0):
                sg = stg_t()
                kb.op(kb.dve, lambda: nc.vector.tensor_copy(out=sg[:, 0:512], in_=pl[0][:, :]), r=[pl[0]], w=[sg])
                kb.op(kb.act, lambda: nc.scalar.activation(out=sg[:, 512:1024], in_=pl[1][:, :], func=AF.Sigmoid), r=[pl[1]], w=[sg])
                kb.dma(kb.sp, S["V"].t[t0:t0 + 128, :], sg[:, 0:512], r=[sg], w=[S["V"]], sb=sg)
                kb.dma(kb.sp, S["OS"].t[t0:t0 + 128, :], sg[:, 512:1024], r=[sg], w=[S["OS"]], sb=sg)

            gemm_tm(kb, tiles, HXT, 8, I["w_tm"].t[l, :, 0:1024], 1024, epi_vo, tag="vo")

            def epi_nv(kb, pl, t0):
                sg = stg_t()
                kb.op(kb.dve, lambda: nc.vector.tensor_copy(out=sg[:, 0:512], in_=pl[0][:, :]), r=[pl[0]], w=[sg])
                kb.dma(kb.sp, S["NAV"].t[t0:t0 + 128, :], sg[:, 0:512], r=[sg], w=[S["NAV"]], sb=sg)

            gemm_tm(kb, tiles, HXT, 8, I["w_tm"].t[l, :, 1024:1536], 512, epi_nv, tag="nv")
        kb.stk = old

    def mlstm_qk(self, l, S):
        kb, nc, I = self.kb, self.nc, self.inp
        NT, N = self.NT, self.N
        CH = min(2048, N)
        old = kb.stk
        with ExitStack() as st:
            kb.stk = st
            cos = kb.sb("rcos", [128, N], F32)
            sin = kb.sb("rsin", [128, N], F32)
            kb.dma(kb.sp, cos[:, :], I["rope_cos"].t[:, :], w=[cos], sb=cos)
            kb.dma(kb.sp, sin[:, :], I["rope_sin"].t[:, :], w=[sin], sb=sin)
            rmat = kb.sb("rmat", [128, 128], BF16)
            kb.dma(kb.pool, rmat[:, :], I["rope_rT"].t[:, :], w=[rmat], sb=rmat)
            cw = kb.sb("cw", [128, 8, 4], F32)
            with nc.allow_non_contiguous_dma(reason="tiny conv weights"):
                for j in range(3):
                    kb.dma(kb.sp, cw[:, :, j], I["m_conv_w"].t[l, j, :].rearrange("(b p) -> p b", p=128), w=[cw], sb=cw)
                kb.dma(kb.sp, cw[:, :, 3], I["m_conv_b"].t[l, :].rearrange("(b p) -> p b", p=128), w=[cw], sb=cw)
            xps = self.stager("xp", [128, CH + 2], BF16, 2)
            tms = self.stager("ctm", [128, CH], F32, 2)
            qss = self.stager("cqs", [128, CH], BF16, 2)
            t1s = self.stager("ct1", [128, 512], F32, 2)
            t2s = self.stager("ct2", [128, 512], F32, 2)
            outs = self.stager("cout", [128, CH], BF16, 2)
            pss = [kb.ps("cps", [128, 512]) for _ in range(2)]
            pi = 0
            segs = [(0, CTX, False)] + [(CTX + c0, min(CH, N - c0), True) for c0 in range(0, N, CH)]
            for fb in range(8):
                scale = 1.0 if fb < 4 else 128.0 ** -0.5
                for (u0, n, is_x) in segs:
                    seg_lo = CTX if is_x else 0
                    seg_hi = NT if is_x else CTX
                    xp, tm, qs, ob = xps(), tms(), qss(), outs()
                    lo = max(u0 - 1, seg_lo)
                    hi = min(u0 + n + 1, seg_hi)
                    if lo == u0:
                        kb.op(kb.pool, lambda xp=xp: nc.gpsimd.memset(xp[:, 0:1], 0.0), w=[xp])
                    if hi == u0 + n:
                        kb.op(kb.pool, lambda xp=xp, n=n: nc.gpsimd.memset(xp[:, n + 1:n + 2], 0.0), w=[xp])
                    kb.dma(kb.sp, xp[:, 1 - (u0 - lo):1 - (u0 - lo) + (hi - lo)], S["QP"].t[fb * 128:(fb + 1) * 128, lo:hi],
                           r=[S["QP"]], w=[xp], sb=xp)
                    kb.op(kb.dve, lambda: nc.vector.tensor_scalar(out=tm[:, 0:n], in0=xp[:, 0:n], scalar1=cw[:, fb, 0:1], scalar2=None,
                                                                 op0=ALU.mult), r=[xp, cw], w=[tm])
                    kb.op(kb.dve, lambda: nc.vector.scalar_tensor_tensor(out=tm[:, 0:n], in0=xp[:, 1:n + 1], scalar=cw[:, fb, 1:2],
                                                                        in1=tm[:, 0:n], op0=ALU.mult, op1=ALU.add), r=[xp, cw, tm], w=[tm])
                    kb.op(kb.dve, lambda: nc.vector.scalar_tensor_tensor(out=tm[:, 0:n], in0=xp[:, 2:n + 2], scalar=cw[:, fb, 2:3],
                                                                        in1=tm[:, 0:n], op0=ALU.mult, op1=ALU.add), r=[xp, cw, tm], w=[tm])
                    if not is_x:
                        kb.op(kb.act, lambda: nc.scalar.activation(out=tm[:, 0:n], in_=tm[:, 0:n], func=AF.Silu, bias=cw[:, fb, 3:4]),
                              r=[tm, cw], w=[tm])
                        kb.op(kb.dve, lambda: nc.vector.tensor_scalar(out=ob[:, 0:n], in0=tm[:, 0:n], scalar1=scale, scalar2=None, op0=ALU.mult),
                              r=[tm], w=[ob])
                    else:
                        kb.op(kb.act, lambda: nc.scalar.activation(out=qs[:, 0:n], in_=tm[:, 0:n], func=AF.Silu, bias=cw[:, fb, 3:4]),
                              r=[tm, cw], w=[qs])
                        x0 = u0 - CTX
                        for c0 in range(0, n, 512):
                            cn = min(512, n - c0)
                            p = pss[pi % 2]
                            pi += 1
                            t1, t2 = t1s(), t2s()
                            mm(kb, p[:, 0:cn], rmat[:, :], qs[:, c0:c0 + cn], True, True, r=[rmat, qs], w=[p])
                            kb.op(kb.dve, lambda: nc.vector.tensor_tensor(out=t1[:, 0:cn], in0=p[:, 0:cn], in1=sin[:, x0 + c0:x0 + c0 + cn],
                                                                         op=ALU.mult), r=[p, sin], w=[t1])
                            kb.op(kb.dve, lambda: nc.vector.tensor_tensor(out=t2[:, 0:cn], in0=qs[:, c0:c0 + cn], in1=cos[:, x0 + c0:x0 + c0 + cn],
                                                                          op=ALU.mult), r=[qs, cos], w=[t2])
                            kb.op(kb.dve, lambda: nc.vector.tensor_tensor(out=t2[:, 0:cn], in0=t1[:, 0:cn], in1=t2[:, 0:cn], op=ALU.add), r=[t1, t2], w=[t2])
                            kb.op(kb.dve, lambda: nc.vector.tensor_scalar(out=ob[:, c0:c0 + cn], in0=t2[:, 0:cn], scalar1=scale, scalar2=None,
                                                                          op0=ALU.mult), r=[t2], w=[ob])
                    kb.dma(kb.sp, S["QK"].t[fb * 128:(fb + 1) * 128, u0:u0 + n], ob[:, 0:n], r=[ob], w=[S["QK"]], sb=ob)
        kb.stk = old

    def mlstm_gates(self, l, S, TS, DEC):
        kb, nc, I = self.kb, self.nc, self.inp
        NT, N = self.NT, self.N
        nxc = N // 128
        PCx = min(16, nxc)
        assert nxc % PCx == 0
        PTM = PCx * 128
        old = kb.stk
        with ExitStack() as st:
            kb.stk = st
            sel = kb.sb("sel", [128, 4], F32)
            osg = kb.sb("osg", [128, 4, 128], F32)
            ones = kb.sb("gones", [128, PTM], F32)
            kb.op(kb.pool, lambda: nc.gpsimd.memset(sel[:, :], 0.0), w=[sel])
            kb.op(kb.pool, lambda: nc.gpsimd.memset(osg[:, :, :], 0.0), w=[osg])
            kb.op(kb.pool, lambda: nc.gpsimd.memset(ones[:, :], 1.0), w=[ones])
            for g in range(4):
                kb.op(kb.pool, lambda g=g: nc.gpsimd.memset(sel[32 * g:32 * g + 1, g:g + 1], 1.0), w=[sel])
                kb.op(kb.pool, lambda g=g: nc.gpsimd.memset(osg[32 * g:32 * g + 1, g, :], 1.0), w=[osg])
            Fb = kb.sb("gF", [128, PTM], F32)
            NB = kb.sb("gNB", [128, PTM], F32)
            A = kb.sb("gA", [128, PTM], F32)
            M = kb.sb("gM", [128, PTM], F32)
            MP = kb.sb("gMP", [128, PTM], F32)
            car = kb.sb("gcar", [128, 4], F32)
            dec = kb.sb("gdec", [128, 16], F32)
            pst = kb.ps("gpst", [128, 512])
            psd = kb.ps("gpsd", [128, 512])
            for d in range(2):
                kb.op(kb.dve, lambda: nc.vector.memset(car[:, :], 0.0), w=[car])
                pieces = [(0, 2)] + [(2 + j * PCx, PCx) for j in range(nxc // PCx)]
                for (pc0, pc) in pieces:
                    PT = pc * 128
                    if d == 0:
                        cu0 = pc0
                    else:
                        cu0 = 0 if pc0 == 0 else 2 + (nxc - (pc0 - 2) - pc)
                    u0 = cu0 * 128
                    fk, ik = (1, 0) if d == 0 else (3, 2)
                    if d == 0:
                        kb.dma(kb.sp, Fb[:, 0:PT], S["GR"].t[fk * 128:(fk + 1) * 128, u0:u0 + PT], r=[S["GR"]], w=[Fb], sb=Fb)
                        kb.dma(kb.sp, A[:, 0:PT], S["GR"].t[ik * 128:(ik + 1) * 128, u0:u0 + PT], r=[S["GR"]], w=[A], sb=A)
                    else:
                        kb.dma(kb.sp, MP[:, 0:PT], S["GR"].t[fk * 128:(fk + 1) * 128, u0:u0 + PT], r=[S["GR"]], w=[MP], sb=MP)
                        kb.dma(kb.sp, M[:, 0:PT], S["GR"].t[ik * 128:(ik + 1) * 128, u0:u0 + PT], r=[S["GR"]], w=[M], sb=M)
                        kb.op(kb.dve, lambda: nc.vector.tensor_copy(out=Fb[:, 0:PT], in_=MP[:, PT - 1::-1] if PT == PTM else MP[:, PT - 1::-1]),
                              r=[MP], w=[Fb])
                        kb.op(kb.dve, lambda: nc.vector.tensor_copy(out=A[:, 0:PT], in_=M[:, PT - 1::-1]), r=[M], w=[A])
                    kb.op(kb.act, lambda: nc.scalar.activation(out=Fb[:, 0:PT], in_=Fb[:, 0:PT], func=AF.Exp, scale=-1.0), r=[Fb], w=[Fb])
                    kb.op(kb.act, lambda: nc.scalar.activation(out=Fb[:, 0:PT], in_=Fb[:, 0:PT], func=AF.Ln, bias=1.0), r=[Fb], w=[Fb])
                    kb.op(kb.dve, lambda: nc.vector.tensor_tensor_scan(out=NB[:, 0:PT], data0=ones[:, 0:PT], data1=Fb[:, 0:PT],
                                                                      initial=car[:, 0:1], op0=ALU.mult, op1=ALU.add), r=[ones, Fb, car], w=[NB])
                    kb.op(kb.dve, lambda: nc.vector.tensor_tensor(out=A[:, 0:PT], in0=A[:, 0:PT], in1=NB[:, 0:PT], op=ALU.add), r=[A, NB], w=[A])
                    kb.op(kb.dve, lambda: nc.vector.tensor_tensor_scan(out=M[:, 0:PT], data0=A[:, 0:PT], data1=A[:, 0:PT],
                                                                      initial=car[:, 1:2], op0=ALU.max, op1=ALU.max), r=[A, car], w=[M])
                    kb.op(kb.dve, lambda: nc.vector.tensor_copy(out=MP[:, 0:128], in_=car[:, 1:2].to_broadcast([128, 128])), r=[car], w=[MP])
                    if pc > 1:
                        kb.op(kb.dve, lambda: nc.vector.tensor_copy(
                            out=MP[:, 128:PT].rearrange("p (c t) -> p c t", t=128),
                            in_=M[:, 127:PT - 128:128].unsqueeze(2).to_broadcast([128, pc - 1, 128])), r=[M], w=[MP])
                    kb.op(kb.dve, lambda: nc.vector.tensor_copy(out=car[:, 2:3], in_=NB[:, PT - 1:PT]), r=[NB], w=[car])
                    kb.op(kb.dve, lambda: nc.vector.tensor_copy(out=car[:, 3:4], in_=M[:, PT - 1:PT]), r=[M], w=[car])
                    kb.op(kb.dve, lambda: nc.vector.tensor_tensor(out=dec[:, 0:pc], in0=MP[:, 0:PT:128], in1=M[:, 127:PT:128], op=ALU.subtract),
                          r=[MP, M], w=[dec])
                    kb.op(kb.act, lambda: nc.scalar.activation(out=dec[:, 0:pc], in_=dec[:, 0:pc], func=AF.Exp), r=[dec], w=[dec])
                    kb.op(kb.dve, lambda: nc.vector.tensor_tensor(out=A[:, 0:PT], in0=A[:, 0:PT], in1=MP[:, 0:PT], op=ALU.subtract), r=[A, MP], w=[A])
                    kb.op(kb.act, lambda: nc.scalar.activation(out=A[:, 0:PT], in_=A[:, 0:PT], func=AF.Exp), r=[A], w=[A])
                    kb.op(kb.dve, lambda: nc.vector.tensor_tensor(out=NB[:, 0:PT], in0=NB[:, 0:PT], in1=MP[:, 0:PT], op=ALU.subtract), r=[NB, MP], w=[NB])
                    kb.op(kb.act, lambda: nc.scalar.activation(out=NB[:, 0:PT], in_=NB[:, 0:PT], func=AF.Exp), r=[NB], w=[NB])
                    kb.op(kb.dve, lambda: nc.vector.tensor_copy(out=car[:, 0:2], in_=car[:, 2:4]), r=[car], w=[car])
                    if d == 0:
                        om, th = A, NB
                    else:
                        kb.op(kb.dve, lambda: nc.vector.tensor_copy(out=Fb[:, 0:PT], in_=A[:, PT - 1::-1]), r=[A], w=[Fb])
                        kb.op(kb.dve, lambda: nc.vector.tensor_copy(out=MP[:, 0:PT], in_=NB[:, PT - 1::-1]), r=[NB], w=[MP])
                        om, th = Fb, MP
                    for j in range(pc):
                        mm(kb, pst[:, j * 8:j * 8 + 4], om[:, j * 128:(j + 1) * 128], sel[:, :], True, True, r=[om, sel], w=[pst], sig=False)
                        mm(kb, pst[:, j * 8 + 4:j * 8 + 8], th[:, j * 128:(j + 1) * 128], sel[:, :], True, True, r=[th, sel], w=[pst],
                           sig=(j == pc - 1))
                    kb.op(kb.dve, lambda: nc.vector.tensor_copy(out=TS[:, cu0:cu0 + pc, d, :],
                                                               in_=pst[:, 0:pc * 8].rearrange("p (c k) -> p c k", k=8)), r=[pst], w=[TS])
                    for g in range(4):
                        mm(kb, psd[:, g * 16:g * 16 + pc], osg[:, g, :], dec[:, 0:pc], True, True, r=[osg, dec], w=[psd], sig=(g == 3))
                    for g in range(4):
                        src = psd[:, g * 16:g * 16 + pc] if d == 0 else psd[:, g * 16 + pc - 1::-1][:, 0:pc] if False else None
                        if d == 0:
                            kb.op(kb.dve, lambda g=g: nc.vector.tensor_copy(out=DEC[:, cu0:cu0 + pc, d, g], in_=psd[:, g * 16:g * 16 + pc]),
                                  r=[psd], w=[DEC])
                        else:
                            kb.op(kb.dve, lambda g=g: nc.vector.tensor_copy(out=DEC[:, cu0:cu0 + pc, d, g],
                                                                           in_=psd[:, g * 16:g * 16 + pc][:, ::-1]), r=[psd], w=[DEC])
        kb.stk = old

    def mlstm_scan(self, l, S, TS, DEC):
        kb, nc, I = self.kb, self.nc, self.inp
        NT, N = self.NT, self.N
        nch = NT // 128
        old = kb.stk
        with ExitStack() as st:
            kb.stk = st
            nw = kb.sb("mnw", [128, 512], F32)
            kb.dma(kb.sp, nw[:, :], I["m_norm"].t[l:l + 1, :].partition_broadcast(128), w=[nw], sb=nw)
            CT32 = kb.sb("CT32", [128, 4, 129], F32)
            CTb = kb.sb("CTb", [128, 4, 129], BF16)
            qTs = self.stager("mq", [128, 4, 128], BF16, 2)
            kTs = self.stager("mk", [128, 4, 128], BF16, 2)
            vs = self.stager("mv", [128, 512], BF16, 2)
            kts = self.stager("mkt", [128, 4, 128], BF16, 2)
            vas = self.stager("mva", [128, 4, 129], BF16, 2)
            pTs = self.stager("mpT", [128, 4, 128], BF16, 2)
            hbs = self.stager("mh", [128, 4, 128], F32, 2)
            hfs = self.stager("mhf", [128, 512], F32, 2)
            oss = self.stager("mos", [128, 512], BF16, 2)
            sts = self.stager("mst", [128, 16], F32, 3)
            tts = self.stager("mtt", [128, 4, 129], F32, 2)
            cen = self.stager("mcen", [128, 4, 128], F32, 2)
            sqs = self.stager("msq", [128, 4, 128], F32, 2)
            ybs = self.stager("myb", [128, 512], BF16, 2)
            yTs = self.stager("myT", [128, 4, 128], BF16, 2)
            ps_s = [kb.ps("mps", [128, 512]) for _ in range(2)]
            ps_n = [kb.ps("mpn", [128, 512]) for _ in range(2)]
            ps_c = [kb.ps("mpc", [128, 512]) for _ in range(2)]
            ps_k = kb.ps("mpk", [128, 1024], BF16)
            for d in range(2):
                kb.op(kb.dve, lambda: nc.vector.memset(CT32[:, :, :], 0.0), w=[CT32])
                kb.op(kb.pool, lambda: nc.gpsimd.memset(CTb[:, :, :], 0.0), w=[CTb])
                order = list(range(nch)) if d == 0 else [1, 0] + list(range(nch - 1, 1, -1))
                mask = self.maskf if d == 0 else self.maskb
                for ci, c in enumerate(order):
                    t0 = c * 128
                    qT, kT, v = qTs(), kTs(), vs()
                    kb.dma(kb.sp, qT[:, :, :], S["QK"].t[0:512, t0:t0 + 128].rearrange("(h p) n -> p h n", p=128), r=[S["QK"]], w=[qT], sb=qT)
                    kb.dma(kb.sp, kT[:, :, :], S["QK"].t[512:1024, t0:t0 + 128].rearrange("(h p) n -> p h n", p=128), r=[S["QK"]], w=[kT], sb=kT)
                    kb.dma(kb.sp, v[:, :], S["V"].t[t0:t0 + 128, :], r=[S["V"]], w=[v], sb=v)
                    if d == 1:
                        hf, osb = hfs(), oss()
                        kb.dma(kb.sp, hf[:, :], S["HF"].t[t0:t0 + 128, :], r=[S["HF"]], w=[hf], sb=hf)
                        kb.dma(kb.sp, osb[:, :], S["OS"].t[t0:t0 + 128, :], r=[S["OS"]], w=[osb], sb=osb)
                    kt = kts()
                    for h in range(4):
                        kb.op(kb.pe, lambda h=h: nc.tensor.transpose(out=ps_k[:, h * 128:(h + 1) * 128], in_=kT[:, h, :], identity=self.identb[:, :]),
                              r=[kT, self.identb], w=[ps_k], sig=(h == 3))
                    kb.op(kb.act, lambda: nc.scalar.copy(out=kt[:, :, :], in_=ps_k[:, 0:512].rearrange("p (h e) -> p h e", e=128)), r=[ps_k], w=[kt])
                    va = vas()
                    for h in range(4):
                        kb.op(kb.dve,
                              (lambda h=h: nc.vector.tensor_scalar(out=va[:, h, 0:128], in0=v[:, h * 128:(h + 1) * 128], scalar1=TS[:, c, d, h:h + 1],
                                                                   scalar2=None, op0=ALU.mult)) if h % 2 == 0 else
                              (lambda h=h: nc.vector.tensor_scalar(out=va[:, h, 0:128], in0=v[:, h * 128:(h + 1) * 128], scalar1=TS[:, c, d, h:h + 1],
                                                                   scalar2=None, op0=ALU.mult)), r=[v, TS], w=[va])
                    kb.op(kb.dve, lambda: nc.vector.tensor_copy(out=va[:, :, 128], in_=TS[:, c, d, 0:4]), r=[TS], w=[va])
                    pS = ps_s[ci % 2]
                    for h in range(4):
                        mm(kb, pS[:, h * 128:(h + 1) * 128], kT[:, h, :], qT[:, h, :], True, True, r=[kT, qT], w=[pS], sig=(h == 3))
                    pT = pTs()
                    kb.op(kb.dve, lambda: nc.vector.tensor_tensor(out=pT[:, :, :], in0=pS[:, :].rearrange("p (h t) -> p h t", t=128),
                                                                 in1=mask[:, :].unsqueeze(1).to_broadcast([128, 4, 128]), op=ALU.mult),
                          r=[pS, mask], w=[pT])
                    hb = hbs()
                    stt = sts()
                    for half in range(2):
                        pN = ps_n[half]
                        for hh in range(2):
                            h = half * 2 + hh
                            mm(kb, pN[:, hh * 129:(hh + 1) * 129], pT[:, h, :], va[:, h, :], True, False, r=[pT, va], w=[pN], sig=False)
                            mm(kb, pN[:, hh * 129:(hh + 1) * 129], qT[:, h, :], CTb[:, h, :], False, True, r=[qT, CTb], w=[pN], sig=True)
                        for hh in range(2):
                            h = half * 2 + hh
                            kb.op(kb.act, lambda h=h, hh=hh, pN=pN: nc.scalar.activation(
                                out=stt[:, 8 + h:9 + h], in_=pN[:, hh * 129 + 128:hh * 129 + 129], func=AF.Abs), r=[pN], w=[stt])
                            kb.op(kb.dve, lambda h=h: nc.vector.tensor_scalar(
                                out=stt[:, h:h + 1], in0=stt[:, 8 + h:9 + h], scalar1=TS[:, c, d, 4 + h:5 + h], scalar2=None,
                                op0=ALU.max), r=[stt, TS], w=[stt])
                            kb.op(kb.dve, lambda h=h: nc.vector.reciprocal(out=stt[:, 4 + h:5 + h], in_=stt[:, h:h + 1]), r=[stt], w=[stt])
                            kb.op(kb.act, lambda h=h, hh=hh, pN=pN: nc.scalar.activation(out=hb[:, h, :], in_=pN[:, hh * 129:hh * 129 + 128], func=AF.Copy,
                                                                                        scale=stt[:, 4 + h:5 + h]), r=[pN, stt], w=[hb])
                    tt = tts()
                    for half in range(2):
                        pC = ps_c[half]
                        for hh in range(2):
                            h = half * 2 + hh
                            mm(kb, pC[:, hh * 129:(hh + 1) * 129], kt[:, h, :], va[:, h, :], True, True, r=[kt, va], w=[pC], sig=(hh == 1))
                        kb.op(kb.dve, lambda half=half, pC=pC: nc.vector.tensor_tensor(
                            out=tt[:, half * 2:half * 2 + 2, :], in0=pC[:, 0:258].rearrange("p (h e) -> p h e", e=129),
                            in1=CT32[:, half * 2:half * 2 + 2, :], op=ALU.add), r=[pC, CT32], w=[tt])
                    for h in range(4):
                        kb.op(kb.dve, lambda h=h: nc.vector.tensor_scalar(out=CT32[:, h, :], in0=tt[:, h, :], scalar1=DEC[:, c, d, h:h + 1], scalar2=None,
                                                                         op0=ALU.mult), r=[tt, DEC], w=[CT32])
                        kb.op(kb.act, lambda h=h: nc.scalar.activation(out=CTb[:, h, :], in_=tt[:, h, :], func=AF.Copy, scale=DEC[:, c, d, h:h + 1]),
                              r=[tt, DEC], w=[CTb])
                    if d == 0:
                        kb.dma(kb.sp, S["HF"].t[t0:t0 + 128, :], hb[:, :, :].rearrange("p h e -> p (h e)"), r=[hb], w=[S["HF"]], sb=hb)
                        continue
                    kb.op(kb.dve, lambda: nc.vector.tensor_tensor(out=hb[:, :, :], in0=hb[:, :, :], in1=hf[:, :].rearrange("p (h e) -> p h e", e=128),
                                                                  op=ALU.add), r=[hb, hf], w=[hb])
                    s2 = sts()
                    ce, sq, yb, yT = cen(), sqs(), ybs(), yTs()
                    kb.op(kb.dve, lambda: nc.vector.reduce_sum(out=s2[:, 0:4], in_=hb[:, :, :], axis=AX.X), r=[hb], w=[s2])
                    kb.op(kb.dve, lambda: nc.vector.tensor_scalar(out=s2[:, 0:4], in0=s2[:, 0:4], scalar1=1.0 / 128, scalar2=None, op0=ALU.mult),
                          r=[s2], w=[s2])
                    kb.op(kb.dve, lambda: nc.vector.tensor_tensor(out=ce[:, :, :], in0=hb[:, :, :], in1=s2[:, 0:4].unsqueeze(2).to_broadcast([128, 4, 128]),
                                                                 op=ALU.subtract), r=[hb, s2], w=[ce])
                    kb.op(kb.dve, lambda: nc.vector.tensor_tensor(out=sq[:, :, :], in0=ce[:, :, :], in1=ce[:, :, :], op=ALU.mult), r=[ce], w=[sq])
                    kb.op(kb.dve, lambda: nc.vector.reduce_sum(out=s2[:, 4:8], in_=sq[:, :, :], axis=AX.X), r=[sq], w=[s2])
                    kb.op(kb.dve, lambda: nc.vector.tensor_scalar(out=s2[:, 4:8], in0=s2[:, 4:8], scalar1=1.0 / 128, scalar2=EPS, op0=ALU.mult,
                                                                 op1=ALU.add), r=[s2], w=[s2])
                    kb.op(kb.act, lambda: nc.scalar.activation(out=s2[:, 8:12], in_=s2[:, 4:8], func=AF.Sqrt), r=[s2], w=[s2])
                    kb.op(kb.dve, lambda: nc.vector.reciprocal(out=s2[:, 12:16], in_=s2[:, 8:12]), r=[s2], w=[s2])
                    kb.op(kb.dve, lambda: nc.vector.tensor_tensor(out=ce[:, :, :], in0=ce[:, :, :], in1=s2[:, 12:16].unsqueeze(2).to_broadcast([128, 4, 128]),
                                                                 op=ALU.mult), r=[ce, s2], w=[ce])
                    kb.op(kb.dve, lambda: nc.vector.tensor_tensor(out=ce[:, :, :], in0=ce[:, :, :], in1=nw[:, :].rearrange("p (h e) -> p h e", e=128),
                                                                  op=ALU.mult), r=[ce, nw], w=[ce])
                    kb.op(kb.dve, lambda: nc.vector.tensor_tensor(out=yb[:, :], in0=ce[:, :, :].rearrange("p h e -> p (h e)"), in1=osb[:, :], op=ALU.mult),
                          r=[ce, osb], w=[yb])
                    for cc in range(4):
                        kb.op(kb.pe, lambda cc=cc: nc.tensor.transpose(out=ps_k[:, cc * 128:(cc + 1) * 128], in_=yb[:, cc * 128:(cc + 1) * 128],
                                                                       identity=self.identb[:, :]), r=[yb, self.identb], w=[ps_k], sig=(cc == 3))
                    kb.op(kb.act, lambda: nc.scalar.copy(out=yT[:, :, :], in_=ps_k[:, 0:512].rearrange("p (a b) -> p a b", b=128)), r=[ps_k], w=[yT])
                    kb.dma(kb.sp, S["YMT"].t.rearrange("(c p) n -> p c n", p=128)[:, :, t0:t0 + 128], yT[:, :, :], r=[yT], w=[S["YMT"]], sb=yT)
        kb.stk = old

    def mlstm(self, l, S):
        kb = self.kb
        nch = self.NT // 128
        old = kb.stk
        with ExitStack() as st:
            kb.stk = st
            import os as _os
            _sk = _os.environ.get("MLSKIP", "")
            if "qk" not in _sk:
                self.mlstm_qk(l, S)
            TS = kb.sb("TS", [128, nch, 2, 8], F32)
            DEC = kb.sb("DEC", [128, nch, 2, 4], F32)
            if "gates" not in _sk:
                self.mlstm_gates(l, S, TS, DEC)
            if "scan" in _sk:
                kb.stk = old
                return
            if "TSd" in self.taps:
                tsd = self.dscr("TSd", [128, nch * 16], F32)
                decd = self.dscr("DECd", [128, nch * 8], F32)
                kb.dma(kb.sp, tsd.t[:, :], TS[:, :, :, :].rearrange("p c d k -> p (c d k)"), r=[TS], w=[tsd], sb=TS)
                kb.dma(kb.sp, decd.t[:, :], DEC[:, :, :, :].rearrange("p c d k -> p (c d k)"), r=[DEC], w=[decd], sb=DEC)
            self.mlstm_scan(l, S, TS, DEC)
        kb.stk = old

    def sincos(self, arg, n, cos_out, sin_out, tmps):
        kb, nc = self.kb, self.nc
        argb, ta, tk, tr = tmps
        INV2PI, MAGIC, C1, C2, PIS = 0.15915494309189535, 12582912.0, 6.28125, 0.0019353071795864769, 3.1415925
        for shift, outb in ((0.0, sin_out), (1.5707963267948966, cos_out)):
            kb.op(kb.dve, lambda shift=shift: nc.vector.tensor_scalar(out=ta[:, 0:n], in0=arg, scalar1=shift, scalar2=None, op0=ALU.add),
                  r=[argb], w=[ta])
            kb.op(kb.dve, lambda: nc.vector.tensor_scalar(out=tk[:, 0:n], in0=ta[:, 0:n], scalar1=INV2PI, scalar2=MAGIC, op0=ALU.mult, op1=ALU.add),
                  r=[ta], w=[tk])
            kb.op(kb.dve, lambda: nc.vector.tensor_scalar(out=tk[:, 0:n], in0=tk[:, 0:n], scalar1=-MAGIC, scalar2=None, op0=ALU.add), r=[tk], w=[tk])
            kb.op(kb.dve, lambda: nc.vector.scalar_tensor_tensor(out=tr[:, 0:n], in0=tk[:, 0:n], scalar=-C1, in1=ta[:, 0:n], op0=ALU.mult, op1=ALU.add),
                  r=[tk, ta], w=[tr])
            kb.op(kb.dve, lambda: nc.vector.scalar_tensor_tensor(out=tr[:, 0:n], in0=tk[:, 0:n], scalar=-C2, in1=tr[:, 0:n], op0=ALU.mult, op1=ALU.add),
                  r=[tk, tr], w=[tr])
            kb.op(kb.dve, lambda: nc.vector.tensor_scalar(out=tr[:, 0:n], in0=tr[:, 0:n], scalar1=PIS, scalar2=-PIS, op0=ALU.min, op1=ALU.max),
                  r=[tr], w=[tr])
            ob, oap = outb
            kb.op(kb.act, lambda oap=oap: nc.scalar.activation(out=oap, in_=tr[:, 0:n], func=AF.Sin), r=[tr], w=[ob])

    def s5(self, l, S):
        kb, nc, I = self.kb, self.nc, self.inp
        NT, N = self.NT, self.N
        nch = NT // 128
        nxc = N // 128
        PCx = min(16, nxc)
        PTM = PCx * 128
        Lc = 128
        old = kb.stk
        with ExitStack() as st:
            kb.stk = st
            prm = kb.sb("s5prm", [128, 16, 2, 8], F32)
            raw = kb.sb("s5raw", [128, 2, 16, 4], F32)
            kb.dma(kb.sp, raw[:, :, :, :], I["s5p"].t[l, :, :, :, :].rearrange("d p q k -> p d q k"), w=[raw], sb=raw)
            dvec = kb.sb("s5d", [32, 16], F32)
            with nc.allow_non_contiguous_dma(reason="tiny skip vector"):
                kb.dma(kb.sp, dvec[:, :], I["s5_d"].t[l, :].rearrange("(q r) -> r q", r=32), w=[dvec], sb=dvec)
            jp1i = kb.sb("jp1i", [128, 128], I32)
            jp1 = kb.sb("jp1", [128, 128], F32)
            kb.op(kb.pool, lambda: nc.gpsimd.iota(jp1i[:, :], pattern=[[1, 128]], base=1, channel_multiplier=0), w=[jp1i])
            kb.op(kb.dve, lambda: nc.vector.tensor_copy(out=jp1[:, :], in_=jp1i[:, :]), r=[jp1i], w=[jp1])
            t4 = [kb.sb("s5t", [128, 128], F32) for _ in range(4)]
            for d in range(2):
                lre = raw[:, d, :, 0]
                kb.op(kb.dve, lambda: nc.vector.tensor_scalar(out=raw[:, d, :, 0], in0=raw[:, d, :, 0], scalar1=-1e-4, scalar2=None, op0=ALU.min),
                      r=[raw], w=[raw])
                kb.op(kb.act, lambda: nc.scalar.activation(out=raw[:, d, :, 2], in_=raw[:, d, :, 2], func=AF.Exp), r=[raw], w=[raw])
                kb.op(kb.dve, lambda: nc.vector.tensor_tensor(out=prm[:, :, d, 0], in0=raw[:, d, :, 0], in1=raw[:, d, :, 2], op=ALU.mult), r=[raw], w=[prm])
                kb.op(kb.dve, lambda: nc.vector.tensor_tensor(out=prm[:, :, d, 1], in0=raw[:, d, :, 1], in1=raw[:, d, :, 2], op=ALU.mult), r=[raw], w=[prm])
                kb.op(kb.act, lambda: nc.scalar.activation(out=prm[:, :, d, 2], in_=prm[:, :, d, 0], func=AF.Exp), r=[prm], w=[prm])
                kb.op(kb.dve, lambda: nc.vector.tensor_copy(out=t4[0][:, 0:16], in_=prm[:, :, d, 1]), r=[prm], w=[t4[0]])
                self.sincos(t4[0][:, 0:16], 16, (prm, prm[:, :, d, 5]), (prm, prm[:, :, d, 6]), (t4[0], t4[1], t4[2], t4[3]))
                nr, ni, den, tq = t4[0], t4[1], t4[2], t4[3]
                kb.op(kb.dve, lambda: nc.vector.tensor_tensor(out=nr[:, 0:16], in0=prm[:, :, d, 2], in1=prm[:, :, d, 5], op=ALU.mult), r=[prm], w=[nr])
                kb.op(kb.dve, lambda: nc.vector.tensor_scalar(out=nr[:, 0:16], in0=nr[:, 0:16], scalar1=-1.0, scalar2=None, op0=ALU.add), r=[nr], w=[nr])
                kb.op(kb.dve, lambda: nc.vector.tensor_tensor(out=ni[:, 0:16], in0=prm[:, :, d, 2], in1=prm[:, :, d, 6], op=ALU.mult), r=[prm], w=[ni])
                kb.op(kb.dve, lambda: nc.vector.tensor_tensor(out=den[:, 0:16], in0=raw[:, d, :, 0], in1=raw[:, d, :, 0], op=ALU.mult), r=[raw], w=[den])
                kb.op(kb.dve, lambda: nc.vector.tensor_tensor(out=tq[:, 0:16], in0=raw[:, d, :, 1], in1=raw[:, d, :, 1], op=ALU.mult), r=[raw], w=[tq])
                kb.op(kb.dve, lambda: nc.vector.tensor_tensor(out=den[:, 0:16], in0=den[:, 0:16], in1=tq[:, 0:16], op=ALU.add), r=[den, tq], w=[den])
                kb.op(kb.dve, lambda: nc.vector.reciprocal(out=den[:, 0:16], in_=den[:, 0:16]), r=[den], w=[den])
                kb.op(kb.dve, lambda: nc.vector.tensor_tensor(out=tq[:, 0:16], in0=nr[:, 0:16], in1=raw[:, d, :, 0], op=ALU.mult), r=[nr, raw], w=[tq])
                kb.op(kb.dve, lambda: nc.vector.tensor_tensor(out=tq[:, 16:32], in0=ni[:, 0:16], in1=raw[:, d, :, 1], op=ALU.mult), r=[ni, raw], w=[tq])
                kb.op(kb.dve, lambda: nc.vector.tensor_tensor(out=tq[:, 0:16], in0=tq[:, 0:16], in1=tq[:, 16:32], op=ALU.add), r=[tq], w=[tq])
                kb.op(kb.dve, lambda: nc.vector.tensor_tensor(out=prm[:, :, d, 3], in0=tq[:, 0:16], in1=den[:, 0:16], op=ALU.mult), r=[tq, den], w=[prm])
                kb.op(kb.dve, lambda: nc.vector.tensor_tensor(out=tq[:, 0:16], in0=ni[:, 0:16], in1=raw[:, d, :, 0], op=ALU.mult), r=[ni, raw], w=[tq])
                kb.op(kb.dve, lambda: nc.vector.tensor_tensor(out=tq[:, 16:32], in0=nr[:, 0:16], in1=raw[:, d, :, 1], op=ALU.mult), r=[nr, raw], w=[tq])
                kb.op(kb.dve, lambda: nc.vector.tensor_tensor(out=tq[:, 0:16], in0=tq[:, 0:16], in1=tq[:, 16:32], op=ALU.subtract), r=[tq], w=[tq])
                kb.op(kb.dve, lambda: nc.vector.tensor_tensor(out=prm[:, :, d, 4], in0=tq[:, 0:16], in1=den[:, 0:16], op=ALU.mult), r=[tq, den], w=[prm])
            PCx = min(8, nxc)
            PTM = PCx * 128
            U = kb.sb("s5u", [32, NT], BF16)
            Ur = kb.sb("s5ur", [32, NT], BF16)
            yt = kb.sb("s5y", [32, NT], F32)
            Braw = self.stager("s5b", [128, 2, 16], F32, 2)
            Craw = self.stager("s5c", [128, 2, 16], F32, 2)
            cpi = kb.sb("s5cpi", [128, 128], I32)
            cp1 = kb.sb("s5cp1", [128, 128], F32)
            kb.op(kb.pool, lambda: nc.gpsimd.iota(cpi[:, :], pattern=[[1, 128]], base=1, channel_multiplier=0), w=[cpi])
            kb.op(kb.dve, lambda: nc.vector.tensor_copy(out=cp1[:, :], in_=cpi[:, :]), r=[cpi], w=[cp1])
            BUF = []
            for d in range(2):
                B_ = dict(
                    bd=kb.sb("s5bd", [128, 2, 32], BF16), cd=kb.sb("s5cd", [128, 2, 32], BF16), bT=kb.sb("s5bT", [32, 2, 128], BF16),
                    bb=kb.sb("s5bb", [128, 2, 16], F32), COS=kb.sb("s5cos", [128, 128], F32), SIN=kb.sb("s5sin", [128, 128], F32),
                    RP=kb.sb("s5rp", [128, 128], F32), RHO0=kb.sb("s5rho0", [128, 128], F32), RHOP=kb.sb("s5rhop", [128, PTM], F32),
                    args=kb.sb("s5args", [128, 128], F32), cc=kb.sb("s5cc", [128, 12, 128], F32),
                    VR=kb.sb("s5vr", [128, PTM], BF16), VI=kb.sb("s5vi", [128, PTM], BF16), T1=kb.sb("s5t1", [128, PTM], BF16),
                    T2=kb.sb("s5t2", [128, PTM], BF16), SR=kb.sb("s5sr", [128, PTM], BF16), SI=kb.sb("s5si", [128, PTM], BF16),
                    COSb=kb.sb("s5cosb", [128, 128], BF16), SINb=kb.sb("s5sinb", [128, 128], BF16),
                    vb=[kb.sb("s5vb", [128, 2, 512], BF16) for _ in range(2)],
                    car=kb.sb("s5car", [128, 4], F32), tt=[kb.sb("s5tt", [128, 128], F32) for _ in range(3)],
                    psv=[kb.ps("s5pv", [128, 512]) for _ in range(2)], psy=kb.ps("s5py", [128, 512]), pst=kb.ps("s5pt", [128, 1024], BF16))
                BUF.append(B_)
            gout = self.stager("s5go", [32, PTM], BF16, 2)
            nxp = nxc // PCx
            T1, T2 = BUF[0]["T1"], BUF[0]["T2"]

            def run_dir(q, d):
                B_ = BUF[d]
                bd, cd, bT, bb, COS, SIN, RP, RHO0, RHOP = (B_[k] for k in ("bd", "cd", "bT", "bb", "COS", "SIN", "RP", "RHO0", "RHOP"))
                args, cc, VR, VI, T1, T2, SR, SI, car, tt = (B_[k] for k in ("args", "cc", "VR", "VI", "T1", "T2", "SR", "SI", "car", "tt"))
                psv, psy, pst = B_["psv"], B_["psy"], B_["pst"]
                Usrc = U if d == 0 else Ur
                xs, th, rho = prm[:, q, d, 0:1], prm[:, q, d, 1:2], prm[:, q, d, 2:3]
                br, cr = Braw(), Craw()
                kb.dma(kb.sp, br[:, :, :], I["s5B"].t[l, d, q, :, :, :], w=[br], sb=br)
                kb.dma(kb.sp, cr[:, :, :], I["s5CT"].t[l, d, q, :, :, :], w=[cr], sb=cr)
                fre, fim = prm[:, q, d, 3:4], prm[:, q, d, 4:5]
                kb.op(kb.dve, lambda: nc.vector.tensor_scalar(out=bb[:, 0, :], in0=br[:, 0, :], scalar1=fre, scalar2=None, op0=ALU.mult), r=[br, prm], w=[bb])
                kb.op(kb.dve, lambda: nc.vector.tensor_scalar(out=bb[:, 1, :], in0=br[:, 1, :], scalar1=fim, scalar2=None, op0=ALU.mult), r=[br, prm], w=[bb])
                kb.op(kb.dve, lambda: nc.vector.tensor_tensor(out=bb[:, 0, :], in0=bb[:, 0, :], in1=bb[:, 1, :], op=ALU.subtract), r=[bb], w=[bb])
                kb.op(kb.dve, lambda: nc.vector.tensor_scalar(out=bb[:, 1, :], in0=br[:, 1, :], scalar1=fre, scalar2=None, op0=ALU.mult), r=[br, prm], w=[bb])
                kb.op(kb.dve, lambda: nc.vector.scalar_tensor_tensor(out=bb[:, 1, :], in0=br[:, 0, :], scalar=fim, in1=bb[:, 1, :], op0=ALU.mult,
                                                                    op1=ALU.add), r=[br, prm, bb], w=[bb])
                kb.op(kb.pool, lambda: nc.gpsimd.memset(bd[:, :, :], 0.0), w=[bd])
                kb.op(kb.pool, lambda: nc.gpsimd.memset(cd[:, :, :], 0.0), w=[cd])
                yield
                for gh in range(2):
                    ps_, pe_ = gh * 64, gh * 64 + 64
                    kb.op(kb.dve, lambda: nc.vector.tensor_copy(out=bd[ps_:pe_, :, gh * 16:gh * 16 + 16], in_=bb[ps_:pe_, :, :]), r=[bb], w=[bd])
                    kb.op(kb.dve, lambda: nc.vector.tensor_copy(out=cd[ps_:pe_, 0, gh * 16:gh * 16 + 16], in_=cr[ps_:pe_, 0, :]), r=[cr], w=[cd])
                    kb.op(kb.dve, lambda: nc.vector.tensor_scalar(out=cd[ps_:pe_, 1, gh * 16:gh * 16 + 16], in0=cr[ps_:pe_, 1, :],
                                                                 scalar1=-1.0, scalar2=None, op0=ALU.mult), r=[cr], w=[cd])
                for ri in range(2):
                    kb.op(kb.pe, lambda: nc.tensor.transpose(out=pst[0:32, ri * 128:(ri + 1) * 128], in_=bd[:, ri, :], identity=self.identb[:, :]),
                          r=[bd, self.identb], w=[pst], sig=(ri == 1))
                kb.op(kb.act, lambda: nc.scalar.copy(out=bT[:, :, :], in_=pst[0:32, 0:256].rearrange("p (a b) -> p a b", b=128)), r=[pst], w=[bT])
                yield
                kb.op(kb.dve, lambda: nc.vector.tensor_scalar(out=args[:, :], in0=jp1[:, :], scalar1=th, scalar2=None, op0=ALU.mult), r=[jp1, prm], w=[args])
                self.sincos(args[:, :], 128, (COS, COS[:, :]), (SIN, SIN[:, :]), (args, tt[0], tt[1], tt[2]))
                kb.op(kb.act, lambda: nc.scalar.activation(out=RP[:, :], in_=jp1[:, :], func=AF.Exp, scale=xs), r=[jp1, prm], w=[RP])
                COSb, SINb = B_["COSb"], B_["SINb"]
                kb.op(kb.act, lambda: nc.scalar.copy(out=COSb[:, :], in_=COS[:, :]), r=[COS], w=[COSb])
                kb.op(kb.act, lambda: nc.scalar.copy(out=SINb[:, :], in_=SIN[:, :]), r=[SIN], w=[SINb])
                kb.op(kb.dve, lambda: nc.vector.tensor_copy(out=RHO0[:, :], in_=rho.to_broadcast([128, 128])), r=[prm], w=[RHO0])
                kb.op(kb.dve, lambda: nc.vector.memset(RHO0[:, 0:1], 0.0), w=[RHO0])
                kb.op(kb.dve, lambda: nc.vector.tensor_copy(out=RHOP[:, :].rearrange("p (c t) -> p c t", t=128),
                                                            in_=RHO0[:, :].unsqueeze(1).to_broadcast([128, PCx, 128])), r=[RHO0], w=[RHOP])
                yield
                kb.op(kb.dve, lambda: nc.vector.tensor_scalar(out=cc[:, 11, :], in0=cp1[:, :], scalar1=th, scalar2=128.0, op0=ALU.mult, op1=ALU.mult),
                      r=[cp1, prm], w=[cc])
                kb.op(kb.dve, lambda: nc.vector.tensor_copy(out=args[:, :], in_=cc[:, 11, :]), r=[cc], w=[args])
                self.sincos(args[:, :], 128, (cc, cc[:, 0, :]), (cc, cc[:, 1, :]), (args, tt[0], tt[1], tt[2]))
                kb.op(kb.act, lambda: nc.scalar.activation(out=cc[:, 2, :], in_=RP[:, 127:128].to_broadcast([128, 128]), func=AF.Copy), r=[RP], w=[cc])
                kb.op(kb.dve, lambda: nc.vector.memset(car[:, :], 0.0), w=[car])
                yield
                pieces = [(0, 2)] + [(2 + j * PCx, PCx) for j in range(nxp)]
                for k, (pc0, pc) in enumerate(pieces):
                    PT = pc * 128
                    p0 = pc0 * 128
                    for b0 in range(0, PT, 512):
                        bn = min(512, PT - b0)
                        pv = psv
                        mm(kb, pv[0][:, 0:bn], bT[:, 0, :], Usrc[:, p0 + b0:p0 + b0 + bn], True, True, r=[bT, Usrc], w=[pv[0]])
                        mm(kb, pv[1][:, 0:bn], bT[:, 1, :], Usrc[:, p0 + b0:p0 + b0 + bn], True, True, r=[bT, Usrc], w=[pv[1]])
                        nb = bn // 128
                        vbb = B_["vb"][(b0 // 512) % 2]
                        kb.op(kb.act, lambda: nc.scalar.copy(out=vbb[:, 0, 0:bn], in_=pv[0][:, 0:bn]), r=[pv[0]], w=[vbb])
                        kb.op(kb.act, lambda: nc.scalar.copy(out=vbb[:, 1, 0:bn], in_=pv[1][:, 0:bn]), r=[pv[1]], w=[vbb])
                        cb = B_["COSb"][:, :].unsqueeze(1).to_broadcast([128, nb, 128])
                        sb_ = B_["SINb"][:, :].unsqueeze(1).to_broadcast([128, nb, 128])
                        v3 = lambda t: t[:, b0:b0 + bn].rearrange("p (c t) -> p c t", t=128)
                        p3 = lambda ri: vbb[:, ri, 0:bn].rearrange("p (c t) -> p c t", t=128)
                        kb.op(kb.dve, lambda: nc.vector.tensor_tensor(out=v3(VR), in0=p3(0), in1=cb, op=ALU.mult), r=[vbb, B_["COSb"]], w=[VR])
                        kb.op(kb.dve, lambda: nc.vector.tensor_tensor(out=v3(T1), in0=p3(1), in1=sb_, op=ALU.mult), r=[vbb, B_["SINb"]], w=[T1])
                        kb.op(kb.dve, lambda: nc.vector.tensor_tensor(out=v3(VI), in0=p3(1), in1=cb, op=ALU.mult), r=[vbb, B_["COSb"]], w=[VI])
                        kb.op(kb.dve, lambda: nc.vector.tensor_tensor(out=v3(T2), in0=p3(0), in1=sb_, op=ALU.mult), r=[vbb, B_["SINb"]], w=[T2])
                        yield
                    kb.op(kb.dve, lambda: nc.vector.tensor_tensor(out=VR[:, 0:PT], in0=VR[:, 0:PT], in1=T1[:, 0:PT], op=ALU.add), r=[VR, T1], w=[VR])
                    kb.op(kb.dve, lambda: nc.vector.tensor_tensor(out=VI[:, 0:PT], in0=VI[:, 0:PT], in1=T2[:, 0:PT], op=ALU.subtract), r=[VI, T2], w=[VI])
                    yield
                    kb.op(kb.dve, lambda: nc.vector.tensor_tensor_scan(out=T1[:, 0:PT], data0=RHOP[:, 0:PT], data1=VR[:, 0:PT], initial=0.0,
                                                                      op0=ALU.mult, op1=ALU.add), r=[RHOP, VR], w=[T1])
                    kb.op(kb.dve, lambda: nc.vector.tensor_tensor_scan(out=T2[:, 0:PT], data0=RHOP[:, 0:PT], data1=VI[:, 0:PT], initial=0.0,
                                                                      op0=ALU.mult, op1=ALU.add), r=[RHOP, VI], w=[T2])
                    yield
                    zr, zi = T1[:, 127:PT:128], T2[:, 127:PT:128]
                    cL, sL = COS[:, 127:128], SIN[:, 127:128]
                    kb.op(kb.dve, lambda: nc.vector.tensor_scalar(out=cc[:, 3, 0:pc], in0=zr, scalar1=cL, scalar2=None, op0=ALU.mult), r=[T1, COS], w=[cc])
                    kb.op(kb.dve, lambda: nc.vector.tensor_scalar(out=cc[:, 9, 0:pc], in0=zi, scalar1=sL, scalar2=None, op0=ALU.mult), r=[T2, SIN], w=[cc])
                    kb.op(kb.dve, lambda: nc.vector.tensor_tensor(out=cc[:, 3, 0:pc], in0=cc[:, 3, 0:pc], in1=cc[:, 9, 0:pc], op=ALU.subtract), r=[cc], w=[cc])
                    kb.op(kb.dve, lambda: nc.vector.tensor_scalar(out=cc[:, 4, 0:pc], in0=zr, scalar1=sL, scalar2=None, op0=ALU.mult), r=[T1, SIN], w=[cc])
                    kb.op(kb.dve, lambda: nc.vector.scalar_tensor_tensor(out=cc[:, 4, 0:pc], in0=zi, scalar=cL, in1=cc[:, 4, 0:pc], op0=ALU.mult, op1=ALU.add),
                          r=[T2, COS, cc], w=[cc])
                    cC, sC = cc[:, 0, pc0:pc0 + pc], cc[:, 1, pc0:pc0 + pc]
                    kb.op(kb.dve, lambda: nc.vector.tensor_tensor(out=cc[:, 5, 0:pc], in0=cc[:, 3, 0:pc], in1=cC, op=ALU.mult), r=[cc], w=[cc])
                    kb.op(kb.dve, lambda: nc.vector.tensor_tensor(out=cc[:, 9, 0:pc], in0=cc[:, 4, 0:pc], in1=sC, op=ALU.mult), r=[cc], w=[cc])
                    kb.op(kb.dve, lambda: nc.vector.tensor_tensor(out=cc[:, 5, 0:pc], in0=cc[:, 5, 0:pc], in1=cc[:, 9, 0:pc], op=ALU.add), r=[cc], w=[cc])
                    kb.op(kb.dve, lambda: nc.vector.tensor_tensor(out=cc[:, 6, 0:pc], in0=cc[:, 4, 0:pc], in1=cC, op=ALU.mult), r=[cc], w=[cc])
                    kb.op(kb.dve, lambda: nc.vector.tensor_tensor(out=cc[:, 9, 0:pc], in0=cc[:, 3, 0:pc], in1=sC, op=ALU.mult), r=[cc], w=[cc])
                    kb.op(kb.dve, lambda: nc.vector.tensor_tensor(out=cc[:, 6, 0:pc], in0=cc[:, 6, 0:pc], in1=cc[:, 9, 0:pc], op=ALU.subtract), r=[cc], w=[cc])
                    yield
                    kb.op(kb.dve, lambda: nc.vector.tensor_tensor_scan(out=cc[:, 9, 0:pc], data0=cc[:, 2, 0:pc], data1=cc[:, 5, 0:pc], initial=car[:, 0:1],
                                                                      op0=ALU.mult, op1=ALU.add), r=[cc, car], w=[cc])
                    kb.op(kb.dve, lambda: nc.vector.tensor_tensor_scan(out=cc[:, 10, 0:pc], data0=cc[:, 2, 0:pc], data1=cc[:, 6, 0:pc], initial=car[:, 1:2],
                                                                      op0=ALU.mult, op1=ALU.add), r=[cc, car], w=[cc])
                    kb.op(kb.dve, lambda: nc.vector.tensor_copy(out=cc[:, 7, 0:1], in_=car[:, 2:3]), r=[car], w=[cc])
                    kb.op(kb.dve, lambda: nc.vector.tensor_copy(out=cc[:, 8, 0:1], in_=car[:, 3:4]), r=[car], w=[cc])
                    kb.op(kb.dve, lambda: nc.vector.tensor_tensor(out=cc[:, 7, 1:pc + 1], in0=cc[:, 9, 0:pc], in1=cC, op=ALU.mult), r=[cc], w=[cc])
                    kb.op(kb.dve, lambda: nc.vector.tensor_tensor(out=cc[:, 5, 0:pc], in0=cc[:, 10, 0:pc], in1=sC, op=ALU.mult), r=[cc], w=[cc])
                    kb.op(kb.dve, lambda: nc.vector.tensor_tensor(out=cc[:, 7, 1:pc + 1], in0=cc[:, 7, 1:pc + 1], in1=cc[:, 5, 0:pc], op=ALU.subtract), r=[cc], w=[cc])
                    kb.op(kb.dve, lambda: nc.vector.tensor_tensor(out=cc[:, 8, 1:pc + 1], in0=cc[:, 9, 0:pc], in1=sC, op=ALU.mult), r=[cc], w=[cc])
                    kb.op(kb.dve, lambda: nc.vector.tensor_tensor(out=cc[:, 5, 0:pc], in0=cc[:, 10, 0:pc], in1=cC, op=ALU.mult), r=[cc], w=[cc])
                    kb.op(kb.dve, lambda: nc.vector.tensor_tensor(out=cc[:, 8, 1:pc + 1], in0=cc[:, 8, 1:pc + 1], in1=cc[:, 5, 0:pc], op=ALU.add), r=[cc], w=[cc])
                    kb.op(kb.dve, lambda: nc.vector.tensor_copy(out=car[:, 0:1], in_=cc[:, 9, pc - 1:pc]), r=[cc], w=[car])
                    kb.op(kb.dve, lambda: nc.vector.tensor_copy(out=car[:, 1:2], in_=cc[:, 10, pc - 1:pc]), r=[cc], w=[car])
                    kb.op(kb.dve, lambda: nc.vector.tensor_copy(out=car[:, 2:3], in_=cc[:, 7, pc:pc + 1]), r=[cc], w=[car])
                    kb.op(kb.dve, lambda: nc.vector.tensor_copy(out=car[:, 3:4], in_=cc[:, 8, pc:pc + 1]), r=[cc], w=[car])
                    yield
                    rpb = RP[:, :].unsqueeze(1).to_broadcast([128, pc, 128])
                    z3 = lambda t: t[:, 0:PT].rearrange("p (c t) -> p c t", t=128)
                    for c_i in range(pc):
                        kb.op(kb.act, lambda: nc.scalar.activation(out=VR[:, c_i * 128:(c_i + 1) * 128], in_=RP[:, :], func=AF.Copy,
                                                                   scale=cc[:, 7, c_i:c_i + 1]), r=[RP, cc], w=[VR])
                        kb.op(kb.act, lambda: nc.scalar.activation(out=VI[:, c_i * 128:(c_i + 1) * 128], in_=RP[:, :], func=AF.Copy,
                                                                   scale=cc[:, 8, c_i:c_i + 1]), r=[RP, cc], w=[VI])
                    kb.op(kb.dve, lambda: nc.vector.tensor_tensor(out=T1[:, 0:PT], in0=T1[:, 0:PT], in1=VR[:, 0:PT], op=ALU.add), r=[T1, VR], w=[T1])
                    kb.op(kb.dve, lambda: nc.vector.tensor_tensor(out=T2[:, 0:PT], in0=T2[:, 0:PT], in1=VI[:, 0:PT], op=ALU.add), r=[T2, VI], w=[T2])
                    yield
                    cb = COSb[:, :].unsqueeze(1).to_broadcast([128, pc, 128])
                    sb_ = SINb[:, :].unsqueeze(1).to_broadcast([128, pc, 128])
                    kb.op(kb.dve, lambda: nc.vector.tensor_tensor(out=z3(VR), in0=z3(T1), in1=cb, op=ALU.mult), r=[T1, COSb], w=[VR])
                    kb.op(kb.dve, lambda: nc.vector.tensor_tensor(out=z3(VI), in0=z3(T2), in1=sb_, op=ALU.mult), r=[T2, SINb], w=[VI])
                    kb.op(kb.dve, lambda: nc.vector.tensor_tensor(out=SR[:, 0:PT], in0=VR[:, 0:PT], in1=VI[:, 0:PT], op=ALU.subtract), r=[VR, VI], w=[SR])
                    yield
                    kb.op(kb.dve, lambda: nc.vector.tensor_tensor(out=z3(VR), in0=z3(T1), in1=sb_, op=ALU.mult), r=[T1, SINb], w=[VR])
                    kb.op(kb.dve, lambda: nc.vector.tensor_tensor(out=z3(VI), in0=z3(T2), in1=cb, op=ALU.mult), r=[T2, COSb], w=[VI])
                    kb.op(kb.dve, lambda: nc.vector.tensor_tensor(out=SI[:, 0:PT], in0=VR[:, 0:PT], in1=VI[:, 0:PT], op=ALU.add), r=[VR, VI], w=[SI])
                    yield
                    if k == 0:
                        second = (d == 1)
                    else:
                        other_k = nxp - (k - 1)
                        second = (other_k < k) or (other_k == k and d == 1)
                    for b0 in range(0, PT, 512):
                        bn = min(512, PT - b0)
                        py = psy
                        mm(kb, py[0:32, 0:bn], cd[:, 0, :], SR[:, b0:b0 + bn], True, False, r=[cd, SR], w=[py], sig=False)
                        mm(kb, py[0:32, 0:bn], cd[:, 1, :], SI[:, b0:b0 + bn], False, True, r=[cd, SI], w=[py], sig=True)
                        pp = p0 + b0
                        if d == 0:
                            u_lo, u_hi = pp, pp + bn - 1
                            src = py[0:32, 0:bn]
                        else:
                            u_hi = (CTX - 1 - pp) if pp < CTX else (NT - 1 - (pp - CTX))
                            u_lo = u_hi - bn + 1
                            src = py[0:32, 0:bn][:, ::-1]
                        if second:
                            kb.op(kb.dve, lambda: nc.vector.tensor_tensor(out=yt[:, u_lo:u_hi + 1], in0=src, in1=yt[:, u_lo:u_hi + 1], op=ALU.add),
                                  r=[py, yt], w=[yt])
                        else:
                            kb.op(kb.dve, lambda: nc.vector.tensor_copy(out=yt[:, u_lo:u_hi + 1], in_=src), r=[py], w=[yt])
                        yield

            for q in range(16):
                kb.dma(kb.sp, U[:, :], S["S5U"].t[32 * q:32 * q + 32, :], r=[S["S5U"]], w=[U], sb=U)
                kb.op(kb.dve, lambda: nc.vector.tensor_copy(out=Ur[:, 0:CTX], in_=U[:, CTX - 1::-1]), r=[U], w=[Ur])
                kb.op(kb.dve, lambda: nc.vector.tensor_copy(out=Ur[:, CTX:NT], in_=U[:, NT - 1:CTX - 1:-1]), r=[U], w=[Ur])
                gens = [run_dir(q, 0), run_dir(q, 1)]
                alive = [True, True]
                while any(alive):
                    for gi_, g_ in enumerate(gens):
                        if alive[gi_]:
                            try:
                                next(g_)
                            except StopIteration:
                                alive[gi_] = False
                for g0 in range(0, NT, PTM):
                    gn = min(PTM, NT - g0)
                    go = gout()
                    ysl, usl = yt[:, g0:g0 + gn], U[:, g0:g0 + gn]
                    G1, G2 = BUF[0]["RHOP"], BUF[1]["RHOP"]
                    a1, a2 = G1[0:32, 0:gn], G2[0:32, 0:gn]
                    kb.op(kb.dve, lambda: nc.vector.scalar_tensor_tensor(out=ysl, in0=usl, scalar=dvec[:, q:q + 1], in1=ysl, op0=ALU.mult, op1=ALU.add),
                          r=[U, dvec, yt], w=[yt])
                    kb.op(kb.dve, lambda: nc.vector.tensor_tensor(out=a1, in0=ysl, in1=ysl, op=ALU.mult), r=[yt], w=[G1])
                    kb.op(kb.dve, lambda: nc.vector.tensor_scalar(out=a1, in0=a1, scalar1=0.044715, scalar2=1.0, op0=ALU.mult, op1=ALU.add), r=[G1], w=[G1])
                    kb.op(kb.dve, lambda: nc.vector.tensor_tensor(out=a1, in0=a1, in1=ysl, op=ALU.mult), r=[G1, yt], w=[G1])
                    kb.op(kb.act, lambda: nc.scalar.activation(out=a2, in_=a1, func=AF.Sigmoid, scale=1.5957691216057308), r=[G1], w=[G2])
                    kb.op(kb.dve, lambda: nc.vector.tensor_tensor(out=go[:, 0:gn], in0=a2, in1=ysl, op=ALU.mult), r=[G2, yt], w=[go])
                    kb.dma(kb.sp, S["GS"].t[32 * q:32 * q + 32, g0:g0 + gn], go[:, 0:gn], r=[go], w=[S["GS"]], sb=go)
        kb.stk = old
        with ExitStack() as st:
            kb.stk = st
            gb = kb.sb("glub", [128, 4], F32)
            with nc.allow_non_contiguous_dma(reason="tiny bias"):
                kb.dma(kb.sp, gb[:, :], I["s5_glu_b"].t[l, :].rearrange("(j p) -> p j", p=128), w=[gb], sb=gb)
            gts = self.stager("glug", [128, 512], BF16, 3)
            sgs = self.stager("glus", [128, 512], BF16, 3)
            ots = self.stager("gluo", [128, 512], BF16, 3)

            def epi(kb, pl, g, j, t0, tn):
                gt, sg, ot = gts(), sgs(), ots()
                kb.dma(kb.sp, gt[:, 0:tn], S["GS"].t[j * 128:(j + 1) * 128, t0:t0 + tn], r=[S["GS"]], w=[gt], sb=gt)
                kb.op(kb.act, lambda: nc.scalar.activation(out=sg[:, 0:tn], in_=pl[0][:, 0:tn], func=AF.Sigmoid, bias=gb[:, j:j + 1]), r=[pl[0], gb], w=[sg])
                kb.op(kb.dve, lambda: nc.vector.tensor_tensor(out=ot[:, 0:tn], in0=sg[:, 0:tn], in1=gt[:, 0:tn], op=ALU.mult), r=[sg, gt], w=[ot])
                kb.dma(kb.sp, S["YST"].t[j * 128:(j + 1) * 128, t0:t0 + tn], ot[:, 0:tn], r=[ot], w=[S["YST"]], sb=ot)

            gemm_fm(kb, NT, [(S["GS"], 4)], [dict(streams=[(0, I["s5_glu_w"].t[l, :, :])], nblk=4)], epi, wbufs=1, tag="glu")
        kb.stk = old

    def na_plan(self):
        N = self.N
        nq = N // 128
        rows = N // GRID_W
        win_r = min(8, rows)
        nkt = min(5, nq)
        plan, cases = [], {}
        for qt in range(nq):
            kt0 = int(np.clip(qt - 2, 0, nq - nkt))
            r0s = tuple(int(np.clip(r - win_r // 2, 0, rows - win_r)) - 2 * kt0 for r in (2 * qt, 2 * qt + 1))
            key = (qt - kt0,) + r0s
            if key not in cases:
                cases[key] = len(cases)
            plan.append((kt0, cases[key]))
        return plan, cases, nkt, win_r

    def na(self, l, S):
        kb, nc, I = self.kb, self.nc, self.inp
        NT, N = self.NT, self.N
        plan, cases, nkt, win_r = self.na_plan()
        nloc = nkt * 128
        old = kb.stk
        with ExitStack() as st:
            kb.stk = st
            kctx = kb.sb("nkc", [128, 4, CTX], BF16)
            vctx = kb.sb("nvc", [128, 2, 512], BF16)
            kb.dma(kb.sp, kctx[:, :, :], S["NAK"].t.rearrange("(c p) n -> p c n", p=128)[:, :, 0:CTX], r=[S["NAK"]], w=[kctx], sb=kctx)
            kb.dma(kb.sp, vctx[:, :, :], S["NAV"].t[0:CTX, :].rearrange("(i p) m -> p i m", p=128), r=[S["NAV"]], w=[vctx], sb=vctx)
            am = kb.sb("nam", [128, 8, nloc], F32)
            qts = self.stager("nq", [128, 4, 128], BF16, 2)
            kts = self.stager("nk", [128, 4, nloc], BF16, 2)
            vts = self.stager("nv", [128, nkt, 512], BF16, 2)
            sbs = self.stager("ns", [128, nloc + CTX], F32, 4)
            pbs = self.stager("np", [128, nloc + CTX], BF16, 4)
            ptb = self.stager("npt", [128, nkt + 2, 128], BF16, 4)
            sts = self.stager("nst", [128, 4], F32, 6)
            yts = self.stager("ny", [128, 512], BF16, 2)
            yTs = self.stager("nyT", [128, 4, 128], BF16, 2)
            psA = [kb.ps("npa", [128, 512]) for _ in range(2)]
            psB = [kb.ps("npb", [128, 512]) for _ in range(2)]
            psT = [kb.ps("npt", [128, 1024], BF16) for _ in range(2)]
            psO = [kb.ps("npo", [128, 512]) for _ in range(2)]
            kb_ps_y = psT[0]
            cur_case = None
            hc = 0
            tiles = [("c", 0), ("c", 1)] + [("x", qt) for qt in range(N // 128)]
            for kind, qt in tiles:
                t0 = qt * 128 if kind == "c" else CTX + qt * 128
                qT = qts()
                kb.dma(kb.sp, qT[:, :, :], S["NAQ"].t.rearrange("(c p) n -> p c n", p=128)[:, :, t0:t0 + 128], r=[S["NAQ"]], w=[qT], sb=qT)
                if kind == "x":
                    kt0, case = plan[qt]
                    k0 = CTX + kt0 * 128
                    kT, vT = kts(), vts()
                    kb.dma(kb.sp, kT[:, :, :], S["NAK"].t.rearrange("(c p) n -> p c n", p=128)[:, :, k0:k0 + nloc], r=[S["NAK"]], w=[kT], sb=kT)
                    kb.dma(kb.sp, vT[:, :, :], S["NAV"].t[k0:k0 + nloc, :].rearrange("(i p) m -> p i m", p=128), r=[S["NAV"]], w=[vT], sb=vT)
                    if case != cur_case:
                        kb.dma(kb.sp, am[:, :, :], I["na_am"].t[l, case, :, :, :].rearrange("h q k -> q h k"), w=[am], sb=am)
                        cur_case = case
                    nk = nloc + CTX
                else:
                    nk = CTX
                yt = yts()
                nkt_all = nk // 128
                for h0 in range(0, 8, 2):
                    ctxs = []
                    for h in (h0, h0 + 1):
                        c_ = dict(h=h, hp=h // 2, pb=(h % 2) * 64, pa=psA[h % 2], pbk=psB[h % 2], pt=psT[h % 2], po=psO[h % 2],
                                  ssb=sbs(), pbf=pbs(), ptt=ptb(), stt=sts())
                        c_["lq"] = qT[c_["pb"]:c_["pb"] + 64, c_["hp"], :]
                        ctxs.append(c_)
                    hc += 2
                    for c_ in ctxs:
                        h, hp, pb, pa, pbk, ssb, lq = c_["h"], c_["hp"], c_["pb"], c_["pa"], c_["pbk"], c_["ssb"], c_["lq"]
                        if kind == "x":
                            n1 = min(512, nloc)
                            mm(kb, pa[:, 0:n1], lq, kT[pb:pb + 64, hp, 0:n1], True, True, r=[qT, kT], w=[pa])
                            if nloc > 512:
                                mm(kb, pbk[:, 0:nloc - 512], lq, kT[pb:pb + 64, hp, 512:nloc], True, True, r=[qT, kT], w=[pbk])
                            mm(kb, pbk[:, 128:128 + CTX], lq, kctx[pb:pb + 64, hp, :], True, True, r=[qT, kctx], w=[pbk])
                        else:
                            mm(kb, pa[:, 0:CTX], lq, kctx[pb:pb + 64, hp, :], True, True, r=[qT, kctx], w=[pa])
                    for c_ in ctxs:
                        h, pa, pbk, ssb = c_["h"], c_["pa"], c_["pbk"], c_["ssb"]
                        if kind == "x":
                            n1 = min(512, nloc)
                            kb.op(kb.dve, lambda: nc.vector.tensor_tensor(out=ssb[:, 0:n1], in0=pa[:, 0:n1], in1=am[:, h, 0:n1], op=ALU.add),
                                  r=[pa, am], w=[ssb])
                            if nloc > 512:
                                kb.op(kb.dve, lambda: nc.vector.tensor_tensor(out=ssb[:, 512:nloc], in0=pbk[:, 0:nloc - 512], in1=am[:, h, 512:nloc],
                                                                             op=ALU.add), r=[pbk, am], w=[ssb])
                            kb.op(kb.act, lambda: nc.scalar.copy(out=ssb[:, nloc:nloc + CTX], in_=pbk[:, 128:128 + CTX]), r=[pbk], w=[ssb])
                        else:
                            kb.op(kb.act, lambda: nc.scalar.copy(out=ssb[:, 0:CTX], in_=pa[:, 0:CTX]), r=[pa], w=[ssb])
                    for c_ in ctxs:
                        ssb, stt = c_["ssb"], c_["stt"]
                        kb.op(kb.dve, lambda: nc.vector.reduce_max(out=stt[:, 0:1], in_=ssb[:, 0:nk], axis=AX.X), r=[ssb], w=[stt])
                        kb.op(kb.dve, lambda: nc.vector.tensor_scalar(out=stt[:, 1:2], in0=stt[:, 0:1], scalar1=-1.0, scalar2=None, op0=ALU.mult),
                              r=[stt], w=[stt])
                    for c_ in ctxs:
                        ssb, stt, pbf = c_["ssb"], c_["stt"], c_["pbf"]
                        kb.op(kb.act, lambda: nc.scalar.activation(out=pbf[:, 0:nk], in_=ssb[:, 0:nk], func=AF.Exp, bias=stt[:, 1:2],
                                                                   accum_out=stt[:, 2:3]), r=[ssb, stt], w=[pbf, stt])
                        kb.op(kb.dve, lambda: nc.vector.reciprocal(out=stt[:, 3:4], in_=stt[:, 2:3]), r=[stt], w=[stt])
                    for c_ in ctxs:
                        pbf, pt = c_["pbf"], c_["pt"]
                        for kt in range(nkt_all):
                            kb.op(kb.pe, lambda kt=kt: nc.tensor.transpose(out=pt[:, kt * 128:(kt + 1) * 128], in_=pbf[:, kt * 128:(kt + 1) * 128],
                                                                           identity=self.identb[:, :]),
                                  r=[pbf, self.identb], w=[pt], sig=(kt == nkt_all - 1))
                    for ci_, c_ in enumerate(ctxs):
                        pt, ptt = c_["pt"], c_["ptt"]
                        if ci_ == 0:
                            kb.op(kb.act, lambda: nc.scalar.copy(out=ptt[:, 0:nkt_all, :], in_=pt[:, 0:nk].rearrange("p (a b) -> p a b", b=128)),
                                  r=[pt], w=[ptt])
                        else:
                            kb.op(kb.dve, lambda: nc.vector.tensor_copy(out=ptt[:, 0:nkt_all, :], in_=pt[:, 0:nk].rearrange("p (a b) -> p a b", b=128)),
                                  r=[pt], w=[ptt])
                    for c_ in ctxs:
                        h, ptt, po, stt = c_["h"], c_["ptt"], c_["po"], c_["stt"]
                        for kt in range(nkt_all):
                            if kind == "x":
                                rv = vT[:, kt, h * 64:(h + 1) * 64] if kt < nkt else vctx[:, kt - nkt, h * 64:(h + 1) * 64]
                                rb = vT if kt < nkt else vctx
                            else:
                                rv, rb = vctx[:, kt, h * 64:(h + 1) * 64], vctx
                            mm(kb, po[:, 0:64], ptt[:, kt, :], rv, kt == 0, kt == nkt_all - 1, r=[ptt, rb], w=[po])
                    for c_ in ctxs:
                        h, po, stt = c_["h"], c_["po"], c_["stt"]
                        kb.op(kb.dve, lambda: nc.vector.tensor_scalar(out=yt[:, h * 64:(h + 1) * 64], in0=po[:, 0:64], scalar1=stt[:, 3:4],
                                                                     scalar2=None, op0=ALU.mult), r=[po, stt], w=[yt])
                pt = kb_ps_y
                yT = yTs()
                for c in range(4):
                    kb.op(kb.pe, lambda c=c: nc.tensor.transpose(out=pt[:, c * 128:(c + 1) * 128], in_=yt[:, c * 128:(c + 1) * 128],
                                                                 identity=self.identb[:, :]), r=[yt, self.identb], w=[pt], sig=(c == 3))
                kb.op(kb.act, lambda: nc.scalar.copy(out=yT[:, :, :], in_=pt[:, 0:512].rearrange("p (a b) -> p a b", b=128)), r=[pt], w=[yT])
                kb.dma(kb.sp, S["YNT"].t.rearrange("(c p) n -> p c n", p=128)[:, :, t0:t0 + 128], yT[:, :, :], r=[yT], w=[S["YNT"]], sb=yT)
        kb.stk = old

    def merge(self, l, S):
        kb, nc, I = self.kb, self.nc, self.inp
        NT = self.NT
        old = kb.stk
        with ExitStack() as st:
            kb.stk = st
            gts = [kb.sb("mgt", [128, 24, 512], BF16) for _ in range(2)]
            t1s = self.stager("mt1", [128, 512], F32, 2)
            t2s = self.stager("mt2", [128, 512], F32, 2)
            t3s = self.stager("mt3", [128, 512], F32, 2)
            ys = self.stager("myo", [128, 512], BF16, 3)
            cur = {"g": None, "i": 0}

            def epi(kb, pl, g, j, t0, tn):
                if j == 0:
                    gt = gts[cur["i"] % 2]
                    cur["i"] += 1
                    kb.dma(kb.sp, gt[:, :, 0:tn], S["GT"].t.rearrange("(c p) n -> p c n", p=128)[:, :, t0:t0 + tn],
                           r=[S["GT"]], w=[gt], sb=gt)
                    cur["g"] = gt
                gt = cur["g"]
                t1, t2, t3, yo = t1s(), t2s(), t3s(), ys()
                kb.op(kb.dve, lambda: nc.vector.tensor_tensor(out=t1[:, 0:tn], in0=pl[0][:, 0:tn], in1=gt[:, j, 0:tn], op=ALU.mult),
                      r=[pl[0], gt], w=[t1])
                kb.op(kb.dve, lambda: nc.vector.tensor_tensor(out=t2[:, 0:tn], in0=pl[1][:, 0:tn], in1=gt[:, 8 + j, 0:tn], op=ALU.mult),
                      r=[pl[1], gt], w=[t2])
                kb.op(kb.dve, lambda: nc.vector.tensor_tensor(out=t3[:, 0:tn], in0=pl[2][:, 0:tn], in1=gt[:, 16 + j, 0:tn], op=ALU.mult),
                      r=[pl[2], gt], w=[t3])
                kb.op(kb.dve, lambda: nc.vector.tensor_tensor(out=t1[:, 0:tn], in0=t1[:, 0:tn], in1=t2[:, 0:tn], op=ALU.add),
                      r=[t1, t2], w=[t1])
                kb.op(kb.dve, lambda: nc.vector.tensor_tensor(out=yo[:, 0:tn], in0=t1[:, 0:tn], in1=t3[:, 0:tn], op=ALU.add),
                      r=[t1, t3], w=[yo])
                kb.dma(kb.sp, S["YT"].t[j * 128:(j + 1) * 128, t0:t0 + tn], yo[:, 0:tn], r=[yo], w=[S["YT"]], sb=yo)

            groups = [dict(streams=[(0, I["w_branch_m"].t[l, :, :]), (1, I["w_branch_na"].t[l, :, :]),
                                    (2, I["w_branch_s5"].t[l, :, :])], nblk=8)]
            gemm_fm(kb, NT, [(S["YMT"], 4), (S["YNT"], 4), (S["YST"], 4)], groups, epi, wbufs=1, tag="mg",
                    t_lo=(CTX if l == self.depth - 1 else 0))
        kb.stk = old

    def make_residual(self, S, mods, gmk_x, prenorm=None, router=None, src_acc=None):
        kb, nc = self.kb, self.nc
        xts = self.stager("rxt", [128, 1024], F32, 3)
        tms = self.stager("rtm", [128, 1024], F32, 2)
        sq = kb.sb("rsq", [128, 1024], BF16)
        sts = self.stager("rst", [128, 8], F32, 3)
        gm = mods["gm"]
        state = {"i": 0, "hb": None, "first_t0": None}
        if prenorm is not None:
            pts = [kb.ps("rptr", [128, 512]) for _ in range(2)]
            hbl = [kb.sb("rhbl", [128, 8, 512], BF16) for _ in range(2)]
            xns = self.stager("rxn", [128, 1024], F32, 2)
            st2 = self.stager("rst2", [128, 4], F32, 3)

        def flush():
            if prenorm is not None and state["hb"] is not None:
                hb, t0f, n = state["hb"], state["first_t0"], state["n"]
                kb.dma(kb.sp, S["HXT"].t.rearrange("(kc p) n -> p kc n", p=128)[:, :, t0f:t0f + n], hb[:, :, 0:n],
                       r=[hb], w=[S["HXT"]], sb=hb)
                state["hb"] = None

        def epi(kb, pl, t0, src=None):
            seg = 1 if t0 < CTX else 0
            k = gmk_x + seg
            xt, tm, stt = xts(), tms(), sts()
            kb.dma(kb.sp, xt[:, :], S["XU"].t[t0:t0 + 128, :], r=[S["XU"]], w=[xt], sb=xt)
            srcs = [(pl[0][:, :], [pl[0]]), (pl[1][:, :], [pl[1]])] if src is None else src
            for h in range(2):
                ap, bufs = srcs[h]
                kb.op(kb.act, lambda ap=ap, h=h: nc.scalar.activation(out=sq[:, h * 512:(h + 1) * 512], in_=ap, func=AF.Square,
                                                                      accum_out=stt[:, 4 + h:5 + h]), r=bufs, w=[sq, stt])
            kb.op(kb.dve, lambda: nc.vector.tensor_tensor(out=stt[:, 0:1], in0=stt[:, 4:5], in1=stt[:, 5:6], op=ALU.add), r=[stt], w=[stt])
            kb.op(kb.dve, lambda: nc.vector.tensor_scalar(out=stt[:, 2:3], in0=stt[:, 0:1], scalar1=1.0 / D, scalar2=EPS,
                                                         op0=ALU.mult, op1=ALU.add), r=[stt], w=[stt])
            kb.op(kb.act, lambda: nc.scalar.activation(out=stt[:, 3:4], in_=stt[:, 2:3], func=AF.Sqrt), r=[stt], w=[stt])
            kb.op(kb.dve, lambda: nc.vector.reciprocal(out=stt[:, 1:2], in_=stt[:, 3:4]), r=[stt], w=[stt])
            for h in range(2):
                ap, bufs = srcs[h]
                kb.op(kb.dve, lambda ap=ap, h=h: nc.vector.scalar_tensor_tensor(
                    out=tm[:, h * 512:(h + 1) * 512], in0=ap, scalar=stt[:, 1:2], in1=gm[:, k, h * 512:(h + 1) * 512],
                    op0=ALU.mult, op1=ALU.mult), r=bufs + [stt, gm], w=[tm])
            kb.op(kb.dve, lambda: nc.vector.tensor_tensor(out=xt[:, :], in0=xt[:, :], in1=tm[:, :], op=ALU.add), r=[xt, tm], w=[xt])
            kb.dma(kb.sp, S["XU"].t[t0:t0 + 128, :], xt[:, :], r=[xt], w=[S["XU"]], sb=xt)
            if prenorm is not None:
                s2 = st2()
                xn = xns()
                self.rstd(xt[:, :], [xt], s2, sq)
                i = state["i"]
                if state["hb"] is None:
                    state["hb"] = hbl[(i // 4) % 2]
                    state["first_t0"] = t0
                    state["n"] = 0
                hb = state["hb"]
                kb.op(kb.dve, lambda: nc.vector.tensor_scalar(out=xn[:, :], in0=xt[:, :], scalar1=s2[:, 1:2], scalar2=None,
                                                             op0=ALU.mult), r=[xt, s2], w=[xn])
                hf = router["hf"]() if (router is not None and seg == 0) else None
                self.norm_T(xn, [xn], None, mods["ab"], 2, seg, pts, 0, hb, state["n"], hf=hf)
                import os as _os
                if hf is not None and _os.environ.get("ROUTER_MODE") != "hf":
                    router["fn"](hf, t0)
                state["n"] += 128
                state["i"] += 1
                if state["n"] == 512:
                    flush()
        return epi, flush

    def make_router(self, l, S):
        kb, nc, I = self.kb, self.nc, self.inp
        j = l // 2
        R = kb.sb("rtw", [128, 8, 8], F32)
        with nc.allow_non_contiguous_dma(reason="tiny router weight"):
            kb.dma(kb.sp, R[:, :, :], I["moe_router"].t[j, :, :].rearrange("(kc p) e -> p kc e", p=128), w=[R], sb=R)
        hfs = self.stager("rhf", [128, 8, 128], F32, 2)
        pr = kb.ps("rtp", [128, 512])
        lgs = self.stager("rlg", [128, 32], F32, 3)

        def fn(hf, t0):
            lg = lgs()
            import os as _os
            if _os.environ.get("ROUTER_OFF"):
                kb.op(kb.dve, lambda: nc.vector.tensor_copy(out=lg[:, 0:8], in_=hf[:, 0, 0:8]), r=[hf], w=[lg])
            else:
                for kc in range(8):
                    mm(kb, pr[:, 0:8], hf[:, kc, :], R[:, kc, :], kc == 0, kc == 7, r=[hf, R], w=[pr])
                kb.op(kb.dve, lambda: nc.vector.tensor_copy(out=lg[:, 0:8], in_=pr[:, 0:8]), r=[pr], w=[lg])
            if _os.environ.get("ROUTER_MODE") == "nomax":
                kb.op(kb.dve, lambda: nc.vector.tensor_copy(out=lg[:, 8:16], in_=lg[:, 0:8]), r=[lg], w=[lg])
            else:
                kb.op(kb.dve, lambda: nc.vector.max(out=lg[:, 8:16], in_=lg[:, 0:8]), r=[lg], w=[lg])
            kb.op(kb.dve, lambda: nc.vector.tensor_tensor(out=lg[:, 16:17], in0=lg[:, 8:9], in1=lg[:, 9:10], op=ALU.subtract), r=[lg], w=[lg])
            kb.op(kb.act, lambda: nc.scalar.activation(out=lg[:, 17:18], in_=lg[:, 16:17], func=AF.Sigmoid), r=[lg], w=[lg])
            kb.op(kb.act, lambda: nc.scalar.activation(out=lg[:, 18:19], in_=lg[:, 16:17], func=AF.Sigmoid, scale=-1.0), r=[lg], w=[lg])
            kb.op(kb.dve, lambda: nc.vector.tensor_scalar(out=lg[:, 24:32], in0=lg[:, 0:8], scalar1=lg[:, 8:9], scalar2=lg[:, 17:18],
                                                         op0=ALU.is_equal, op1=ALU.mult), r=[lg], w=[lg])
            kb.op(kb.dve, lambda: nc.vector.tensor_scalar(out=lg[:, 0:8], in0=lg[:, 0:8], scalar1=lg[:, 9:10], scalar2=lg[:, 18:19],
                                                         op0=ALU.is_equal, op1=ALU.mult), r=[lg], w=[lg])
            kb.op(kb.dve, lambda: nc.vector.tensor_tensor(out=self.cmbt[:, t0 // 128, :], in0=lg[:, 24:32], in1=lg[:, 0:8], op=ALU.add),
                  r=[lg], w=[self.cmbt])
        return {"hf": hfs, "fn": fn}

    def router_phase(self, l, S, mods):
        kb, nc, I = self.kb, self.nc, self.inp
        j = l // 2
        ab = mods["ab"]
        old = kb.stk
        with ExitStack() as st:
            kb.stk = st
            R = kb.sb("rtw", [128, 8, 8], F32)
            with nc.allow_non_contiguous_dma(reason="tiny router weight"):
                kb.dma(kb.sp, R[:, :, :], I["moe_router"].t[j, :, :].rearrange("(kc p) e -> p kc e", p=128), w=[R], sb=R)
            xts = self.stager("qxt", [128, 1024], F32, 3)
            sq = kb.sb("qsq", [128, 1024], BF16)
            sts = self.stager("qst", [128, 4], F32, 3)
            hfs = self.stager("qhf", [128, 8, 128], F32, 2)
            lgs = self.stager("qlg", [128, 32], F32, 3)
            pts = [kb.ps("qpt", [128, 512]) for _ in range(4)]
            prs = [kb.ps("qpr", [128, 512]) for _ in range(2)]
            for ti, t0 in enumerate(range(CTX, self.NT, 128)):
                xt, stt, hf, lg = xts(), sts(), hfs(), lgs()
                kb.dma(kb.sp, xt[:, :], S["XU"].t[t0:t0 + 128, :], r=[S["XU"]], w=[xt], sb=xt)
                self.rstd(xt[:, :], [xt], stt, sq)
                kb.op(kb.dve, lambda: nc.vector.tensor_scalar(out=xt[:, :], in0=xt[:, :], scalar1=stt[:, 1:2], scalar2=None, op0=ALU.mult),
                      r=[xt, stt], w=[xt])
                for half in range(2):
                    p = pts[(ti % 2) * 2 + half]
                    for q in range(4):
                        kc = half * 4 + q
                        kb.op(kb.pe, lambda p=p, q=q, kc=kc: nc.tensor.transpose(out=p[:, q * 128:(q + 1) * 128], in_=xt[:, kc * 128:(kc + 1) * 128],
                                                                                 identity=self.ident[:, :]), r=[xt, self.ident], w=[p], sig=(q == 3))
                    for q in range(4):
                        kc = half * 4 + q
                        if q % 2 == 0:
                            kb.op(kb.dve, lambda p=p, q=q, kc=kc: nc.vector.tensor_scalar(
                                out=hf[:, kc, :], in0=p[:, q * 128:(q + 1) * 128], scalar1=ab[:, 2, kc, 0:1], scalar2=ab[:, 3, kc, 0:1],
                                op0=ALU.mult, op1=ALU.add), r=[p, ab], w=[hf])
                        else:
                            kb.op(kb.act, lambda p=p, q=q, kc=kc: nc.scalar.activation(
                                out=hf[:, kc, :], in_=p[:, q * 128:(q + 1) * 128], func=AF.Identity,
                                scale=ab[:, 2, kc, 0:1], bias=ab[:, 3, kc, 0:1]), r=[p, ab], w=[hf])
                pr = prs[ti % 2]
                for kc in range(8):
                    mm(kb, pr[:, 0:8], hf[:, kc, :], R[:, kc, :], kc == 0, kc == 7, r=[hf, R], w=[pr])
                kb.op(kb.dve, lambda: nc.vector.tensor_copy(out=lg[:, 0:8], in_=pr[:, 0:8]), r=[pr], w=[lg])
                kb.op(kb.dve, lambda: nc.vector.max(out=lg[:, 8:16], in_=lg[:, 0:8]), r=[lg], w=[lg])
                kb.op(kb.dve, lambda: nc.vector.tensor_tensor(out=lg[:, 16:17], in0=lg[:, 8:9], in1=lg[:, 9:10], op=ALU.subtract), r=[lg], w=[lg])
                kb.op(kb.act, lambda: nc.scalar.activation(out=lg[:, 17:18], in_=lg[:, 16:17], func=AF.Sigmoid), r=[lg], w=[lg])
                kb.op(kb.act, lambda: nc.scalar.activation(out=lg[:, 18:19], in_=lg[:, 16:17], func=AF.Sigmoid, scale=-1.0), r=[lg], w=[lg])
                kb.op(kb.dve, lambda: nc.vector.tensor_scalar(out=lg[:, 24:32], in0=lg[:, 0:8], scalar1=lg[:, 8:9], scalar2=lg[:, 17:18],
                                                             op0=ALU.is_equal, op1=ALU.mult), r=[lg], w=[lg])
                kb.op(kb.dve, lambda: nc.vector.tensor_scalar(out=lg[:, 0:8], in0=lg[:, 0:8], scalar1=lg[:, 9:10], scalar2=lg[:, 18:19],
                                                             op0=ALU.is_equal, op1=ALU.mult), r=[lg], w=[lg])
                kb.op(kb.dve, lambda: nc.vector.tensor_tensor(out=self.cmbt[:, t0 // 128, :], in0=lg[:, 24:32], in1=lg[:, 0:8], op=ALU.add),
                      r=[lg], w=[self.cmbt])
        kb.stk = old

    def moe(self, l, S, mods):
        kb, nc, I = self.kb, self.nc, self.inp
        j = l // 2
        NT = self.NT
        xt_tiles = list(range(CTX, NT, 128))
        for e in range(NEXP):
            AT = S["AT2E"][e % 2]
            self.swiglu_fm(S["HXT"], I["moe_w_gate"].t[j, e, :, :], I["moe_w_up"].t[j, e, :, :], EFF, AT, f"me{e}",
                           t_lo=(CTX if l == self.depth - 1 else 0))
            old = kb.stk
            with ExitStack() as st:
                kb.stk = st
                cmb = self.cmbt
                accs = self.stager("acc", [128, 1024], F32, 3)
                if e == NEXP - 1:
                    res_epi, _ = self.make_residual(S, mods, 2)

                def epi(kb, pl, t0, e=e):
                    acc = accs()
                    ti = t0 // 128
                    if e == 0:
                        for h in range(2):
                            kb.op(kb.dve if h == 0 else kb.act,
                                  (lambda h=h: nc.vector.tensor_scalar(out=acc[:, h * 512:(h + 1) * 512], in0=pl[h][:, :],
                                                                       scalar1=cmb[:, ti, e:e + 1], scalar2=None, op0=ALU.mult)) if h == 0 else
                                  (lambda h=h: nc.scalar.activation(out=acc[:, h * 512:(h + 1) * 512], in_=pl[h][:, :], func=AF.Copy,
                                                                    scale=cmb[:, ti, e:e + 1])),
                                  r=[pl[h], cmb], w=[acc])
                    else:
                        kb.dma(kb.sp, acc[:, :], S["ACC"].t[t0:t0 + 128, :], r=[S["ACC"]], w=[acc], sb=acc)
                        for h in range(2):
                            kb.op(kb.dve, lambda h=h: nc.vector.scalar_tensor_tensor(
                                out=acc[:, h * 512:(h + 1) * 512], in0=pl[h][:, :], scalar=cmb[:, ti, e:e + 1],
                                in1=acc[:, h * 512:(h + 1) * 512], op0=ALU.mult, op1=ALU.add), r=[pl[h], cmb, acc], w=[acc])
                    if e < NEXP - 1:
                        kb.dma(kb.sp, S["ACC"].t[t0:t0 + 128, :], acc[:, :], r=[acc], w=[S["ACC"]], sb=acc)
                    else:
                        res_epi(kb, None, t0, src=[(acc[:, 0:512], [acc]), (acc[:, 512:1024], [acc])])

                gemm_tm(kb, xt_tiles, AT, EFF // 128, I["moe_w_down"].t[j, e, :, :], 1024, epi, tag=f"md{e}", npairs=3)
            kb.stk = old

    def outproj(self, l, S, mods, router=None):
        kb, nc, I = self.kb, self.nc, self.inp
        old = kb.stk
        with ExitStack() as st:
            kb.stk = st
            router = self.make_router(l, S) if router else None
            epi, flush = self.make_residual(S, mods, 0, prenorm=True, router=router)
            tiles = list(range(CTX if l == self.depth - 1 else 0, self.NT, 128))
            gemm_tm(kb, tiles, S["YT"], 8, I["w_out"].t[l, :, :], 1024, epi, tag="op", npairs=2)
            flush()
        kb.stk = old

    def swiglu_fm(self, A, Wg, Wu, ff, AT2, tag, t_lo=0):
        kb, nc = self.kb, self.nc
        old = kb.stk
        with ExitStack() as st:
            kb.stk = st
            sg = self.stager("sws", [128, 512], F32, 3)
            so = self.stager("swo", [128, 512], BF16, 3)

            def epi(kb, pl, g, j, t0, tn):
                s1, o1 = sg(), so()
                kb.op(kb.act, lambda: nc.scalar.activation(out=s1[:, 0:tn], in_=pl[0][:, 0:tn], func=AF.Silu), r=[pl[0]], w=[s1])
                kb.op(kb.dve, lambda: nc.vector.tensor_tensor(out=o1[:, 0:tn], in0=pl[1][:, 0:tn], in1=s1[:, 0:tn], op=ALU.mult),
                      r=[pl[1], s1], w=[o1])
                r0 = (g["b0"] + j) * 128
                kb.dma(kb.sp, AT2.t[r0:r0 + 128, t0:t0 + tn], o1[:, 0:tn], r=[o1], w=[AT2], sb=o1)

            nb = ff // 128
            groups = []
            b0 = 0
            while b0 < nb:
                n = min(4, nb - b0)
                groups.append(dict(streams=[(0, Wg[:, b0 * 128:(b0 + n) * 128]), (0, Wu[:, b0 * 128:(b0 + n) * 128])], nblk=n, b0=b0))
                b0 += n
            gemm_fm(kb, self.NT, [(A, 8)], groups, epi, tag=tag, t_lo=t_lo)
        kb.stk = old

    def ffn_dense(self, l, S, mods):
        kb, nc, I = self.kb, self.nc, self.inp
        j = l // 2
        self.swiglu_fm(S["HXT"], I["ffn_w_gate"].t[j, :, :], I["ffn_w_up"].t[j, :, :], FF, S["AT2"], "ff")
        old = kb.stk
        with ExitStack() as st:
            kb.stk = st
            epi, flush = self.make_residual(S, mods, 2)
            tiles = list(range(0, self.NT, 128))
            gemm_tm(kb, tiles, S["AT2"], FF // 128, I["ffn_w_down"].t[j, :, :], 1024, epi, tag="fd", npairs=3)
        kb.stk = old

    def declare_inputs(self):
        NT = self.NT
        L = self.depth
        self.din("xu", [NT, D])
        self.din("cc", [2, D])
        self.din("ada_w", [L, D, 6 * D])
        self.din("ada_b", [L, 6 * D])
        for n in ("norm_mix_pre", "norm_mix_post", "norm_ffn_pre", "norm_ffn_post"):
            self.din(n, [L, D])
        self.din("w_fm", [L, D, 6144])
        self.din("w_tm", [L, D, 1536])
        self.din("gate_b_sp", [L, 4, 128])
        for n in ("w_branch_m", "w_branch_na", "w_branch_s5"):
            self.din(n, [L, 512, D])
        self.din("w_out", [L, D, D])
        self.din("ffn_w_gate", [1, D, FF])
        self.din("ffn_w_up", [1, D, FF])
        self.din("ffn_w_down", [1, FF, D])
        self.din("moe_router", [1, D, NEXP])
        self.din("moe_w_gate", [1, NEXP, D, EFF])
        self.din("moe_w_up", [1, NEXP, D, EFF])
        self.din("moe_w_down", [1, NEXP, EFF, D])
        self.din("rope_cos", [128, self.N])
        self.din("rope_sin", [128, self.N])
        self.din("rope_rT", [128, 128])
        self.din("m_conv_w", [L, 3, 1024])
        self.din("m_conv_b", [L, 1024])
        self.din("m_norm", [L, 512])
        self.din("s5p", [L, 2, 128, 16, 4])
        self.din("s5B", [L, 2, 16, 128, 2, 16])
        self.din("s5CT", [L, 2, 16, 128, 2, 16])
        self.din("s5_d", [L, 512])
        self.din("s5_glu_w", [L, 512, 512])
        self.din("s5_glu_b", [L, 512])
        plan, cases, nkt, _ = self.na_plan()
        self.din("na_am", [L, len(cases), 8, 128, nkt * 128])
        for n in self.dbg_in:
            self.din("dbg_" + n, list(self.dbg_in[n][0]), self.dbg_in[n][1])

    def scratch(self):
        NT = self.NT
        S = {}
        S["XU"] = self.dscr("XU", [NT, D], F32)
        S["HXT"] = self.dscr("HXT", [D, NT], BF16)
        S["QP"] = self.dscr("QP", [1024, NT], BF16)
        S["NAQ"] = self.dscr("NAQ", [512, NT], BF16)
        S["NAK"] = self.dscr("NAK", [512, NT], BF16)
        S["S5U"] = self.dscr("S5U", [512, NT], BF16)
        S["GT"] = self.dscr("GT", [3072, NT], BF16)
        S["GR"] = self.dscr("GR", [512, NT], F32)
        S["V"] = self.dscr("V", [NT, 512], BF16)
        S["OS"] = self.dscr("OS", [NT, 512], BF16)
        S["NAV"] = self.dscr("NAV", [NT, 512], BF16)
        for n in ("YMT", "YNT", "YST"):
            S[n] = self.dscr(n, [512, NT], BF16)
        S["GS"] = self.dscr("GS", [512, NT], BF16)
        S["QK"] = self.dscr("QK", [1024, NT], BF16)
        S["HF"] = self.dscr("HF", [NT, 512], F32)
        S["YT"] = self.dscr("YT", [D, NT], BF16)
        S["AT2"] = self.dscr("AT2", [FF, NT], BF16)
        S["AT2E"] = [self.dscr(f"AT2E{i}", [EFF, NT], BF16) for i in range(2)]
        S["ACC"] = self.dscr("ACC", [NT, D], F32)
        S["CMB"] = self.dscr("CMB", [NT, NEXP], F32)
        return S

    def build(self, stop_after=None):
        kb, nc = self.kb, self.nc
        self.declare_inputs()
        S = self.scratch()
        self.S = S
        self.consts()
        NT = self.NT
        cp = kb.sb("cpsem", [1, 1], F32)
        for t0 in range(0, NT, 1024):
            n = min(1024, NT - t0)
            kb.dma(kb.sp, S["XU"].t[t0:t0 + n, :], self.inp["xu"].t[t0:t0 + n, :], w=[S["XU"]], sb=cp)
        for l in range(self.depth):
            old = kb.stk
            with ExitStack() as lst:
                kb.stk = lst
                mods = self.adaln(l)
                self.mods = mods
                self.prenorm_to_hxt(l, mods, S["XU"], S["HXT"], which=0)
                if "skip_mix" not in self.taps:
                    self.proj(l, S["HXT"], S)
                if stop_after == ("proj", l):
                    kb.stk = old
                    break
                if "YMT" not in self.dbg_in:
                    self.mlstm(l, S)
                if stop_after == ("mlstm", l):
                    kb.stk = old
                    break
                if "YST" not in self.dbg_in:
                    self.s5(l, S)
                if stop_after == ("s5", l):
                    kb.stk = old
                    break
                if "YNT" not in self.dbg_in:
                    self.na(l, S)
                if stop_after == ("na", l):
                    kb.stk = old
                    break
                for n in ("YMT", "YNT", "YST"):
                    if n in self.dbg_in:
                        dcp = kb.sb("dcp", [1, 1], F32)
                        kb.dma(kb.sp, S[n].t[:, :], self.inp["dbg_" + n].t[l, :, :], w=[S[n]], sb=dcp)
                self.merge(l, S)
                if stop_after == ("merge", l):
                    kb.stk = old
                    break
                is_moe = (l % 2 == 1)
                if is_moe:
                    self.cmbt = kb.sb("cmbt", [128, self.NT // 128, 8], F32)
                import os as _os
                self.outproj(l, S, mods, router=False)
                if is_moe:
                    self.router_phase(l, S, mods)
                if stop_after == ("outproj", l):
                    kb.stk = old
                    break
                if is_moe:
                    self.moe(l, S, mods)
                else:
                    self.ffn_dense(l, S, mods)
                if stop_after == ("ffn", l):
                    kb.stk = old
                    break
                if l == self.depth - 1:
                    yout = kb.dram("y", [self.N, D], F32, kind="ExternalOutput")
                    self.out["y"] = yout
                    dcp2 = kb.sb("dcp2", [1, 1], F32)
                    for t0 in range(0, self.N, 1024):
                        n = min(1024, self.N - t0)
                        kb.dma(kb.sp, yout.t[t0:t0 + n, :], S["XU"].t[CTX + t0:CTX + t0 + n, :], r=[S["XU"]], w=[yout], sb=dcp2)
            kb.stk = old
        self.finish()
        return nc

    def finish(self):
        kb = self.kb
        allb = list(self.out.values()) + [b for v in self.S.values() for b in (v if isinstance(v, list) else [v])]
        for b in allb:
            for s, v in list(b.w.items()):
                if kb.sp.known.get(s, 0) < v:
                    kb.sp.h.wait_ge(s.h, v)
                    kb.sp.known[s] = v
        for e in (kb.pe, kb.dve, kb.act, kb.pool):
            if e.sem.cnt > 0:
                kb.sp.h.wait_ge(e.sem.h, e.sem.cnt)
        for s in list(kb.dsems) + list(kb.retired):
            if s.cnt > 0:
                kb.sp.h.wait_ge(s.h, s.cnt)


def prep_shared(inp):
    L = inp["w_in"].shape[0]
    w_in = np.asarray(inp["w_in"], np.float32)
    w_fm = np.zeros((L, D, 6144), np.float32)
    w_fm[:, :, 0:1024] = w_in[:, :, 0:1024]
    w_fm[:, :, 1024:1536] = w_in[:, :, 2064:2576]
    w_fm[:, :, 1536:2048] = w_in[:, :, 2576:3088]
    w_fm[:, :, 2048:2560] = w_in[:, :, 3600:4112]
    w_fm[:, :, 2560:5632] = w_in[:, :, 4112:7184]
    gate_b_sp = np.zeros((L, 4, 128), np.float32)
    gate_b_sp[:, 1, :] = 30.0
    gate_b_sp[:, 3, :] = 30.0
    for k in range(4):
        for g in range(4):
            w_fm[:, :, 5632 + k * 128 + 32 * g] = w_in[:, :, 2048 + 4 * k + g]
            gate_b_sp[:, k, 32 * g] = np.asarray(inp["m_gate_b"], np.float32)[:, 4 * k + g]
    w_tm = np.concatenate([w_in[:, :, 1024:1536], w_in[:, :, 1536:2048], w_in[:, :, 3088:3600]], axis=2)
    sh = {"w_fm": w_fm, "w_tm": np.ascontiguousarray(w_tm), "gate_b_sp": gate_b_sp}
    sh["na_am"] = na_tables(inp["na_rpb"], inp["x"].shape[1])
    sh["rope_cos"], sh["rope_sin"], sh["rope_rT"] = rope_tables(inp["x"].shape[1])
    L = inp["w_in"].shape[0]
    f32 = lambda k: np.asarray(inp[k], np.float32)
    tostate = lambda a: a.reshape(L, 2, 16, 128).transpose(0, 1, 3, 2)
    logdt_rep = np.repeat(f32("s5_log_dt")[:, :, :, None], 64, axis=3)
    sh["s5p"] = np.ascontiguousarray(np.stack([tostate(f32("s5_lam_re")), tostate(f32("s5_lam_im")), tostate(logdt_rep),
                                               tostate(logdt_rep)], axis=-1))
    bcat = np.stack([f32("s5_b_re"), f32("s5_b_im")], axis=4)
    sh["s5B"] = np.ascontiguousarray(bcat.reshape(L, 2, 16, 128, 2, 16))
    ccat = np.stack([f32("s5_c_re"), f32("s5_c_im")], axis=3)
    ccat = np.stack([f32("s5_c_re").transpose(0, 1, 2, 4, 3), f32("s5_c_im").transpose(0, 1, 2, 4, 3)], axis=4)
    sh["s5CT"] = np.ascontiguousarray(ccat.reshape(L, 2, 16, 128, 2, 16))
    for n in ("s5_d", "s5_glu_w", "s5_glu_b"):
        sh[n] = np.ascontiguousarray(f32(n))
    for n in ("m_conv_w", "m_conv_b", "m_norm"):
        sh[n] = np.ascontiguousarray(np.asarray(inp[n], np.float32))
    for n in ("ada_w", "ada_b", "norm_mix_pre", "norm_mix_post", "norm_ffn_pre", "norm_ffn_post", "w_branch_m", "w_branch_na",
              "w_branch_s5", "w_out", "ffn_w_gate", "ffn_w_up", "ffn_w_down", "moe_router", "moe_w_gate", "moe_w_up", "moe_w_down"):
        sh[n] = np.ascontiguousarray(np.asarray(inp[n], np.float32))
    return sh


def na_tables(rpb, N):
    rpb = np.asarray(rpb, np.float32)
    L = rpb.shape[0]
    nq = N // 128
    rows = N // GRID_W
    win_r = min(8, rows)
    nkt = min(5, nq)
    cases = {}
    for qt in range(nq):
        kt0 = int(np.clip(qt - 2, 0, nq - nkt))
        r0s = tuple(int(np.clip(r - win_r // 2, 0, rows - win_r)) - 2 * kt0 for r in (2 * qt, 2 * qt + 1))
        key = (qt - kt0,) + r0s
        if key not in cases:
            cases[key] = len(cases)
    am = np.full((L, len(cases), 8, 128, nkt * 128), -30000.0, np.float32)
    q = np.arange(128)
    qr_par, qc = q // 64, q % 64
    k = np.arange(nkt * 128)
    krow_rel, kc = k // 64, k % 64
    col_start = np.clip(qc - 8, 0, 48)
    col_ok = (kc[None, :] >= col_start[:, None]) & (kc[None, :] < col_start[:, None] + 16)
    dc = np.clip(kc[None, :] - qc[:, None] + 15, 0, 30)
    for (dq, r0a, r0b), ci in cases.items():
        qrow_rel = 2 * dq + qr_par
        r0 = np.where(qr_par == 0, r0a, r0b)
        row_ok = (krow_rel[None, :] >= r0[:, None]) & (krow_rel[None, :] < r0[:, None] + win_r)
        dr = np.clip(krow_rel[None, :] - qrow_rel[:, None] + 7, 0, 14)
        ok = row_ok & col_ok
        g = rpb[:, :, dr, dc]
        am[:, ci] = np.where(ok[None, None], g, np.float32(-30000.0))
    return am


def rope_tables(N):
    pos = np.arange(N, dtype=np.int32)
    row = (pos // GRID_W).astype(np.float32)
    col = (pos % GRID_W).astype(np.float32)
    n_freq = 32
    inv = (np.float32(10000.0) ** (-np.arange(n_freq, dtype=np.float32) / np.float32(n_freq))).astype(np.float32)
    ar = (row[:, None] * inv).astype(np.float32)
    ac = (col[:, None] * inv).astype(np.float32)
    ang = np.concatenate([ar, ar, ac, ac], axis=1)
    cos = np.cos(ang).astype(np.float32).T
    sin = np.sin(ang).astype(np.float32).T
    R = np.zeros((128, 128), np.float32)
    for d in range(128):
        if d % 64 < 32:
            R[d, d + 32] = -1.0
        else:
            R[d, d - 32] = 1.0
    return np.ascontiguousarray(cos), np.ascontiguousarray(sin), np.ascontiguousarray(R.T)


def prep_core(inp, b):
    xu = np.concatenate([np.asarray(inp["ctx"][b], np.float32), np.asarray(inp["x"][b], np.float32)], axis=0)
    cc = np.stack([np.asarray(inp["c"][b], np.float32), np.asarray(inp["c_ctx"], np.float32)], axis=0)
    return {"xu": np.ascontiguousarray(xu), "cc": np.ascontiguousarray(cc)}


_CACHE = {}


def kernel(**inputs):
    x = np.asarray(inputs["x"])
    B, N, _ = x.shape
    depth = int(np.asarray(inputs["w_in"]).shape[0])
    key = (N, depth)
    if key not in _CACHE:
        P = Prog(N, depth=depth)
        P.build()
        _CACHE[key] = P
    P = _CACHE[key]
    sh = prep_shared(inputs)
    in_maps = []
    for b in range(B):
        core = prep_core(inputs, b)
        in_maps.append({k: (sh[k] if k in sh else core[k]) for k in P.inp})
    res = run_bass_kernel_spmd(P.nc, in_maps, core_ids=list(range(B)))
    out = np.stack([np.asarray(r["y"], np.float32) for r in res.results], axis=0)
    return out
```

```python
import numpy as np
from contextlib import ExitStack
import concourse.bass as bass
import concourse.mybir as mybir
from concourse.bass_utils import run_bass_kernel_spmd
from concourse.alu_op_type import AluOpType as ALU

F32 = mybir.dt.float32
BF16 = mybir.dt.bfloat16
I32 = mybir.dt.int32
AF = mybir.ActivationFunctionType
AX = mybir.AxisListType

D = 1024
KD = 8
CTX = 256
GRID_W = 64
EPS = 1e-6
FF = 2816
EFF = 3584
NEXP = 8


SEM_LIMIT = 30000


class Sem:
    def __init__(self, h):
        self.h = h
        self.cnt = 0


class Buf:
    def __init__(self, t, name):
        self.t = t
        self.name = name
        self.w = {}
        self.r = {}
        self.dsem = None
        self.multi = False

    def __getitem__(self, k):
        return self.t[k]


class Eng:
    def __init__(self, h, sem, name):
        self.h = h
        self.sem = sem
        self.name = name
        self.known = {}


class KB:
    def __init__(self, nc):
        self.nc = nc
        self.glob = ExitStack()
        self.stk = self.glob
        self.pe_sems = set()
        self.pe = Eng(nc.tensor, self._sem("pe"), "pe")
        self.dve = Eng(nc.vector, self._sem("dve"), "dve")
        self.act = Eng(nc.scalar, self._sem("act"), "act")
        self.pool = Eng(nc.gpsimd, self._sem("pool"), "pool")
        self.sp = Eng(nc.sync, self._sem("sp"), "sp")
        self.dsems = []
        self.retired = []
        self.ndma = 0
        self.uid = 0
        self.free_dsems = []
        self.freed = {}
        self.live = []

    def _sem(self, name):
        return Sem(self.glob.enter_context(self.nc.semaphore(name)))

    def _track(self, b):
        b.r = dict(self.freed)
        self.stk.callback(self._release, b)
        return b

    def _release(self, b):
        if b.dsem is not None:
            self.free_dsems.append(b.dsem)
            b.dsem = None
        for dd in (b.w, b.r):
            for s, v in dd.items():
                if self.freed.get(s, 0) < v:
                    self.freed[s] = v

    def sb(self, name, shape, dt):
        self.uid += 1
        t = self.stk.enter_context(self.nc.sbuf_tensor(f"{name}_{self.uid}", list(shape), dt))
        return self._track(Buf(t, name))

    def ps(self, name, shape, dt=F32):
        self.uid += 1
        t = self.stk.enter_context(self.nc.psum_tensor(f"{name}_{self.uid}", list(shape), dt))
        return self._track(Buf(t, name))

    def dram(self, name, shape, dt, kind="Internal"):
        t = self.nc.dram_tensor(name, list(shape), dt, kind=kind).ap()
        b = Buf(t, name)
        b.multi = True
        return b

    def _deps(self, e, r, w):
        deps = {}
        for b in r:
            for s, v in b.w.items():
                if deps.get(s, 0) < v:
                    deps[s] = v
        for b in w:
            for dd in ((b.r,) if b.multi else (b.w, b.r)):
                for s, v in dd.items():
                    if deps.get(s, 0) < v:
                        deps[s] = v
        for s, v in deps.items():
            if e is self.pe and (s is e.sem or s in self.pe_sems):
                continue
            if e.known.get(s, 0) < v:
                e.h.wait_ge(s.h, v)
                e.known[s] = v

    def _mark(self, s, v, r, w):
        for b in r:
            if b.r.get(s, 0) < v:
                b.r[s] = v
        for b in w:
            if b.multi:
                if b.w.get(s, 0) < v:
                    b.w[s] = v
            else:
                b.w = {s: v}
                b.r = {}

    def op(self, e, fn, r=(), w=(), sig=True):
        self._deps(e, r, w)
        ins = fn()
        sm = e.sem
        if sig:
            sm.cnt += 1
            ins.then_inc(sm.h, 1)
            v = sm.cnt
            if sm.cnt >= SEM_LIMIT:
                self.retired.append(sm)
                if e is self.pe:
                    self.pe_sems.add(sm)
                e.sem = self._sem(f"{e.name}{len(self.retired)}")
        else:
            v = sm.cnt + 1
        self._mark(sm, v, r, w)
        return ins

    def dma(self, q, out, in_, r=(), w=(), sb=None, **kw):
        self._deps(q, r, w)
        if sb.dsem is None:
            if self.free_dsems:
                sb.dsem = self.free_dsems.pop()
            else:
                sb.dsem = self._sem(f"d{len(self.dsems)}")
                self.dsems.append(sb.dsem)
        if sb.dsem.cnt + 16 > SEM_LIMIT:
            self.retired.append(sb.dsem)
            sb.dsem = self._sem(f"d{len(self.dsems)}")
            self.dsems.append(sb.dsem)
        s = sb.dsem
        ins = q.h.dma_start(out=out, in_=in_, **kw)
        s.cnt += 16
        ins.then_inc(s.h, 16)
        self._mark(s, s.cnt, r, w)
        self.ndma += 1
        return ins

    def wait_all(self, e, bufs):
        self._deps(e, bufs, ())


def mm(kb, out_ap, lhsT, rhs, start, stop, r, w, sig=None, **kw):
    if sig is None:
        sig = stop
    return kb.op(kb.pe, lambda: kb.nc.tensor.matmul(out_ap, lhsT=lhsT, rhs=rhs, start=start, stop=stop, **kw),
                 r=r, w=w, sig=sig)


def tok_blocks(NT, bs=512, t_lo=0):
    out = []
    t = t_lo
    while t < NT:
        n = min(bs, NT - t)
        out.append((t, n))
        t += n
    return out


def gemm_fm(kb, NT, srcs, groups, epilogue, wbufs=2, tag="g", t_lo=0):
    nc = kb.nc
    old = kb.stk
    with ExitStack() as st:
        kb.stk = st
        maxblk = max(g["nblk"] for g in groups)
        nstream = max(len(g["streams"]) for g in groups)
        wt = {}
        for si in range(nstream):
            kcs = max(srcs[g["streams"][si][0]][1] for g in groups if len(g["streams"]) > si)
            wt[si] = [kb.sb(f"{tag}w{si}", [128, kcs, maxblk * 128], BF16) for _ in range(wbufs)]
        abuf = {}
        for ai, (A, Kc) in enumerate(srcs):
            abuf[ai] = [kb.sb(f"{tag}a{ai}", [128, Kc, 512], BF16) for _ in range(2)]
        nps = 8 // nstream
        pss = [[kb.ps(f"{tag}p", [128, 512]) for _ in range(nstream)] for _ in range(min(nps, 4))]
        pi = 0
        tbs = tok_blocks(NT, t_lo=t_lo)
        seq = [(gi, ti) for gi in range(len(groups)) for ti in range(len(tbs))]
        wloaded = {}

        def load_w(gi):
            if gi in wloaded or gi >= len(groups):
                return
            g = groups[gi]
            ws = []
            for si, (src_idx, W_ap) in enumerate(g["streams"]):
                Kc = srcs[src_idx][1]
                wb = wt[si][gi % wbufs]
                kb.dma(kb.pool, wb[:, 0:Kc, 0:g["nblk"] * 128],
                       W_ap.rearrange("(kc p) m -> p kc m", p=128), w=[wb], sb=wb)
                ws.append(wb)
            wloaded[gi] = ws

        def load_a(idx):
            gi, ti = seq[idx]
            t0, tn = tbs[ti]
            ab = {}
            for ai in sorted(set(s_[0] for s_ in groups[gi]["streams"])):
                A, Kc = srcs[ai]
                b = abuf[ai][idx % 2]
                kb.dma(kb.sp, b[:, :, 0:tn], A.t.rearrange("(kc p) n -> p kc n", p=128)[:, :, t0:t0 + tn],
                       r=[A], w=[b], sb=b)
                ab[ai] = b
            return ab

        load_w(0)
        pending = load_a(0)
        for idx, (gi, ti) in enumerate(seq):
            g = groups[gi]
            t0, tn = tbs[ti]
            ws = wloaded[gi]
            if ti == 0 and wbufs > 1:
                load_w(gi + 1)
            ab = pending
            if idx + 1 < len(seq):
                if seq[idx + 1][0] != gi:
                    load_w(seq[idx + 1][0])
                pending = load_a(idx + 1)
            for j in range(g["nblk"]):
                pl = pss[pi % len(pss)]
                pi += 1
                for si, (src_idx, W_ap) in enumerate(g["streams"]):
                    Kc = srcs[src_idx][1]
                    for kc in range(Kc):
                        mm(kb, pl[si][:, 0:tn], ws[si][:, kc, j * 128:(j + 1) * 128], ab[src_idx][:, kc, 0:tn],
                           kc == 0, kc == Kc - 1, r=[ws[si], ab[src_idx]], w=[pl[si]])
                epilogue(kb, pl[:len(g["streams"])], g, j, t0, tn)
    kb.stk = old


def gemm_tm(kb, tiles, A, Kc, W_ap, M, epilogue, tag="t", wtile=None, npairs=None):
    old = kb.stk
    with ExitStack() as st:
        kb.stk = st
        nh = (M + 511) // 512
        if wtile is None:
            wb = kb.sb(f"{tag}w", [128, Kc, M], BF16)
            kb.dma(kb.pool, wb[:, :, :], W_ap.rearrange("(kc p) m -> p kc m", p=128), w=[wb], sb=wb)
        else:
            wb = wtile
        abufs = [kb.sb(f"{tag}a", [128, Kc, 512], BF16) for _ in range(2)]
        if npairs is None:
            npairs = 8 // nh if nh > 1 else 4
        pss = [[kb.ps(f"{tag}p", [128, 512]) for _ in range(nh)] for _ in range(npairs)]
        pi = 0
        runs = []
        for t0 in tiles:
            if runs and runs[-1][0] + 128 * len(runs[-1][1]) == t0 and len(runs[-1][1]) < 4:
                runs[-1][1].append(t0)
            else:
                runs.append((t0, [t0]))
        def load_run(ri):
            r0, ts = runs[ri]
            b = abufs[ri % 2]
            n = 128 * len(ts)
            kb.dma(kb.sp, b[:, :, 0:n], A.t.rearrange("(kc p) n -> p kc n", p=128)[:, :, r0:r0 + n],
                   r=[A], w=[b], sb=b)
            return b

        pending = load_run(0)
        for ri, (r0, ts) in enumerate(runs):
            b = pending
            if ri + 1 < len(runs):
                pending = load_run(ri + 1)
            for ti, t0 in enumerate(ts):
                pl = pss[pi % len(pss)]
                pi += 1
                for h in range(nh):
                    mw = min(512, M - h * 512)
                    for kc in range(Kc):
                        mm(kb, pl[h][:, 0:mw], b[:, kc, ti * 128:(ti + 1) * 128], wb[:, kc, h * 512:h * 512 + mw],
                           kc == 0, kc == Kc - 1, r=[wb, b], w=[pl[h]])
                epilogue(kb, pl, t0)
    kb.stk = old


class Prog:
    def __init__(self, N, depth=2, taps=()):
        self.N = N
        self.NT = N + CTX
        self.depth = depth
        self.taps = set(taps)
        self.dbg_in = {}
        self.nc = bass.Bass("TRN2", target_bir_lowering=False)
        self.kb = KB(self.nc)
        self.inp = {}
        self.out = {}

    def din(self, name, shape, dt=F32):
        b = self.kb.dram(name, shape, dt, kind="ExternalInput")
        self.inp[name] = b
        return b

    def dscr(self, name, shape, dt, tap=None):
        kind = "ExternalOutput" if name in self.taps else "Internal"
        b = self.kb.dram(name, shape, dt, kind=kind)
        if kind == "ExternalOutput":
            self.out[name] = b
        return b

    def consts(self):
        kb, nc = self.kb, self.nc
        self.ident = kb.sb("ident", [128, 128], F32)
        ones = kb.sb("ones", [128, 128], F32)
        kb.op(kb.pool, lambda: nc.gpsimd.memset(ones[:, :], 1.0), w=[ones])
        kb.op(kb.pool, lambda: nc.gpsimd.affine_select(out=self.ident[:, :], in_=ones[:, :], pattern=[[-1, 128]],
                                                      compare_op=ALU.is_equal, fill=0.0, base=0, channel_multiplier=1),
              r=[ones], w=[self.ident])
        self.ones = ones
        self.identb = kb.sb("identb", [128, 128], BF16)
        kb.op(kb.dve, lambda: nc.vector.tensor_copy(out=self.identb[:, :], in_=self.ident[:, :]),
              r=[self.ident], w=[self.identb])
        self.maskf = kb.sb("maskf", [128, 128], F32)
        self.maskb = kb.sb("maskb", [128, 128], F32)
        kb.op(kb.pool, lambda: nc.gpsimd.affine_select(out=self.maskf[:, :], in_=ones[:, :], pattern=[[1, 128]],
                                                      compare_op=ALU.is_ge, fill=0.0, base=0, channel_multiplier=-1),
              r=[ones], w=[self.maskf])
        kb.op(kb.pool, lambda: nc.gpsimd.affine_select(out=self.maskb[:, :], in_=ones[:, :], pattern=[[-1, 128]],
                                                      compare_op=ALU.is_ge, fill=0.0, base=0, channel_multiplier=1),
              r=[ones], w=[self.maskb])

    def adaln(self, l):
        kb, nc, I = self.kb, self.nc, self.inp
        res = {}
        modf = kb.sb("modf", [128, 48, 2], F32)
        ab = kb.sb("ab", [128, 4, 8, 2], F32)
        gm = kb.sb("gm", [128, 4, 1024], F32)
        gmrow = self.dscr(f"gmrow{l}", [4, 1024], F32)
        old = kb.stk
        with ExitStack() as st:
            kb.stk = st
            cc = kb.sb("cc", [128, 8, 2], F32)
            sc = kb.sb("sc", [128, 8, 2], F32)
            with nc.allow_non_contiguous_dma(reason="tiny conditioning vectors"):
                for s in range(2):
                    kb.dma(kb.sp, cc[:, :, s], I["cc"].t[s, :].rearrange("(kc p) -> p kc", p=128), w=[cc], sb=cc)
                bf = kb.sb("adab", [128, 48], F32)
                kb.dma(kb.sp, bf[:, :], I["ada_b"].t[l, :].rearrange("(j p) -> p j", p=128), w=[bf], sb=bf)
                gn = kb.sb("gn", [128, 2, 8], F32)
                kb.dma(kb.sp, gn[:, 0, :], I["norm_mix_pre"].t[l, :].rearrange("(j p) -> p j", p=128), w=[gn], sb=gn)
                kb.dma(kb.sp, gn[:, 1, :], I["norm_ffn_pre"].t[l, :].rearrange("(j p) -> p j", p=128), w=[gn], sb=gn)
            kb.op(kb.act, lambda: nc.scalar.activation(out=sc[:, :, :], in_=cc[:, :, :], func=AF.Silu), r=[cc], w=[sc])
            brow = kb.sb("brow", [1, 2, 1024], F32)
            grow = kb.sb("grow", [1, 2, 1024], F32)
            kb.dma(kb.sp, brow[0:1, 0, :], I["ada_b"].t[l:l + 1, 2 * 1024:3 * 1024], w=[brow], sb=brow)
            kb.dma(kb.sp, brow[0:1, 1, :], I["ada_b"].t[l:l + 1, 5 * 1024:6 * 1024], w=[brow], sb=brow)
            kb.dma(kb.sp, grow[0:1, 0, :], I["norm_mix_post"].t[l:l + 1, :], w=[grow], sb=grow)
            kb.dma(kb.sp, grow[0:1, 1, :], I["norm_ffn_post"].t[l:l + 1, :], w=[grow], sb=grow)
            rows = kb.sb("rows", [1, 4, 1024], F32)
            wts = [kb.sb("adaw", [128, 8, 1024], F32) for _ in range(2)]
            psf = [kb.ps("adap", [128, 512]) for _ in range(2)]
            psr = [kb.ps("adar", [128, 512]) for _ in range(2)]
            pi = 0
            for i in range(6):
                wb = wts[i % 2]
                kb.dma(kb.sp, wb[:, :, :],
                       I["ada_w"].t[l, :, i * 1024:(i + 1) * 1024].rearrange("(kc p) m -> p kc m", p=128), w=[wb], sb=wb)
                for jb in range(8):
                    p = psf[pi % 2]
                    pi += 1
                    for kc in range(8):
                        mm(kb, p[:, 0:2], wb[:, kc, jb * 128:(jb + 1) * 128], sc[:, kc, :], kc == 0, kc == 7,
                           r=[wb, sc], w=[p])
                    j = i * 8 + jb
                    kb.op(kb.dve, lambda p=p, j=j: nc.vector.tensor_scalar(out=modf[:, j, :], in0=p[:, 0:2], scalar1=bf[:, j:j + 1],
                                                                         scalar2=None, op0=ALU.add), r=[p, bf], w=[modf])
                if i in (2, 5):
                    gi = 0 if i == 2 else 1
                    for s in range(2):
                        for h in range(2):
                            p = psr[(s * 2 + h) % 2]
                            for kc in range(8):
                                mm(kb, p[0:1, 0:512], sc[:, kc, s:s + 1], wb[:, kc, h * 512:(h + 1) * 512], kc == 0, kc == 7,
                                   r=[wb, sc], w=[p])
                            ro = rows[0:1, gi * 2 + s, h * 512:(h + 1) * 512]
                            kb.op(kb.dve, lambda p=p, ro=ro, gi=gi, h=h: nc.vector.tensor_tensor(
                                out=ro, in0=p[0:1, 0:512], in1=brow[0:1, gi, h * 512:(h + 1) * 512], op=ALU.add),
                                r=[p, brow], w=[rows])
                            kb.op(kb.dve, lambda ro=ro, gi=gi, h=h: nc.vector.tensor_tensor(
                                out=ro, in0=ro, in1=grow[0:1, gi, h * 512:(h + 1) * 512], op=ALU.mult),
                                r=[rows, grow], w=[rows])
            kb.dma(kb.sp, gmrow.t[:, :], rows[0:1, :, :], r=[rows], w=[gmrow], sb=rows)
            for which, (sh_i, sc_i, gidx) in enumerate(((0, 1, 0), (3, 4, 1))):
                for s in range(2):
                    a_out = ab[:, 2 * which, :, s]
                    b_out = ab[:, 2 * which + 1, :, s]
                    kb.op(kb.dve, lambda a_out=a_out, sc_i=sc_i, s=s, gidx=gidx: nc.vector.scalar_tensor_tensor(
                        out=a_out, in0=modf[:, sc_i * 8:(sc_i + 1) * 8, s], scalar=1.0, in1=gn[:, gidx, :],
                        op0=ALU.add, op1=ALU.mult), r=[modf, gn], w=[ab])
                    kb.op(kb.dve, lambda b_out=b_out, sh_i=sh_i, s=s: nc.vector.tensor_copy(
                        out=b_out, in_=modf[:, sh_i * 8:(sh_i + 1) * 8, s]), r=[modf], w=[ab])
            kb.wait_all(kb.dve, [ab])
        kb.stk = old
        for k in range(4):
            kb.dma(kb.sp, gm[:, k, :], gmrow.t[k:k + 1, :].partition_broadcast(128), r=[gmrow], w=[gm], sb=gm)
        res["ab"] = ab
        res["gm"] = gm
        res["modf"] = modf
        return res

    def norm_stats(self, xt, rstd, sq):
        raise NotImplementedError

    def prenorm_to_hxt(self, l, mods, XU, HXT, which=0):
        kb, nc = self.kb, self.nc
        ab = mods["ab"]
        old = kb.stk
        with ExitStack() as st:
            kb.stk = st
            xts = [kb.sb("xt", [128, 1024], F32) for _ in range(3)]
            sq = kb.sb("sq", [128, 1024], BF16)
            stats = [kb.sb("st", [128, 4], F32) for _ in range(3)]
            pts = [kb.ps("ptr", [128, 512]) for _ in range(4)]
            hbl = [kb.sb("hbl", [128, 8, 512], BF16) for _ in range(2)]
            ntile = self.NT // 128
            for i in range(ntile):
                seg = 1 if i < 2 else 0
                xt = xts[i % 3]
                stt = stats[i % 3]
                kb.dma(kb.sp, xt[:, :], XU.t[i * 128:(i + 1) * 128, :], r=[XU], w=[xt], sb=xt)
                self.rstd(xt[:, :], [xt], stt, sq)
                hb = hbl[(i // 4) % 2]
                self.norm_T(xt, [xt], stt, ab, 2 * which, seg, pts, (i % 2) * 2, hb, (i % 4) * 128)
                if i % 4 == 3 or i == ntile - 1:
                    n = ((i % 4) + 1) * 128
                    t0 = (i // 4) * 512
                    kb.dma(kb.sp, HXT.t.rearrange("(kc p) n -> p kc n", p=128)[:, :, t0:t0 + n], hb[:, :, 0:n],
                           r=[hb], w=[HXT], sb=hb)
        kb.stk = old

    def rstd(self, x_ap, xbufs, stt, sq):
        kb, nc = self.kb, self.nc
        kb.op(kb.act, lambda: nc.scalar.activation(out=sq[:, :], in_=x_ap, func=AF.Square, accum_out=stt[:, 0:1]),
              r=xbufs, w=[sq, stt])
        kb.op(kb.dve, lambda: nc.vector.tensor_scalar(out=stt[:, 2:3], in0=stt[:, 0:1], scalar1=1.0 / D, scalar2=EPS,
                                                     op0=ALU.mult, op1=ALU.add), r=[stt], w=[stt])
        kb.op(kb.act, lambda: nc.scalar.activation(out=stt[:, 3:4], in_=stt[:, 2:3], func=AF.Sqrt), r=[stt], w=[stt])
        kb.op(kb.dve, lambda: nc.vector.reciprocal(out=stt[:, 1:2], in_=stt[:, 3:4]), r=[stt], w=[stt])

    def norm_T(self, xt, xbufs, stt, ab, abi, seg, pts, pbase, hb, hoff, hf=None):
        kb, nc = self.kb, self.nc
        if stt is not None:
            kb.op(kb.dve, lambda: nc.vector.tensor_scalar(out=xt[:, :], in0=xt[:, :], scalar1=stt[:, 1:2], scalar2=None,
                                                         op0=ALU.mult), r=xbufs + [stt], w=[xt])
        for half in range(2):
            p = pts[pbase + half]
            for q in range(4):
                kc = half * 4 + q
                kb.op(kb.pe, lambda p=p, q=q, kc=kc: nc.tensor.transpose(out=p[:, q * 128:(q + 1) * 128],
                                                                         in_=xt[:, kc * 128:(kc + 1) * 128],
                                                                         identity=self.ident[:, :]),
                      r=[xt, self.ident], w=[p], sig=(q == 3))
            for q in range(4):
                kc = half * 4 + q
                eng = kb.dve if q % 2 == 0 else kb.act
                if eng is kb.dve:
                    kb.op(kb.dve, lambda p=p, q=q, kc=kc: nc.vector.tensor_scalar(
                        out=hb[:, kc, hoff:hoff + 128], in0=p[:, q * 128:(q + 1) * 128],
                        scalar1=ab[:, abi, kc, seg:seg + 1], scalar2=ab[:, abi + 1, kc, seg:seg + 1],
                        op0=ALU.mult, op1=ALU.add), r=[p, ab], w=[hb])
                else:
                    kb.op(kb.act, lambda p=p, q=q, kc=kc: nc.scalar.activation(
                        out=hb[:, kc, hoff:hoff + 128], in_=p[:, q * 128:(q + 1) * 128], func=AF.Identity,
                        scale=ab[:, abi, kc, seg:seg + 1], bias=ab[:, abi + 1, kc, seg:seg + 1]), r=[p, ab], w=[hb])
                if hf is not None:
                    kb.op(kb.dve, lambda p=p, q=q, kc=kc: nc.vector.tensor_scalar(
                        out=hf[:, kc, :], in0=p[:, q * 128:(q + 1) * 128],
                        scalar1=ab[:, abi, kc, seg:seg + 1], scalar2=ab[:, abi + 1, kc, seg:seg + 1],
                        op0=ALU.mult, op1=ALU.add), r=[p, ab], w=[hf])

    def stager(self, name, shape, dt, n=3):
        bufs = [self.kb.sb(name, shape, dt) for _ in range(n)]
        state = {"i": 0}

        def nxt():
            b = bufs[state["i"] % n]
            state["i"] += 1
            return b
        return nxt

    def proj(self, l, HXT, S):
        kb, nc, I = self.kb, self.nc, self.inp
        NT = self.NT
        old = kb.stk
        with ExitStack() as st:
            kb.stk = st
            gb = kb.sb("gbias", [128, 4], F32)
            with nc.allow_non_contiguous_dma(reason="tiny"):
                kb.dma(kb.sp, gb[:, :], I["gate_b_sp"].t[l, :, :].rearrange("k p -> p k"), w=[gb], sb=gb)
            stg_b = self.stager("stgb", [128, 512], BF16, 4)
            stg_f = self.stager("stgf", [128, 512], F32, 2)
            blockmap = {}
            for b in range(8):
                blockmap[b] = (S["QP"], b * 128, "copy", None)
            for b in range(4):
                blockmap[8 + b] = (S["NAQ"], b * 128, "scale", 0.125)
                blockmap[12 + b] = (S["NAK"], b * 128, "copy", None)
                blockmap[16 + b] = (S["S5U"], b * 128, "copy", None)
                blockmap[44 + b] = (S["GR"], b * 128, "bias", b)
            for b in range(24):
                blockmap[20 + b] = (S["GT"], b * 128, "sigmoid", None)
            cnt = {"i": 0}

            def epi(kb, pl, g, j, t0, tn):
                blk = g["b0"] + j
                dest, row0, kind, prm = blockmap[blk]
                p = pl[0]
                if kind == "bias":
                    sg = stg_f()
                    kb.op(kb.dve, lambda: nc.vector.tensor_scalar(out=sg[:, 0:tn], in0=p[:, 0:tn], scalar1=gb[:, prm:prm + 1],
                                                                 scalar2=None, op0=ALU.add), r=[p, gb], w=[sg])
                else:
                    sg = stg_b()
                    if kind == "sigmoid":
                        kb.op(kb.act, lambda: nc.scalar.activation(out=sg[:, 0:tn], in_=p[:, 0:tn], func=AF.Sigmoid), r=[p], w=[sg])
                    elif kind == "scale":
                        kb.op(kb.dve, lambda: nc.vector.tensor_scalar(out=sg[:, 0:tn], in0=p[:, 0:tn], scalar1=prm, scalar2=None,
                                                                     op0=ALU.mult), r=[p], w=[sg])
                    else:
                        cnt["i"] += 1
                        if cnt["i"] % 2:
                            kb.op(kb.dve, lambda: nc.vector.tensor_copy(out=sg[:, 0:tn], in_=p[:, 0:tn]), r=[p], w=[sg])
                        else:
                            kb.op(kb.act, lambda: nc.scalar.copy(out=sg[:, 0:tn], in_=p[:, 0:tn]), r=[p], w=[sg])
                kb.dma(kb.sp, dest.t[row0:row0 + 128, t0:t0 + tn], sg[:, 0:tn], r=[sg], w=[dest], sb=sg)

            groups = []
            for gi in range(6):
                groups.append(dict(streams=[(0, I["w_fm"].t[l, :, gi * 1024:(gi + 1) * 1024])], nblk=8, b0=gi * 8))
            gemm_fm(kb, NT, [(HXT, 8)], groups, epi, tag="pj")

            stg_t = self.stager("stgt", [128, 1024], BF16, 3)
            tiles = list(range(0, NT, 128))

            def epi_vo(kb, pl, t0):
                sg = stg_t()
                kb.op(kb.dve, lambda: nc.vector.tensor_copy(out=sg[:, 0:512], in_=pl[0][:, :]), r=[pl[0]], w=[sg])
                kb.op(kb.act, lambda: nc.scalar.activation(out=sg[:, 512:1024], in_=pl[1][:, :], func=AF.Sigmoid), r=[pl[1]], w=[sg])
                kb.dma(kb.sp, S["V"].t[t0:t0 + 128, :], sg[:, 0:512], r=[sg], w=[S["V"]], sb=sg)
                kb.dma(kb.sp, S["OS"].t[t0:t0 + 128, :], sg[:, 512:1024], r=[sg], w=[S["OS"]], sb=sg)

            gemm_tm(kb, tiles, HXT, 8, I["w_tm"].t[l, :, 0:1024], 1024, epi_vo, tag="vo")

            def epi_nv(kb, pl, t0):
                sg = stg_t()
                kb.op(kb.dve, lambda: nc.vector.tensor_copy(out=sg[:, 0:512], in_=pl[0][:, :]), r=[pl[0]], w=[sg])
                kb.dma(kb.sp, S["NAV"].t[t0:t0 + 128, :], sg[:, 0:512], r=[sg], w=[S["NAV"]], sb=sg)

            gemm_tm(kb, tiles, HXT, 8, I["w_tm"].t[l, :, 1024:1536], 512, epi_nv, tag="nv")
        kb.stk = old

    def mlstm_qk(self, l, S):
        kb, nc, I = self.kb, self.nc, self.inp
        NT, N = self.NT, self.N
        CH = min(2048, N)
        old = kb.stk
        with ExitStack() as st:
            kb.stk = st
            cos = kb.sb("rcos", [128, N], F32)
            sin = kb.sb("rsin", [128, N], F32)
            kb.dma(kb.sp, cos[:, :], I["rope_cos"].t[:, :], w=[cos], sb=cos)
            kb.dma(kb.sp, sin[:, :], I["rope_sin"].t[:, :], w=[sin], sb=sin)
            rmat = kb.sb("rmat", [128, 128], BF16)
            kb.dma(kb.pool, rmat[:, :], I["rope_rT"].t[:, :], w=[rmat], sb=rmat)
            cw = kb.sb("cw", [128, 8, 4], F32)
            with nc.allow_non_contiguous_dma(reason="tiny conv weights"):
                for j in range(3):
                    kb.dma(kb.sp, cw[:, :, j], I["m_conv_w"].t[l, j, :].rearrange("(b p) -> p b", p=128), w=[cw], sb=cw)
                kb.dma(kb.sp, cw[:, :, 3], I["m_conv_b"].t[l, :].rearrange("(b p) -> p b", p=128), w=[cw], sb=cw)
            xps = self.stager("xp", [128, CH + 2], BF16, 2)
            tms = self.stager("ctm", [128, CH], F32, 2)
            qss = self.stager("cqs", [128, CH], BF16, 2)
            t1s = self.stager("ct1", [128, 512], F32, 2)
            t2s = self.stager("ct2", [128, 512], F32, 2)
            outs = self.stager("cout", [128, CH], BF16, 2)
            pss = [kb.ps("cps", [128, 512]) for _ in range(2)]
            pi = 0
            segs = [(0, CTX, False)] + [(CTX + c0, min(CH, N - c0), True) for c0 in range(0, N, CH)]
            for fb in range(8):
                scale = 1.0 if fb < 4 else 128.0 ** -0.5
                for (u0, n, is_x) in segs:
                    seg_lo = CTX if is_x else 0
                    seg_hi = NT if is_x else CTX
                    xp, tm, qs, ob = xps(), tms(), qss(), outs()
                    lo = max(u0 - 1, seg_lo)
                    hi = min(u0 + n + 1, seg_hi)
                    if lo == u0:
                        kb.op(kb.pool, lambda xp=xp: nc.gpsimd.memset(xp[:, 0:1], 0.0), w=[xp])
                    if hi == u0 + n:
                        kb.op(kb.pool, lambda xp=xp, n=n: nc.gpsimd.memset(xp[:, n + 1:n + 2], 0.0), w=[xp])
                    kb.dma(kb.sp, xp[:, 1 - (u0 - lo):1 - (u0 - lo) + (hi - lo)], S["QP"].t[fb * 128:(fb + 1) * 128, lo:hi],
                           r=[S["QP"]], w=[xp], sb=xp)
                    kb.op(kb.dve, lambda: nc.vector.tensor_scalar(out=tm[:, 0:n], in0=xp[:, 0:n], scalar1=cw[:, fb, 0:1], scalar2=None,
                                                                 op0=ALU.mult), r=[xp, cw], w=[tm])
                    kb.op(kb.dve, lambda: nc.vector.scalar_tensor_tensor(out=tm[:, 0:n], in0=xp[:, 1:n + 1], scalar=cw[:, fb, 1:2],
                                                                        in1=tm[:, 0:n], op0=ALU.mult, op1=ALU.add), r=[xp, cw, tm], w=[tm])
                    kb.op(kb.dve, lambda: nc.vector.scalar_tensor_tensor(out=tm[:, 0:n], in0=xp[:, 2:n + 2], scalar=cw[:, fb, 2:3],
                                                                        in1=tm[:, 0:n], op0=ALU.mult, op1=ALU.add), r=[xp, cw, tm], w=[tm])
                    if not is_x:
                        kb.op(kb.act, lambda: nc.scalar.activation(out=tm[:, 0:n], in_=tm[:, 0:n], func=AF.Silu, bias=cw[:, fb, 3:4]),
                              r=[tm, cw], w=[tm])
                        kb.op(kb.dve, lambda: nc.vector.tensor_scalar(out=ob[:, 0:n], in0=tm[:, 0:n], scalar1=scale, scalar2=None, op0=ALU.mult),
                              r=[tm], w=[ob])
                    else:
                        kb.op(kb.act, lambda: nc.scalar.activation(out=qs[:, 0:n], in_=tm[:, 0:n], func=AF.Silu, bias=cw[:, fb, 3:4]),
                              r=[tm, cw], w=[qs])
                        x0 = u0 - CTX
                        for c0 in range(0, n, 512):
                            cn = min(512, n - c0)
                            p = pss[pi % 2]
                            pi += 1
                            t1, t2 = t1s(), t2s()
                            mm(kb, p[:, 0:cn], rmat[:, :], qs[:, c0:c0 + cn], True, True, r=[rmat, qs], w=[p])
                            kb.op(kb.dve, lambda: nc.vector.tensor_tensor(out=t1[:, 0:cn], in0=p[:, 0:cn], in1=sin[:, x0 + c0:x0 + c0 + cn],
                                                                         op=ALU.mult), r=[p, sin], w=[t1])
                            kb.op(kb.dve, lambda: nc.vector.tensor_tensor(out=t2[:, 0:cn], in0=qs[:, c0:c0 + cn], in1=cos[:, x0 + c0:x0 + c0 + cn],
                                                                          op=ALU.mult), r=[qs, cos], w=[t2])
                            kb.op(kb.dve, lambda: nc.vector.tensor_tensor(out=t2[:, 0:cn], in0=t1[:, 0:cn], in1=t2[:, 0:cn], op=ALU.add), r=[t1, t2], w=[t2])
                            kb.op(kb.dve, lambda: nc.vector.tensor_scalar(out=ob[:, c0:c0 + cn], in0=t2[:, 0:cn], scalar1=scale, scalar2=None,
                                                                          op0=ALU.mult), r=[t2], w=[ob])
                    kb.dma(kb.sp, S["QK"].t[fb * 128:(fb + 1) * 128, u0:u0 + n], ob[:, 0:n], r=[ob], w=[S["QK"]], sb=ob)
        kb.stk = old

    def mlstm_gates(self, l, S, TS, DEC):
        kb, nc, I = self.kb, self.nc, self.inp
        NT, N = self.NT, self.N
        nxc = N // 128
        PCx = min(16, nxc)
        assert nxc % PCx == 0
        PTM = PCx * 128
        old = kb.stk
        with ExitStack() as st:
            kb.stk = st
            sel = kb.sb("sel", [128, 4], F32)
            osg = kb.sb("osg", [128, 4, 128], F32)
            ones = kb.sb("gones", [128, PTM], F32)
            kb.op(kb.pool, lambda: nc.gpsimd.memset(sel[:, :], 0.0), w=[sel])
            kb.op(kb.pool, lambda: nc.gpsimd.memset(osg[:, :, :], 0.0), w=[osg])
            kb.op(kb.pool, lambda: nc.gpsimd.memset(ones[:, :], 1.0), w=[ones])
            for g in range(4):
                kb.op(kb.pool, lambda g=g: nc.gpsimd.memset(sel[32 * g:32 * g + 1, g:g + 1], 1.0), w=[sel])
                kb.op(kb.pool, lambda g=g: nc.gpsimd.memset(osg[32 * g:32 * g + 1, g, :], 1.0), w=[osg])
            Fb = kb.sb("gF", [128, PTM], F32)
            NB = kb.sb("gNB", [128, PTM], F32)
            A = kb.sb("gA", [128, PTM], F32)
            M = kb.sb("gM", [128, PTM], F32)
            MP = kb.sb("gMP", [128, PTM], F32)
            car = kb.sb("gcar", [128, 4], F32)
            dec = kb.sb("gdec", [128, 16], F32)
            pst = kb.ps("gpst", [128, 512])
            psd = kb.ps("gpsd", [128, 512])
            for d in range(2):
                kb.op(kb.dve, lambda: nc.vector.memset(car[:, :], 0.0), w=[car])
                pieces = [(0, 2)] + [(2 + j * PCx, PCx) for j in range(nxc // PCx)]
                for (pc0, pc) in pieces:
                    PT = pc * 128
                    if d == 0:
                        cu0 = pc0
                    else:
                        cu0 = 0 if pc0 == 0 else 2 + (nxc - (pc0 - 2) - pc)
                    u0 = cu0 * 128
                    fk, ik = (1, 0) if d == 0 else (3, 2)
                    if d == 0:
                        kb.dma(kb.sp, Fb[:, 0:PT], S["GR"].t[fk * 128:(fk + 1) * 128, u0:u0 + PT], r=[S["GR"]], w=[Fb], sb=Fb)
                        kb.dma(kb.sp, A[:, 0:PT], S["GR"].t[ik * 128:(ik + 1) * 128, u0:u0 + PT], r=[S["GR"]], w=[A], sb=A)
                    else:
                        kb.dma(kb.sp, MP[:, 0:PT], S["GR"].t[fk * 128:(fk + 1) * 128, u0:u0 + PT], r=[S["GR"]], w=[MP], sb=MP)
                        kb.dma(kb.sp, M[:, 0:PT], S["GR"].t[ik * 128:(ik + 1) * 128, u0:u0 + PT], r=[S["GR"]], w=[M], sb=M)
                        kb.op(kb.dve, lambda: nc.vector.tensor_copy(out=Fb[:, 0:PT], in_=MP[:, PT - 1::-1] if PT == PTM else MP[:, PT - 1::-1]),
                              r=[MP], w=[Fb])
                        kb.op(kb.dve, lambda: nc.vector.tensor_copy(out=A[:, 0:PT], in_=M[:, PT - 1::-1]), r=[M], w=[A])
                    kb.op(kb.act, lambda: nc.scalar.activation(out=Fb[:, 0:PT], in_=Fb[:, 0:PT], func=AF.Exp, scale=-1.0), r=[Fb], w=[Fb])
                    kb.op(kb.act, lambda: nc.scalar.activation(out=Fb[:, 0:PT], in_=Fb[:, 0:PT], func=AF.Ln, bias=1.0), r=[Fb], w=[Fb])
                    kb.op(kb.dve, lambda: nc.vector.tensor_tensor_scan(out=NB[:, 0:PT], data0=ones[:, 0:PT], data1=Fb[:, 0:PT],
                                                                      initial=car[:, 0:1], op0=ALU.mult, op1=ALU.add), r=[ones, Fb, car], w=[NB])
                    kb.op(kb.dve, lambda: nc.vector.tensor_tensor(out=A[:, 0:PT], in0=A[:, 0:PT], in1=NB[:, 0:PT], op=ALU.add), r=[A, NB], w=[A])
                    kb.op(kb.dve, lambda: nc.vector.tensor_tensor_scan(out=M[:, 0:PT], data0=A[:, 0:PT], data1=A[:, 0:PT],
                                                                      initial=car[:, 1:2], op0=ALU.max, op1=ALU.max), r=[A, car], w=[M])
                    kb.op(kb.dve, lambda: nc.vector.tensor_copy(out=MP[:, 0:128], in_=car[:, 1:2].to_broadcast([128, 128])), r=[car], w=[MP])
                    if pc > 1:
                        kb.op(kb.dve, lambda: nc.vector.tensor_copy(
                            out=MP[:, 128:PT].rearrange("p (c t) -> p c t", t=128),
                            in_=M[:, 127:PT - 128:128].unsqueeze(2).to_broadcast([128, pc - 1, 128])), r=[M], w=[MP])
                    kb.op(kb.dve, lambda: nc.vector.tensor_copy(out=car[:, 2:3], in_=NB[:, PT - 1:PT]), r=[NB], w=[car])
                    kb.op(kb.dve, lambda: nc.vector.tensor_copy(out=car[:, 3:4], in_=M[:, PT - 1:PT]), r=[M], w=[car])
                    kb.op(kb.dve, lambda: nc.vector.tensor_tensor(out=dec[:, 0:pc], in0=MP[:, 0:PT:128], in1=M[:, 127:PT:128], op=ALU.subtract),
                          r=[MP, M], w=[dec])
                    kb.op(kb.act, lambda: nc.scalar.activation(out=dec[:, 0:pc], in_=dec[:, 0:pc], func=AF.Exp), r=[dec], w=[dec])
                    kb.op(kb.dve, lambda: nc.vector.tensor_tensor(out=A[:, 0:PT], in0=A[:, 0:PT], in1=MP[:, 0:PT], op=ALU.subtract), r=[A, MP], w=[A])
                    kb.op(kb.act, lambda: nc.scalar.activation(out=A[:, 0:PT], in_=A[:, 0:PT], func=AF.Exp), r=[A], w=[A])
                    kb.op(kb.dve, lambda: nc.vector.tensor_tensor(out=NB[:, 0:PT], in0=NB[:, 0:PT], in1=MP[:, 0:PT], op=ALU.subtract), r=[NB, MP], w=[NB])
                    kb.op(kb.act, lambda: nc.scalar.activation(out=NB[:, 0:PT], in_=NB[:, 0:PT], func=AF.Exp), r=[NB], w=[NB])
                    kb.op(kb.dve, lambda: nc.vector.tensor_copy(out=car[:, 0:2], in_=car[:, 2:4]), r=[car], w=[car])
                    if d == 0:
                        om, th = A, NB
                    else:
                        kb.op(kb.dve, lambda: nc.vector.tensor_copy(out=Fb[:, 0:PT], in_=A[:, PT - 1::-1]), r=[A], w=[Fb])
                        kb.op(kb.dve, lambda: nc.vector.tensor_copy(out=MP[:, 0:PT], in_=NB[:, PT - 1::-1]), r=[NB], w=[MP])
                        om, th = Fb, MP
                    for j in range(pc):
                        mm(kb, pst[:, j * 8:j * 8 + 4], om[:, j * 128:(j + 1) * 128], sel[:, :], True, True, r=[om, sel], w=[pst], sig=False)
                        mm(kb, pst[:, j * 8 + 4:j * 8 + 8], th[:, j * 128:(j + 1) * 128], sel[:, :], True, True, r=[th, sel], w=[pst],
                           sig=(j == pc - 1))
                    kb.op(kb.dve, lambda: nc.vector.tensor_copy(out=TS[:, cu0:cu0 + pc, d, :],
                                                               in_=pst[:, 0:pc * 8].rearrange("p (c k) -> p c k", k=8)), r=[pst], w=[TS])
                    for g in range(4):
                        mm(kb, psd[:, g * 16:g * 16 + pc], osg[:, g, :], dec[:, 0:pc], True, True, r=[osg, dec], w=[psd], sig=(g == 3))
                    for g in range(4):
                        src = psd[:, g * 16:g * 16 + pc] if d == 0 else psd[:, g * 16 + pc - 1::-1][:, 0:pc] if False else None
                        if d == 0:
                            kb.op(kb.dve, lambda g=g: nc.vector.tensor_copy(out=DEC[:, cu0:cu0 + pc, d, g], in_=psd[:, g * 16:g * 16 + pc]),
                                  r=[psd], w=[DEC])
                        else:
                            kb.op(kb.dve, lambda g=g: nc.vector.tensor_copy(out=DEC[:, cu0:cu0 + pc, d, g],
                                                                           in_=psd[:, g * 16:g * 16 + pc][:, ::-1]), r=[psd], w=[DEC])
        kb.stk = old

    def mlstm_scan(self, l, S, TS, DEC):
        kb, nc, I = self.kb, self.nc, self.inp
        NT, N = self.NT, self.N
        nch = NT // 128
        old = kb.stk
        with ExitStack() as st:
            kb.stk = st
            nw = kb.sb("mnw", [128, 512], F32)
            kb.dma(kb.sp, nw[:, :], I["m_norm"].t[l:l + 1, :].partition_broadcast(128), w=[nw], sb=nw)
            CT32 = kb.sb("CT32", [128, 4, 129], F32)
            CTb = kb.sb("CTb", [128, 4, 129], BF16)
            qTs = self.stager("mq", [128, 4, 128], BF16, 2)
            kTs = self.stager("mk", [128, 4, 128], BF16, 2)
            vs = self.stager("mv", [128, 512], BF16, 2)
            kts = self.stager("mkt", [128, 4, 128], BF16, 2)
            vas = self.stager("mva", [128, 4, 129], BF16, 2)
            pTs = self.stager("mpT", [128, 4, 128], BF16, 2)
            hbs = self.stager("mh", [128, 4, 128], F32, 2)
            hfs = self.stager("mhf", [128, 512], F32, 2)
            oss = self.stager("mos", [128, 512], BF16, 2)
            sts = self.stager("mst", [128, 16], F32, 3)
            tts = self.stager("mtt", [128, 4, 129], F32, 2)
            cen = self.stager("mcen", [128, 4, 128], F32, 2)
            sqs = self.stager("msq", [128, 4, 128], F32, 2)
            ybs = self.stager("myb", [128, 512], BF16, 2)
            yTs = self.stager("myT", [128, 4, 128], BF16, 2)
            ps_s = [kb.ps("mps", [128, 512]) for _ in range(2)]
            ps_n = [kb.ps("mpn", [128, 512]) for _ in range(2)]
            ps_c = [kb.ps("mpc", [128, 512]) for _ in range(2)]
            ps_k = kb.ps("mpk", [128, 1024], BF16)
            for d in range(2):
                kb.op(kb.dve, lambda: nc.vector.memset(CT32[:, :, :], 0.0), w=[CT32])
                kb.op(kb.pool, lambda: nc.gpsimd.memset(CTb[:, :, :], 0.0), w=[CTb])
                order = list(range(nch)) if d == 0 else [1, 0] + list(range(nch - 1, 1, -1))
                mask = self.maskf if d == 0 else self.maskb
                for ci, c in enumerate(order):
                    t0 = c * 128
                    qT, kT, v = qTs(), kTs(), vs()
                    kb.dma(kb.sp, qT[:, :, :], S["QK"].t[0:512, t0:t0 + 128].rearrange("(h p) n -> p h n", p=128), r=[S["QK"]], w=[qT], sb=qT)
                    kb.dma(kb.sp, kT[:, :, :], S["QK"].t[512:1024, t0:t0 + 128].rearrange("(h p) n -> p h n", p=128), r=[S["QK"]], w=[kT], sb=kT)
                    kb.dma(kb.sp, v[:, :], S["V"].t[t0:t0 + 128, :], r=[S["V"]], w=[v], sb=v)
                    if d == 1:
                        hf, osb = hfs(), oss()
                        kb.dma(kb.sp, hf[:, :], S["HF"].t[t0:t0 + 128, :], r=[S["HF"]], w=[hf], sb=hf)
                        kb.dma(kb.sp, osb[:, :], S["OS"].t[t0:t0 + 128, :], r=[S["OS"]], w=[osb], sb=osb)
                    kt = kts()
                    for h in range(4):
                        kb.op(kb.pe, lambda h=h: nc.tensor.transpose(out=ps_k[:, h * 128:(h + 1) * 128], in_=kT[:, h, :], identity=self.identb[:, :]),
                              r=[kT, self.identb], w=[ps_k], sig=(h == 3))
                    kb.op(kb.act, lambda: nc.scalar.copy(out=kt[:, :, :], in_=ps_k[:, 0:512].rearrange("p (h e) -> p h e", e=128)), r=[ps_k], w=[kt])
                    va = vas()
                    for h in range(4):
                        kb.op(kb.dve,
                              (lambda h=h: nc.vector.tensor_scalar(out=va[:, h, 0:128], in0=v[:, h * 128:(h + 1) * 128], scalar1=TS[:, c, d, h:h + 1],
                                                                   scalar2=None, op0=ALU.mult)) if h % 2 == 0 else
                              (lambda h=h: nc.vector.tensor_scalar(out=va[:, h, 0:128], in0=v[:, h * 128:(h + 1) * 128], scalar1=TS[:, c, d, h:h + 1],
                                                                   scalar2=None, op0=ALU.mult)), r=[v, TS], w=[va])
                    kb.op(kb.dve, lambda: nc.vector.tensor_copy(out=va[:, :, 128], in_=TS[:, c, d, 0:4]), r=[TS], w=[va])
                    pS = ps_s[ci % 2]
                    for h in range(4):
                        mm(kb, pS[:, h * 128:(h + 1) * 128], kT[:, h, :], qT[:, h, :], True, True, r=[kT, qT], w=[pS], sig=(h == 3))
                    pT = pTs()
                    kb.op(kb.dve, lambda: nc.vector.tensor_tensor(out=pT[:, :, :], in0=pS[:, :].rearrange("p (h t) -> p h t", t=128),
                                                                 in1=mask[:, :].unsqueeze(1).to_broadcast([128, 4, 128]), op=ALU.mult),
                          r=[pS, mask], w=[pT])
                    hb = hbs()
                    stt = sts()
                    for half in range(2):
                        pN = ps_n[half]
                        for hh in range(2):
                            h = half * 2 + hh
                            mm(kb, pN[:, hh * 129:(hh + 1) * 129], pT[:, h, :], va[:, h, :], True, False, r=[pT, va], w=[pN], sig=False)
                            mm(kb, pN[:, hh * 129:(hh + 1) * 129], qT[:, h, :], CTb[:, h, :], False, True, r=[qT, CTb], w=[pN], sig=True)
                        for hh in range(2):
                            h = half * 2 + hh
                            kb.op(kb.act, lambda h=h, hh=hh, pN=pN: nc.scalar.activation(
                                out=stt[:, 8 + h:9 + h], in_=pN[:, hh * 129 + 128:hh * 129 + 129], func=AF.Abs), r=[pN], w=[stt])
                            kb.op(kb.dve, lambda h=h: nc.vector.tensor_scalar(
                                out=stt[:, h:h + 1], in0=stt[:, 8 + h:9 + h], scalar1=TS[:, c, d, 4 + h:5 + h], scalar2=None,
                                op0=ALU.max), r=[stt, TS], w=[stt])
                            kb.op(kb.dve, lambda h=h: nc.vector.reciprocal(out=stt[:, 4 + h:5 + h], in_=stt[:, h:h + 1]), r=[stt], w=[stt])
                            kb.op(kb.act, lambda h=h, hh=hh, pN=pN: nc.scalar.activation(out=hb[:, h, :], in_=pN[:, hh * 129:hh * 129 + 128], func=AF.Copy,
                                                                                        scale=stt[:, 4 + h:5 + h]), r=[pN, stt], w=[hb])
                    tt = tts()
                    for half in range(2):
                        pC = ps_c[half]
                        for hh in range(2):
                            h = half * 2 + hh
                            mm(kb, pC[:, hh * 129:(hh + 1) * 129], kt[:, h, :], va[:, h, :], True, True, r=[kt, va], w=[pC], sig=(hh == 1))
                        kb.op(kb.dve, lambda half=half, pC=pC: nc.vector.tensor_tensor(
                            out=tt[:, half * 2:half * 2 + 2, :], in0=pC[:, 0:258].rearrange("p (h e) -> p h e", e=129),
                            in1=CT32[:, half * 2:half * 2 + 2, :], op=ALU.add), r=[pC, CT32], w=[tt])
                    for h in range(4):
                        kb.op(kb.dve, lambda h=h: nc.vector.tensor_scalar(out=CT32[:, h, :], in0=tt[:, h, :], scalar1=DEC[:, c, d, h:h + 1], scalar2=None,
                                                                         op0=ALU.mult), r=[tt, DEC], w=[CT32])
                        kb.op(kb.act, lambda h=h: nc.scalar.activation(out=CTb[:, h, :], in_=tt[:, h, :], func=AF.Copy, scale=DEC[:, c, d, h:h + 1]),
                              r=[tt, DEC], w=[CTb])
                    if d == 0:
                        kb.dma(kb.sp, S["HF"].t[t0:t0 + 128, :], hb[:, :, :].rearrange("p h e -> p (h e)"), r=[hb], w=[S["HF"]], sb=hb)
                        continue
                    kb.op(kb.dve, lambda: nc.vector.tensor_tensor(out=hb[:, :, :], in0=hb[:, :, :], in1=hf[:, :].rearrange("p (h e) -> p h e", e=128),
                                                                  op=ALU.add), r=[hb, hf], w=[hb])
                    s2 = sts()
                    ce, sq, yb, yT = cen(), sqs(), ybs(), yTs()
                    kb.op(kb.dve, lambda: nc.vector.reduce_sum(out=s2[:, 0:4], in_=hb[:, :, :], axis=AX.X), r=[hb], w=[s2])
                    kb.op(kb.dve, lambda: nc.vector.tensor_scalar(out=s2[:, 0:4], in0=s2[:, 0:4], scalar1=1.0 / 128, scalar2=None, op0=ALU.mult),
                          r=[s2], w=[s2])
                    kb.op(kb.dve, lambda: nc.vector.tensor_tensor(out=ce[:, :, :], in0=hb[:, :, :], in1=s2[:, 0:4].unsqueeze(2).to_broadcast([128, 4, 128]),
                                                                 op=ALU.subtract), r=[hb, s2], w=[ce])
                    kb.op(kb.dve, lambda: nc.vector.tensor_tensor(out=sq[:, :, :], in0=ce[:, :, :], in1=ce[:, :, :], op=ALU.mult), r=[ce], w=[sq])
                    kb.op(kb.dve, lambda: nc.vector.reduce_sum(out=s2[:, 4:8], in_=sq[:, :, :], axis=AX.X), r=[sq], w=[s2])
                    kb.op(kb.dve, lambda: nc.vector.tensor_scalar(out=s2[:, 4:8], in0=s2[:, 4:8], scalar1=1.0 / 128, scalar2=EPS, op0=ALU.mult,
                                                                 op1=ALU.add), r=[s2], w=[s2])
                    kb.op(kb.act, lambda: nc.scalar.activation(out=s2[:, 8:12], in_=s2[:, 4:8], func=AF.Sqrt), r=[s2], w=[s2])
                    kb.op(kb.dve, lambda: nc.vector.reciprocal(out=s2[:, 12:16], in_=s2[:, 8:12]), r=[s2], w=[s2])
                    kb.op(kb.dve, lambda: nc.vector.tensor_tensor(out=ce[:, :, :], in0=ce[:, :, :], in1=s2[:, 12:16].unsqueeze(2).to_broadcast([128, 4, 128]),
                                                                 op=ALU.mult), r=[ce, s2], w=[ce])
                    kb.op(kb.dve, lambda: nc.vector.tensor_tensor(out=ce[:, :, :], in0=ce[:, :, :], in1=nw[:, :].rearrange("p (h e) -> p h e", e=128),
                                                                  op=ALU.mult), r=[ce, nw], w=[ce])
                    kb.op(kb.dve, lambda: nc.vector.tensor_tensor(out=yb[:, :], in0=ce[:, :, :].rearrange("p h e -> p (h e)"), in1=osb[:, :], op=ALU.mult),
                          r=[ce, osb], w=[yb])
                    for cc in range(4):
                        kb.op(kb.pe, lambda cc=cc: nc.tensor.transpose(out=ps_k[:, cc * 128:(cc + 1) * 128], in_=yb[:, cc * 128:(cc + 1) * 128],
                                                                       identity=self.identb[:, :]), r=[yb, self.identb], w=[ps_k], sig=(cc == 3))
                    kb.op(kb.act, lambda: nc.scalar.copy(out=yT[:, :, :], in_=ps_k[:, 0:512].rearrange("p (a b) -> p a b", b=128)), r=[ps_k], w=[yT])
                    kb.dma(kb.sp, S["YMT"].t.rearrange("(c p) n -> p c n", p=128)[:, :, t0:t0 + 128], yT[:, :, :], r=[yT], w=[S["YMT"]], sb=yT)
        kb.stk = old

    def mlstm(self, l, S):
        kb = self.kb
        nch = self.NT // 128
        old = kb.stk
        with ExitStack() as st:
            kb.stk = st
            import os as _os
            _sk = _os.environ.get("MLSKIP", "")
            if "qk" not in _sk:
                self.mlstm_qk(l, S)
            TS = kb.sb("TS", [128, nch, 2, 8], F32)
            DEC = kb.sb("DEC", [128, nch, 2, 4], F32)
            if "gates" not in _sk:
                self.mlstm_gates(l, S, TS, DEC)
            if "scan" in _sk:
                kb.stk = old
                return
            if "TSd" in self.taps:
                tsd = self.dscr("TSd", [128, nch * 16], F32)
                decd = self.dscr("DECd", [128, nch * 8], F32)
                kb.dma(kb.sp, tsd.t[:, :], TS[:, :, :, :].rearrange("p c d k -> p (c d k)"), r=[TS], w=[tsd], sb=TS)
                kb.dma(kb.sp, decd.t[:, :], DEC[:, :, :, :].rearrange("p c d k -> p (c d k)"), r=[DEC], w=[decd], sb=DEC)
            self.mlstm_scan(l, S, TS, DEC)
        kb.stk = old

    def sincos(self, arg, n, cos_out, sin_out, tmps):
        kb, nc = self.kb, self.nc
        argb, ta, tk, tr = tmps
        INV2PI, MAGIC, C1, C2, PIS = 0.15915494309189535, 12582912.0, 6.28125, 0.0019353071795864769, 3.1415925
        for shift, outb in ((0.0, sin_out), (1.5707963267948966, cos_out)):
            kb.op(kb.dve, lambda shift=shift: nc.vector.tensor_scalar(out=ta[:, 0:n], in0=arg, scalar1=shift, scalar2=None, op0=ALU.add),
                  r=[argb], w=[ta])
            kb.op(kb.dve, lambda: nc.vector.tensor_scalar(out=tk[:, 0:n], in0=ta[:, 0:n], scalar1=INV2PI, scalar2=MAGIC, op0=ALU.mult, op1=ALU.add),
                  r=[ta], w=[tk])
            kb.op(kb.dve, lambda: nc.vector.tensor_scalar(out=tk[:, 0:n], in0=tk[:, 0:n], scalar1=-MAGIC, scalar2=None, op0=ALU.add), r=[tk], w=[tk])
            kb.op(kb.dve, lambda: nc.vector.scalar_tensor_tensor(out=tr[:, 0:n], in0=tk[:, 0:n], scalar=-C1, in1=ta[:, 0:n], op0=ALU.mult, op1=ALU.add),
                  r=[tk, ta], w=[tr])
            kb.op(kb.dve, lambda: nc.vector.scalar_tensor_tensor(out=tr[:, 0:n], in0=tk[:, 0:n], scalar=-C2, in1=tr[:, 0:n], op0=ALU.mult, op1=ALU.add),
                  r=[tk, tr], w=[tr])
            kb.op(kb.dve, lambda: nc.vector.tensor_scalar(out=tr[:, 0:n], in0=tr[:, 0:n], scalar1=PIS, scalar2=-PIS, op0=ALU.min, op1=ALU.max),
                  r=[tr], w=[tr])
            ob, oap = outb
            kb.op(kb.act, lambda oap=oap: nc.scalar.activation(out=oap, in_=tr[:, 0:n], func=AF.Sin), r=[tr], w=[ob])

    def s5(self, l, S):
        kb, nc, I = self.kb, self.nc, self.inp
        NT, N = self.NT, self.N
        nch = NT // 128
        nxc = N // 128
        PCx = min(16, nxc)
        PTM = PCx * 128
        Lc = 128
        old = kb.stk
        with ExitStack() as st:
            kb.stk = st
            prm = kb.sb("s5prm", [128, 16, 2, 8], F32)
            raw = kb.sb("s5raw", [128, 2, 16, 4], F32)
            kb.dma(kb.sp, raw[:, :, :, :], I["s5p"].t[l, :, :, :, :].rearrange("d p q k -> p d q k"), w=[raw], sb=raw)
            dvec = kb.sb("s5d", [32, 16], F32)
            with nc.allow_non_contiguous_dma(reason="tiny skip vector"):
                kb.dma(kb.sp, dvec[:, :], I["s5_d"].t[l, :].rearrange("(q r) -> r q", r=32), w=[dvec], sb=dvec)
            jp1i = kb.sb("jp1i", [128, 128], I32)
            jp1 = kb.sb("jp1", [128, 128], F32)
            kb.op(kb.pool, lambda: nc.gpsimd.iota(jp1i[:, :], pattern=[[1, 128]], base=1, channel_multiplier=0), w=[jp1i])
            kb.op(kb.dve, lambda: nc.vector.tensor_copy(out=jp1[:, :], in_=jp1i[:, :]), r=[jp1i], w=[jp1])
            t4 = [kb.sb("s5t", [128, 128], F32) for _ in range(4)]
            for d in range(2):
                lre = raw[:, d, :, 0]
                kb.op(kb.dve, lambda: nc.vector.tensor_scalar(out=raw[:, d, :, 0], in0=raw[:, d, :, 0], scalar1=-1e-4, scalar2=None, op0=ALU.min),
                      r=[raw], w=[raw])
                kb.op(kb.act, lambda: nc.scalar.activation(out=raw[:, d, :, 2], in_=raw[:, d, :, 2], func=AF.Exp), r=[raw], w=[raw])
                kb.op(kb.dve, lambda: nc.vector.tensor_tensor(out=prm[:, :, d, 0], in0=raw[:, d, :, 0], in1=raw[:, d, :, 2], op=ALU.mult), r=[raw], w=[prm])
                kb.op(kb.dve, lambda: nc.vector.tensor_tensor(out=prm[:, :, d, 1], in0=raw[:, d, :, 1], in1=raw[:, d, :, 2], op=ALU.mult), r=[raw], w=[prm])
                kb.op(kb.act, lambda: nc.scalar.activation(out=prm[:, :, d, 2], in_=prm[:, :, d, 0], func=AF.Exp), r=[prm], w=[prm])
                kb.op(kb.dve, lambda: nc.vector.tensor_copy(out=t4[0][:, 0:16], in_=prm[:, :, d, 1]), r=[prm], w=[t4[0]])
                self.sincos(t4[0][:, 0:16], 16, (prm, prm[:, :, d, 5]), (prm, prm[:, :, d, 6]), (t4[0], t4[1], t4[2], t4[3]))
                nr, ni, den, tq = t4[0], t4[1], t4[2], t4[3]
                kb.op(kb.dve, lambda: nc.vector.tensor_tensor(out=nr[:, 0:16], in0=prm[:, :, d, 2], in1=prm[:, :, d, 5], op=ALU.mult), r=[prm], w=[nr])
                kb.op(kb.dve, lambda: nc.vector.tensor_scalar(out=nr[:, 0:16], in0=nr[:, 0:16], scalar1=-1.0, scalar2=None, op0=ALU.add), r=[nr], w=[nr])
                kb.op(kb.dve, lambda: nc.vector.tensor_tensor(out=ni[:, 0:16], in0=prm[:, :, d, 2], in1=prm[:, :, d, 6], op=ALU.mult), r=[prm], w=[ni])
                kb.op(kb.dve, lambda: nc.vector.tensor_tensor(out=den[:, 0:16], in0=raw[:, d, :, 0], in1=raw[:, d, :, 0], op=ALU.mult), r=[raw], w=[den])
                kb.op(kb.dve, lambda: nc.vector.tensor_tensor(out=tq[:, 0:16], in0=raw[:, d, :, 1], in1=raw[:, d, :, 1], op=ALU.mult), r=[raw], w=[tq])
                kb.op(kb.dve, lambda: nc.vector.tensor_tensor(out=den[:, 0:16], in0=den[:, 0:16], in1=tq[:, 0:16], op=ALU.add), r=[den, tq], w=[den])
                kb.op(kb.dve, lambda: nc.vector.reciprocal(out=den[:, 0:16], in_=den[:, 0:16]), r=[den], w=[den])
                kb.op(kb.dve, lambda: nc.vector.tensor_tensor(out=tq[:, 0:16], in0=nr[:, 0:16], in1=raw[:, d, :, 0], op=ALU.mult), r=[nr, raw], w=[tq])
                kb.op(kb.dve, lambda: nc.vector.tensor_tensor(out=tq[:, 16:32], in0=ni[:, 0:16], in1=raw[:, d, :, 1], op=ALU.mult), r=[ni, raw], w=[tq])
                kb.op(kb.dve, lambda: nc.vector.tensor_tensor(out=tq[:, 0:16], in0=tq[:, 0:16], in1=tq[:, 16:32], op=ALU.add), r=[tq], w=[tq])
                kb.op(kb.dve, lambda: nc.vector.tensor_tensor(out=prm[:, :, d, 3], in0=tq[:, 0:16], in1=den[:, 0:16], op=ALU.mult), r=[tq, den], w=[prm])
                kb.op(kb.dve, lambda: nc.vector.tensor_tensor(out=tq[:, 0:16], in0=ni[:, 0:16], in1=raw[:, d, :, 0], op=ALU.mult), r=[ni, raw], w=[tq])
                kb.op(kb.dve, lambda: nc.vector.tensor_tensor(out=tq[:, 16:32], in0=nr[:, 0:16], in1=raw[:, d, :, 1], op=ALU.mult), r=[nr, raw], w=[tq])
                kb.op(kb.dve, lambda: nc.vector.tensor_tensor(out=tq[:, 0:16], in0=tq[:, 0:16], in1=tq[:, 16:32], op=ALU.subtract), r=[tq], w=[tq])
                kb.op(kb.dve, lambda: nc.vector.tensor_tensor(out=prm[:, :, d, 4], in0=tq[:, 0:16], in1=den[:, 0:16], op=ALU.mult), r=[tq, den], w=[prm])
            PCx = min(16, nxc)
            PTM = PCx * 128
            U = kb.sb("s5u", [32, NT], BF16)
            Ur = kb.sb("s5ur", [32, NT], BF16)
            yt = kb.sb("s5y", [32, NT], F32)
            Braw = self.stager("s5b", [128, 2, 16], F32, 2)
            Craw = self.stager("s5c", [128, 2, 16], F32, 2)
            cpi = kb.sb("s5cpi", [128, 128], I32)
            cp1 = kb.sb("s5cp1", [128, 128], F32)
            kb.op(kb.pool, lambda: nc.gpsimd.iota(cpi[:, :], pattern=[[1, 128]], base=1, channel_multiplier=0), w=[cpi])
            kb.op(kb.dve, lambda: nc.vector.tensor_copy(out=cp1[:, :], in_=cpi[:, :]), r=[cpi], w=[cp1])
            BUF = []
            for d in range(2):
                B_ = dict(
                    bd=kb.sb("s5bd", [128, 2, 32], BF16), cd=kb.sb("s5cd", [128, 2, 32], BF16), bT=kb.sb("s5bT", [32, 2, 128], BF16),
                    bb=kb.sb("s5bb", [128, 2, 16], F32), COS=kb.sb("s5cos", [128, 128], F32), SIN=kb.sb("s5sin", [128, 128], F32),
                    RP=kb.sb("s5rp", [128, 128], F32), RHO0=kb.sb("s5rho0", [128, 128], F32), RHOP=kb.sb("s5rhop", [128, PTM], F32),
                    args=kb.sb("s5args", [128, 128], F32), cc=kb.sb("s5cc", [128, 12, 128], F32),
                    VR=kb.sb("s5vr", [128, PTM], BF16), VI=kb.sb("s5vi", [128, PTM], BF16), T1=kb.sb("s5t1", [128, PTM], BF16),
                    T2=kb.sb("s5t2", [128, PTM], BF16), SR=kb.sb("s5sr", [128, PTM], BF16), SI=kb.sb("s5si", [128, PTM], BF16),
                    COSb=kb.sb("s5cosb", [128, 128], BF16), SINb=kb.sb("s5sinb", [128, 128], BF16),
                    car=kb.sb("s5car", [128, 4], F32), tt=[kb.sb("s5tt", [128, 128], F32) for _ in range(3)],
                    psv=[kb.ps("s5pv", [128, 512]) for _ in range(2)], psy=kb.ps("s5py", [128, 512]), pst=kb.ps("s5pt", [128, 1024], BF16))
                BUF.append(B_)
            gout = self.stager("s5go", [32, PTM], BF16, 2)
            nxp = nxc // PCx
            T1, T2 = BUF[0]["T1"], BUF[0]["T2"]

            def run_dir(q, d):
                B_ = BUF[d]
                bd, cd, bT, bb, COS, SIN, RP, RHO0, RHOP = (B_[k] for k in ("bd", "cd", "bT", "bb", "COS", "SIN", "RP", "RHO0", "RHOP"))
                args, cc, VR, VI, T1, T2, SR, SI, car, tt = (B_[k] for k in ("args", "cc", "VR", "VI", "T1", "T2", "SR", "SI", "car", "tt"))
                psv, psy, pst = B_["psv"], B_["psy"], B_["pst"]
                Usrc = U if d == 0 else Ur
                xs, th, rho = prm[:, q, d, 0:1], prm[:, q, d, 1:2], prm[:, q, d, 2:3]
                br, cr = Braw(), Craw()
                kb.dma(kb.sp, br[:, :, :], I["s5B"].t[l, d, q, :, :, :], w=[br], sb=br)
                kb.dma(kb.sp, cr[:, :, :], I["s5CT"].t[l, d, q, :, :, :], w=[cr], sb=cr)
                fre, fim = prm[:, q, d, 3:4], prm[:, q, d, 4:5]
                kb.op(kb.dve, lambda: nc.vector.tensor_scalar(out=bb[:, 0, :], in0=br[:, 0, :], scalar1=fre, scalar2=None, op0=ALU.mult), r=[br, prm], w=[bb])
                kb.op(kb.dve, lambda: nc.vector.tensor_scalar(out=bb[:, 1, :], in0=br[:, 1, :], scalar1=fim, scalar2=None, op0=ALU.mult), r=[br, prm], w=[bb])
                kb.op(kb.dve, lambda: nc.vector.tensor_tensor(out=bb[:, 0, :], in0=bb[:, 0, :], in1=bb[:, 1, :], op=ALU.subtract), r=[bb], w=[bb])
                kb.op(kb.dve, lambda: nc.vector.tensor_scalar(out=bb[:, 1, :], in0=br[:, 1, :], scalar1=fre, scalar2=None, op0=ALU.mult), r=[br, prm], w=[bb])
                kb.op(kb.dve, lambda: nc.vector.scalar_tensor_tensor(out=bb[:, 1, :], in0=br[:, 0, :], scalar=fim, in1=bb[:, 1, :], op0=ALU.mult,
                                                                    op1=ALU.add), r=[br, prm, bb], w=[bb])
                kb.op(kb.pool, lambda: nc.gpsimd.memset(bd[:, :, :], 0.0), w=[bd])
                kb.op(kb.pool, lambda: nc.gpsimd.memset(cd[:, :, :], 0.0), w=[cd])
                yield
                for gh in range(2):
                    ps_, pe_ = gh * 64, gh * 64 + 64
                    kb.op(kb.dve, lambda: nc.vector.tensor_copy(out=bd[ps_:pe_, :, gh * 16:gh * 16 + 16], in_=bb[ps_:pe_, :, :]), r=[bb], w=[bd])
                    kb.op(kb.dve, lambda: nc.vector.tensor_copy(out=cd[ps_:pe_, 0, gh * 16:gh * 16 + 16], in_=cr[ps_:pe_, 0, :]), r=[cr], w=[cd])
                    kb.op(kb.dve, lambda: nc.vector.tensor_scalar(out=cd[ps_:pe_, 1, gh * 16:gh * 16 + 16], in0=cr[ps_:pe_, 1, :],
                                                                 scalar1=-1.0, scalar2=None, op0=ALU.mult), r=[cr], w=[cd])
                for ri in range(2):
                    kb.op(kb.pe, lambda: nc.tensor.transpose(out=pst[0:32, ri * 128:(ri + 1) * 128], in_=bd[:, ri, :], identity=self.identb[:, :]),
                          r=[bd, self.identb], w=[pst], sig=(ri == 1))
                kb.op(kb.act, lambda: nc.scalar.copy(out=bT[:, :, :], in_=pst[0:32, 0:256].rearrange("p (a b) -> p a b", b=128)), r=[pst], w=[bT])
                yield
                kb.op(kb.dve, lambda: nc.vector.tensor_scalar(out=args[:, :], in0=jp1[:, :], scalar1=th, scalar2=None, op0=ALU.mult), r=[jp1, prm], w=[args])
                self.sincos(args[:, :], 128, (COS, COS[:, :]), (SIN, SIN[:, :]), (args, tt[0], tt[1], tt[2]))
                kb.op(kb.act, lambda: nc.scalar.activation(out=RP[:, :], in_=jp1[:, :], func=AF.Exp, scale=xs), r=[jp1, prm], w=[RP])
                COSb, SINb = B_["COSb"], B_["SINb"]
                kb.op(kb.act, lambda: nc.scalar.copy(out=COSb[:, :], in_=COS[:, :]), r=[COS], w=[COSb])
                kb.op(kb.act, lambda: nc.scalar.copy(out=SINb[:, :], in_=SIN[:, :]), r=[SIN], w=[SINb])
                kb.op(kb.dve, lambda: nc.vector.tensor_copy(out=RHO0[:, :], in_=rho.to_broadcast([128, 128])), r=[prm], w=[RHO0])
                kb.op(kb.dve, lambda: nc.vector.memset(RHO0[:, 0:1], 0.0), w=[RHO0])
                kb.op(kb.dve, lambda: nc.vector.tensor_copy(out=RHOP[:, :].rearrange("p (c t) -> p c t", t=128),
                                                            in_=RHO0[:, :].unsqueeze(1).to_broadcast([128, PCx, 128])), r=[RHO0], w=[RHOP])
                yield
                kb.op(kb.dve, lambda: nc.vector.tensor_scalar(out=cc[:, 11, :], in0=cp1[:, :], scalar1=th, scalar2=128.0, op0=ALU.mult, op1=ALU.mult),
                      r=[cp1, prm], w=[cc])
                kb.op(kb.dve, lambda: nc.vector.tensor_copy(out=args[:, :], in_=cc[:, 11, :]), r=[cc], w=[args])
                self.sincos(args[:, :], 128, (cc, cc[:, 0, :]), (cc, cc[:, 1, :]), (args, tt[0], tt[1], tt[2]))
                kb.op(kb.act, lambda: nc.scalar.activation(out=cc[:, 2, :], in_=RP[:, 127:128].to_broadcast([128, 128]), func=AF.Copy), r=[RP], w=[cc])
                kb.op(kb.dve, lambda: nc.vector.memset(car[:, :], 0.0), w=[car])
                yield
                pieces = [(0, 2)] + [(2 + j * PCx, PCx) for j in range(nxp)]
                for k, (pc0, pc) in enumerate(pieces):
                    PT = pc * 128
                    p0 = pc0 * 128
                    for b0 in range(0, PT, 512):
                        bn = min(512, PT - b0)
                        pv = psv
                        mm(kb, pv[0][:, 0:bn], bT[:, 0, :], Usrc[:, p0 + b0:p0 + b0 + bn], True, True, r=[bT, Usrc], w=[pv[0]])
                        mm(kb, pv[1][:, 0:bn], bT[:, 1, :], Usrc[:, p0 + b0:p0 + b0 + bn], True, True, r=[bT, Usrc], w=[pv[1]])
                        nb = bn // 128
                        cb = COS[:, :].unsqueeze(1).to_broadcast([128, nb, 128])
                        sb_ = SIN[:, :].unsqueeze(1).to_broadcast([128, nb, 128])
                        v3 = lambda t: t[:, b0:b0 + bn].rearrange("p (c t) -> p c t", t=128)
                        p3 = lambda t: t[:, 0:bn].rearrange("p (c t) -> p c t", t=128)
                        kb.op(kb.dve, lambda: nc.vector.tensor_tensor(out=v3(VR), in0=p3(pv[0]), in1=cb, op=ALU.mult), r=[pv[0], COS], w=[VR])
                        kb.op(kb.dve, lambda: nc.vector.tensor_tensor(out=v3(T1), in0=p3(pv[1]), in1=sb_, op=ALU.mult), r=[pv[1], SIN], w=[T1])
                        kb.op(kb.dve, lambda: nc.vector.tensor_tensor(out=v3(VI), in0=p3(pv[1]), in1=cb, op=ALU.mult), r=[pv[1], COS], w=[VI])
                        kb.op(kb.dve, lambda: nc.vector.tensor_tensor(out=v3(T2), in0=p3(pv[0]), in1=sb_, op=ALU.mult), r=[pv[0], SIN], w=[T2])
                        yield
                    kb.op(kb.dve, lambda: nc.vector.tensor_tensor(out=VR[:, 0:PT], in0=VR[:, 0:PT], in1=T1[:, 0:PT], op=ALU.add), r=[VR, T1], w=[VR])
                    kb.op(kb.dve, lambda: nc.vector.tensor_tensor(out=VI[:, 0:PT], in0=VI[:, 0:PT], in1=T2[:, 0:PT], op=ALU.subtract), r=[VI, T2], w=[VI])
                    yield
                    kb.op(kb.dve, lambda: nc.vector.tensor_tensor_scan(out=T1[:, 0:PT], data0=RHOP[:, 0:PT], data1=VR[:, 0:PT], initial=0.0,
                                                                      op0=ALU.mult, op1=ALU.add), r=[RHOP, VR], w=[T1])
                    kb.op(kb.dve, lambda: nc.vector.tensor_tensor_scan(out=T2[:, 0:PT], data0=RHOP[:, 0:PT], data1=VI[:, 0:PT], initial=0.0,
                                                                      op0=ALU.mult, op1=ALU.add), r=[RHOP, VI], w=[T2])
                    yield
                    zr, zi = T1[:, 127:PT:128], T2[:, 127:PT:128]
                    cL, sL = COS[:, 127:128], SIN[:, 127:128]
                    kb.op(kb.dve, lambda: nc.vector.tensor_scalar(out=cc[:, 3, 0:pc], in0=zr, scalar1=cL, scalar2=None, op0=ALU.mult), r=[T1, COS], w=[cc])
                    kb.op(kb.dve, lambda: nc.vector.tensor_scalar(out=cc[:, 9, 0:pc], in0=zi, scalar1=sL, scalar2=None, op0=ALU.mult), r=[T2, SIN], w=[cc])
                    kb.op(kb.dve, lambda: nc.vector.tensor_tensor(out=cc[:, 3, 0:pc], in0=cc[:, 3, 0:pc], in1=cc[:, 9, 0:pc], op=ALU.subtract), r=[cc], w=[cc])
                    kb.op(kb.dve, lambda: nc.vector.tensor_scalar(out=cc[:, 4, 0:pc], in0=zr, scalar1=sL, scalar2=None, op0=ALU.mult), r=[T1, SIN], w=[cc])
                    kb.op(kb.dve, lambda: nc.vector.scalar_tensor_tensor(out=cc[:, 4, 0:pc], in0=zi, scalar=cL, in1=cc[:, 4, 0:pc], op0=ALU.mult, op1=ALU.add),
                          r=[T2, COS, cc], w=[cc])
                    cC, sC = cc[:, 0, pc0:pc0 + pc], cc[:, 1, pc0:pc0 + pc]
                    kb.op(kb.dve, lambda: nc.vector.tensor_tensor(out=cc[:, 5, 0:pc], in0=cc[:, 3, 0:pc], in1=cC, op=ALU.mult), r=[cc], w=[cc])
                    kb.op(kb.dve, lambda: nc.vector.tensor_tensor(out=cc[:, 9, 0:pc], in0=cc[:, 4, 0:pc], in1=sC, op=ALU.mult), r=[cc], w=[cc])
                    kb.op(kb.dve, lambda: nc.vector.tensor_tensor(out=cc[:, 5, 0:pc], in0=cc[:, 5, 0:pc], in1=cc[:, 9, 0:pc], op=ALU.add), r=[cc], w=[cc])
                    kb.op(kb.dve, lambda: nc.vector.tensor_tensor(out=cc[:, 6, 0:pc], in0=cc[:, 4, 0:pc], in1=cC, op=ALU.mult), r=[cc], w=[cc])
                    kb.op(kb.dve, lambda: nc.vector.tensor_tensor(out=cc[:, 9, 0:pc], in0=cc[:, 3, 0:pc], in1=sC, op=ALU.mult), r=[cc], w=[cc])
                    kb.op(kb.dve, lambda: nc.vector.tensor_tensor(out=cc[:, 6, 0:pc], in0=cc[:, 6, 0:pc], in1=cc[:, 9, 0:pc], op=ALU.subtract), r=[cc], w=[cc])
                    yield
                    kb.op(kb.dve, lambda: nc.vector.tensor_tensor_scan(out=cc[:, 9, 0:pc], data0=cc[:, 2, 0:pc], data1=cc[:, 5, 0:pc], initial=car[:, 0:1],
                                                                      op0=ALU.mult, op1=ALU.add), r=[cc, car], w=[cc])
                    kb.op(kb.dve, lambda: nc.vector.tensor_tensor_scan(out=cc[:, 10, 0:pc], data0=cc[:, 2, 0:pc], data1=cc[:, 6, 0:pc], initial=car[:, 1:2],
                                                                      op0=ALU.mult, op1=ALU.add), r=[cc, car], w=[cc])
                    kb.op(kb.dve, lambda: nc.vector.tensor_copy(out=cc[:, 7, 0:1], in_=car[:, 2:3]), r=[car], w=[cc])
                    kb.op(kb.dve, lambda: nc.vector.tensor_copy(out=cc[:, 8, 0:1], in_=car[:, 3:4]), r=[car], w=[cc])
                    kb.op(kb.dve, lambda: nc.vector.tensor_tensor(out=cc[:, 7, 1:pc + 1], in0=cc[:, 9, 0:pc], in1=cC, op=ALU.mult), r=[cc], w=[cc])
                    kb.op(kb.dve, lambda: nc.vector.tensor_tensor(out=cc[:, 5, 0:pc], in0=cc[:, 10, 0:pc], in1=sC, op=ALU.mult), r=[cc], w=[cc])
                    kb.op(kb.dve, lambda: nc.vector.tensor_tensor(out=cc[:, 7, 1:pc + 1], in0=cc[:, 7, 1:pc + 1], in1=cc[:, 5, 0:pc], op=ALU.subtract), r=[cc], w=[cc])
                    kb.op(kb.dve, lambda: nc.vector.tensor_tensor(out=cc[:, 8, 1:pc + 1], in0=cc[:, 9, 0:pc], in1=sC, op=ALU.mult), r=[cc], w=[cc])
                    kb.op(kb.dve, lambda: nc.vector.tensor_tensor(out=cc[:, 5, 0:pc], in0=cc[:, 10, 0:pc], in1=cC, op=ALU.mult), r=[cc], w=[cc])
                    kb.op(kb.dve, lambda: nc.vector.tensor_tensor(out=cc[:, 8, 1:pc + 1], in0=cc[:, 8, 1:pc + 1], in1=cc[:, 5, 0:pc], op=ALU.add), r=[cc], w=[cc])
                    kb.op(kb.dve, lambda: nc.vector.tensor_copy(out=car[:, 0:1], in_=cc[:, 9, pc - 1:pc]), r=[cc], w=[car])
                    kb.op(kb.dve, lambda: nc.vector.tensor_copy(out=car[:, 1:2], in_=cc[:, 10, pc - 1:pc]), r=[cc], w=[car])
                    kb.op(kb.dve, lambda: nc.vector.tensor_copy(out=car[:, 2:3], in_=cc[:, 7, pc:pc + 1]), r=[cc], w=[car])
                    kb.op(kb.dve, lambda: nc.vector.tensor_copy(out=car[:, 3:4], in_=cc[:, 8, pc:pc + 1]), r=[cc], w=[car])
                    yield
                    rpb = RP[:, :].unsqueeze(1).to_broadcast([128, pc, 128])
                    z3 = lambda t: t[:, 0:PT].rearrange("p (c t) -> p c t", t=128)
                    for c_i in range(pc):
                        kb.op(kb.act, lambda: nc.scalar.activation(out=VR[:, c_i * 128:(c_i + 1) * 128], in_=RP[:, :], func=AF.Copy,
                                                                   scale=cc[:, 7, c_i:c_i + 1]), r=[RP, cc], w=[VR])
                        kb.op(kb.act, lambda: nc.scalar.activation(out=VI[:, c_i * 128:(c_i + 1) * 128], in_=RP[:, :], func=AF.Copy,
                                                                   scale=cc[:, 8, c_i:c_i + 1]), r=[RP, cc], w=[VI])
                    kb.op(kb.dve, lambda: nc.vector.tensor_tensor(out=T1[:, 0:PT], in0=T1[:, 0:PT], in1=VR[:, 0:PT], op=ALU.add), r=[T1, VR], w=[T1])
                    kb.op(kb.dve, lambda: nc.vector.tensor_tensor(out=T2[:, 0:PT], in0=T2[:, 0:PT], in1=VI[:, 0:PT], op=ALU.add), r=[T2, VI], w=[T2])
                    yield
                    cb = COSb[:, :].unsqueeze(1).to_broadcast([128, pc, 128])
                    sb_ = SINb[:, :].unsqueeze(1).to_broadcast([128, pc, 128])
                    kb.op(kb.dve, lambda: nc.vector.tensor_tensor(out=z3(VR), in0=z3(T1), in1=cb, op=ALU.mult), r=[T1, COSb], w=[VR])
                    kb.op(kb.dve, lambda: nc.vector.tensor_tensor(out=z3(VI), in0=z3(T2), in1=sb_, op=ALU.mult), r=[T2, SINb], w=[VI])
                    kb.op(kb.dve, lambda: nc.vector.tensor_tensor(out=SR[:, 0:PT], in0=VR[:, 0:PT], in1=VI[:, 0:PT], op=ALU.subtract), r=[VR, VI], w=[SR])
                    yield
                    kb.op(kb.dve, lambda: nc.vector.tensor_tensor(out=z3(VR), in0=z3(T1), in1=sb_, op=ALU.mult), r=[T1, SINb], w=[VR])
                    kb.op(kb.dve, lambda: nc.vector.tensor_tensor(out=z3(VI), in0=z3(T2), in1=cb, op=ALU.mult), r=[T2, COSb], w=[VI])
                    kb.op(kb.dve, lambda: nc.vector.tensor_tensor(out=SI[:, 0:PT], in0=VR[:, 0:PT], in1=VI[:, 0:PT], op=ALU.add), r=[VR, VI], w=[SI])
                    yield
                    if k == 0:
                        second = (d == 1)
                    else:
                        other_k = nxp - (k - 1)
                        second = (other_k < k) or (other_k == k and d == 1)
                    for b0 in range(0, PT, 512):
                        bn = min(512, PT - b0)
                        py = psy
                        mm(kb, py[0:32, 0:bn], cd[:, 0, :], SR[:, b0:b0 + bn], True, False, r=[cd, SR], w=[py], sig=False)
                        mm(kb, py[0:32, 0:bn], cd[:, 1, :], SI[:, b0:b0 + bn], False, True, r=[cd, SI], w=[py], sig=True)
                        pp = p0 + b0
                        if d == 0:
                            u_lo, u_hi = pp, pp + bn - 1
                            src = py[0:32, 0:bn]
                        else:
                            u_hi = (CTX - 1 - pp) if pp < CTX else (NT - 1 - (pp - CTX))
                            u_lo = u_hi - bn + 1
                            src = py[0:32, 0:bn][:, ::-1]
                        if second:
                            kb.op(kb.dve, lambda: nc.vector.tensor_tensor(out=yt[:, u_lo:u_hi + 1], in0=src, in1=yt[:, u_lo:u_hi + 1], op=ALU.add),
                                  r=[py, yt], w=[yt])
                        else:
                            kb.op(kb.dve, lambda: nc.vector.tensor_copy(out=yt[:, u_lo:u_hi + 1], in_=src), r=[py], w=[yt])
                        yield

            for q in range(16):
                kb.dma(kb.sp, U[:, :], S["S5U"].t[32 * q:32 * q + 32, :], r=[S["S5U"]], w=[U], sb=U)
                kb.op(kb.dve, lambda: nc.vector.tensor_copy(out=Ur[:, 0:CTX], in_=U[:, CTX - 1::-1]), r=[U], w=[Ur])
                kb.op(kb.dve, lambda: nc.vector.tensor_copy(out=Ur[:, CTX:NT], in_=U[:, NT - 1:CTX - 1:-1]), r=[U], w=[Ur])
                gens = [run_dir(q, 0), run_dir(q, 1)]
                alive = [True, True]
                while any(alive):
                    for gi_, g_ in enumerate(gens):
                        if alive[gi_]:
                            try:
                                next(g_)
                            except StopIteration:
                                alive[gi_] = False
                for g0 in range(0, NT, PTM):
                    gn = min(PTM, NT - g0)
                    go = gout()
                    ysl, usl = yt[:, g0:g0 + gn], U[:, g0:g0 + gn]
                    G1, G2 = BUF[0]["RHOP"], BUF[1]["RHOP"]
                    a1, a2 = G1[0:32, 0:gn], G2[0:32, 0:gn]
                    kb.op(kb.dve, lambda: nc.vector.scalar_tensor_tensor(out=ysl, in0=usl, scalar=dvec[:, q:q + 1], in1=ysl, op0=ALU.mult, op1=ALU.add),
                          r=[U, dvec, yt], w=[yt])
                    kb.op(kb.dve, lambda: nc.vector.tensor_tensor(out=a1, in0=ysl, in1=ysl, op=ALU.mult), r=[yt], w=[G1])
                    kb.op(kb.dve, lambda: nc.vector.tensor_scalar(out=a1, in0=a1, scalar1=0.044715, scalar2=1.0, op0=ALU.mult, op1=ALU.add), r=[G1], w=[G1])
                    kb.op(kb.dve, lambda: nc.vector.tensor_tensor(out=a1, in0=a1, in1=ysl, op=ALU.mult), r=[G1, yt], w=[G1])
                    kb.op(kb.act, lambda: nc.scalar.activation(out=a2, in_=a1, func=AF.Sigmoid, scale=1.5957691216057308), r=[G1], w=[G2])
                    kb.op(kb.dve, lambda: nc.vector.tensor_tensor(out=go[:, 0:gn], in0=a2, in1=ysl, op=ALU.mult), r=[G2, yt], w=[go])
                    kb.dma(kb.sp, S["GS"].t[32 * q:32 * q + 32, g0:g0 + gn], go[:, 0:gn], r=[go], w=[S["GS"]], sb=go)
        kb.stk = old
        with ExitStack() as st:
            kb.stk = st
            gb = kb.sb("glub", [128, 4], F32)
            with nc.allow_non_contiguous_dma(reason="tiny bias"):
                kb.dma(kb.sp, gb[:, :], I["s5_glu_b"].t[l, :].rearrange("(j p) -> p j", p=128), w=[gb], sb=gb)
            gts = self.stager("glug", [128, 512], BF16, 3)
            sgs = self.stager("glus", [128, 512], BF16, 3)
            ots = self.stager("gluo", [128, 512], BF16, 3)

            def epi(kb, pl, g, j, t0, tn):
                gt, sg, ot = gts(), sgs(), ots()
                kb.dma(kb.sp, gt[:, 0:tn], S["GS"].t[j * 128:(j + 1) * 128, t0:t0 + tn], r=[S["GS"]], w=[gt], sb=gt)
                kb.op(kb.act, lambda: nc.scalar.activation(out=sg[:, 0:tn], in_=pl[0][:, 0:tn], func=AF.Sigmoid, bias=gb[:, j:j + 1]), r=[pl[0], gb], w=[sg])
                kb.op(kb.dve, lambda: nc.vector.tensor_tensor(out=ot[:, 0:tn], in0=sg[:, 0:tn], in1=gt[:, 0:tn], op=ALU.mult), r=[sg, gt], w=[ot])
                kb.dma(kb.sp, S["YST"].t[j * 128:(j + 1) * 128, t0:t0 + tn], ot[:, 0:tn], r=[ot], w=[S["YST"]], sb=ot)

            gemm_fm(kb, NT, [(S["GS"], 4)], [dict(streams=[(0, I["s5_glu_w"].t[l, :, :])], nblk=4)], epi, wbufs=1, tag="glu")
        kb.stk = old

    def na_plan(self):
        N = self.N
        nq = N // 128
        rows = N // GRID_W
        win_r = min(8, rows)
        nkt = min(5, nq)
        plan, cases = [], {}
        for qt in range(nq):
            kt0 = int(np.clip(qt - 2, 0, nq - nkt))
            r0s = tuple(int(np.clip(r - win_r // 2, 0, rows - win_r)) - 2 * kt0 for r in (2 * qt, 2 * qt + 1))
            key = (qt - kt0,) + r0s
            if key not in cases:
                cases[key] = len(cases)
            plan.append((kt0, cases[key]))
        return plan, cases, nkt, win_r

    def na(self, l, S):
        kb, nc, I = self.kb, self.nc, self.inp
        NT, N = self.NT, self.N
        plan, cases, nkt, win_r = self.na_plan()
        nloc = nkt * 128
        old = kb.stk
        with ExitStack() as st:
            kb.stk = st
            kctx = kb.sb("nkc", [128, 4, CTX], BF16)
            vctx = kb.sb("nvc", [128, 2, 512], BF16)
            kb.dma(kb.sp, kctx[:, :, :], S["NAK"].t.rearrange("(c p) n -> p c n", p=128)[:, :, 0:CTX], r=[S["NAK"]], w=[kctx], sb=kctx)
            kb.dma(kb.sp, vctx[:, :, :], S["NAV"].t[0:CTX, :].rearrange("(i p) m -> p i m", p=128), r=[S["NAV"]], w=[vctx], sb=vctx)
            am = kb.sb("nam", [128, 8, nloc], F32)
            qts = self.stager("nq", [128, 4, 128], BF16, 2)
            kts = self.stager("nk", [128, 4, nloc], BF16, 2)
            vts = self.stager("nv", [128, nkt, 512], BF16, 2)
            sbs = self.stager("ns", [128, nloc + CTX], F32, 4)
            pbs = self.stager("np", [128, nloc + CTX], BF16, 4)
            ptb = self.stager("npt", [128, nkt + 2, 128], BF16, 4)
            sts = self.stager("nst", [128, 4], F32, 6)
            yts = self.stager("ny", [128, 512], BF16, 2)
            yTs = self.stager("nyT", [128, 4, 128], BF16, 2)
            psA = [kb.ps("npa", [128, 512]) for _ in range(2)]
            psB = [kb.ps("npb", [128, 512]) for _ in range(2)]
            psT = [kb.ps("npt", [128, 1024], BF16) for _ in range(2)]
            psO = [kb.ps("npo", [128, 512]) for _ in range(2)]
            kb_ps_y = psT[0]
            cur_case = None
            hc = 0
            tiles = [("c", 0), ("c", 1)] + [("x", qt) for qt in range(N // 128)]
            for kind, qt in tiles:
                t0 = qt * 128 if kind == "c" else CTX + qt * 128
                qT = qts()
                kb.dma(kb.sp, qT[:, :, :], S["NAQ"].t.rearrange("(c p) n -> p c n", p=128)[:, :, t0:t0 + 128], r=[S["NAQ"]], w=[qT], sb=qT)
                if kind == "x":
                    kt0, case = plan[qt]
                    k0 = CTX + kt0 * 128
                    kT, vT = kts(), vts()
                    kb.dma(kb.sp, kT[:, :, :], S["NAK"].t.rearrange("(c p) n -> p c n", p=128)[:, :, k0:k0 + nloc], r=[S["NAK"]], w=[kT], sb=kT)
                    kb.dma(kb.sp, vT[:, :, :], S["NAV"].t[k0:k0 + nloc, :].rearrange("(i p) m -> p i m", p=128), r=[S["NAV"]], w=[vT], sb=vT)
                    if case != cur_case:
                        kb.dma(kb.sp, am[:, :, :], I["na_am"].t[l, case, :, :, :].rearrange("h q k -> q h k"), w=[am], sb=am)
                        cur_case = case
                    nk = nloc + CTX
                else:
                    nk = CTX
                yt = yts()
                nkt_all = nk // 128
                for h0 in range(0, 8, 2):
                    ctxs = []
                    for h in (h0, h0 + 1):
                        c_ = dict(h=h, hp=h // 2, pb=(h % 2) * 64, pa=psA[h % 2], pbk=psB[h % 2], pt=psT[h % 2], po=psO[h % 2],
                                  ssb=sbs(), pbf=pbs(), ptt=ptb(), stt=sts())
                        c_["lq"] = qT[c_["pb"]:c_["pb"] + 64, c_["hp"], :]
                        ctxs.append(c_)
                    hc += 2
                    for c_ in ctxs:
                        h, hp, pb, pa, pbk, ssb, lq = c_["h"], c_["hp"], c_["pb"], c_["pa"], c_["pbk"], c_["ssb"], c_["lq"]
                        if kind == "x":
                            n1 = min(512, nloc)
                            mm(kb, pa[:, 0:n1], lq, kT[pb:pb + 64, hp, 0:n1], True, True, r=[qT, kT], w=[pa])
                            if nloc > 512:
                                mm(kb, pbk[:, 0:nloc - 512], lq, kT[pb:pb + 64, hp, 512:nloc], True, True, r=[qT, kT], w=[pbk])
                            mm(kb, pbk[:, 128:128 + CTX], lq, kctx[pb:pb + 64, hp, :], True, True, r=[qT, kctx], w=[pbk])
                        else:
                            mm(kb, pa[:, 0:CTX], lq, kctx[pb:pb + 64, hp, :], True, True, r=[qT, kctx], w=[pa])
                    for c_ in ctxs:
                        h, pa, pbk, ssb = c_["h"], c_["pa"], c_["pbk"], c_["ssb"]
                        if kind == "x":
                            n1 = min(512, nloc)
                            kb.op(kb.dve, lambda: nc.vector.tensor_tensor(out=ssb[:, 0:n1], in0=pa[:, 0:n1], in1=am[:, h, 0:n1], op=ALU.add),
                                  r=[pa, am], w=[ssb])
                            if nloc > 512:
                                kb.op(kb.dve, lambda: nc.vector.tensor_tensor(out=ssb[:, 512:nloc], in0=pbk[:, 0:nloc - 512], in1=am[:, h, 512:nloc],
                                                                             op=ALU.add), r=[pbk, am], w=[ssb])
                            kb.op(kb.act, lambda: nc.scalar.copy(out=ssb[:, nloc:nloc + CTX], in_=pbk[:, 128:128 + CTX]), r=[pbk], w=[ssb])
                        else:
                            kb.op(kb.act, lambda: nc.scalar.copy(out=ssb[:, 0:CTX], in_=pa[:, 0:CTX]), r=[pa], w=[ssb])
                    for c_ in ctxs:
                        ssb, stt = c_["ssb"], c_["stt"]
                        kb.op(kb.dve, lambda: nc.vector.reduce_max(out=stt[:, 0:1], in_=ssb[:, 0:nk], axis=AX.X), r=[ssb], w=[stt])
                        kb.op(kb.dve, lambda: nc.vector.tensor_scalar(out=stt[:, 1:2], in0=stt[:, 0:1], scalar1=-1.0, scalar2=None, op0=ALU.mult),
                              r=[stt], w=[stt])
                    for c_ in ctxs:
                        ssb, stt, pbf = c_["ssb"], c_["stt"], c_["pbf"]
                        kb.op(kb.act, lambda: nc.scalar.activation(out=pbf[:, 0:nk], in_=ssb[:, 0:nk], func=AF.Exp, bias=stt[:, 1:2],
                                                                   accum_out=stt[:, 2:3]), r=[ssb, stt], w=[pbf, stt])
                        kb.op(kb.dve, lambda: nc.vector.reciprocal(out=stt[:, 3:4], in_=stt[:, 2:3]), r=[stt], w=[stt])
                    for c_ in ctxs:
                        pbf, pt = c_["pbf"], c_["pt"]
                        for kt in range(nkt_all):
                            kb.op(kb.pe, lambda kt=kt: nc.tensor.transpose(out=pt[:, kt * 128:(kt + 1) * 128], in_=pbf[:, kt * 128:(kt + 1) * 128],
                                                                           identity=self.identb[:, :]),
                                  r=[pbf, self.identb], w=[pt], sig=(kt == nkt_all - 1))
                    for ci_, c_ in enumerate(ctxs):
                        pt, ptt = c_["pt"], c_["ptt"]
                        if ci_ == 0:
                            kb.op(kb.act, lambda: nc.scalar.copy(out=ptt[:, 0:nkt_all, :], in_=pt[:, 0:nk].rearrange("p (a b) -> p a b", b=128)),
                                  r=[pt], w=[ptt])
                        else:
                            kb.op(kb.dve, lambda: nc.vector.tensor_copy(out=ptt[:, 0:nkt_all, :], in_=pt[:, 0:nk].rearrange("p (a b) -> p a b", b=128)),
                                  r=[pt], w=[ptt])
                    for c_ in ctxs:
                        h, ptt, po, stt = c_["h"], c_["ptt"], c_["po"], c_["stt"]
                        for kt in range(nkt_all):
                            if kind == "x":
                                rv = vT[:, kt, h * 64:(h + 1) * 64] if kt < nkt else vctx[:, kt - nkt, h * 64:(h + 1) * 64]
                                rb = vT if kt < nkt else vctx
                            else:
                                rv, rb = vctx[:, kt, h * 64:(h + 1) * 64], vctx
                            mm(kb, po[:, 0:64], ptt[:, kt, :], rv, kt == 0, kt == nkt_all - 1, r=[ptt, rb], w=[po])
                    for c_ in ctxs:
                        h, po, stt = c_["h"], c_["po"], c_["stt"]
                        kb.op(kb.dve, lambda: nc.vector.tensor_scalar(out=yt[:, h * 64:(h + 1) * 64], in0=po[:, 0:64], scalar1=stt[:, 3:4],
                                                                     scalar2=None, op0=ALU.mult), r=[po, stt], w=[yt])
                pt = kb_ps_y
                yT = yTs()
                for c in range(4):
                    kb.op(kb.pe, lambda c=c: nc.tensor.transpose(out=pt[:, c * 128:(c + 1) * 128], in_=yt[:, c * 128:(c + 1) * 128],
                                                                 identity=self.identb[:, :]), r=[yt, self.identb], w=[pt], sig=(c == 3))
                kb.op(kb.act, lambda: nc.scalar.copy(out=yT[:, :, :], in_=pt[:, 0:512].rearrange("p (a b) -> p a b", b=128)), r=[pt], w=[yT])
                kb.dma(kb.sp, S["YNT"].t.rearrange("(c p) n -> p c n", p=128)[:, :, t0:t0 + 128], yT[:, :, :], r=[yT], w=[S["YNT"]], sb=yT)
        kb.stk = old

    def merge(self, l, S):
        kb, nc, I = self.kb, self.nc, self.inp
        NT = self.NT
        old = kb.stk
        with ExitStack() as st:
            kb.stk = st
            gts = [kb.sb("mgt", [128, 24, 512], BF16) for _ in range(2)]
            t1s = self.stager("mt1", [128, 512], F32, 2)
            t2s = self.stager("mt2", [128, 512], F32, 2)
            t3s = self.stager("mt3", [128, 512], F32, 2)
            ys = self.stager("myo", [128, 512], BF16, 3)
            cur = {"g": None, "i": 0}

            def epi(kb, pl, g, j, t0, tn):
                if j == 0:
                    gt = gts[cur["i"] % 2]
                    cur["i"] += 1
                    kb.dma(kb.sp, gt[:, :, 0:tn], S["GT"].t.rearrange("(c p) n -> p c n", p=128)[:, :, t0:t0 + tn],
                           r=[S["GT"]], w=[gt], sb=gt)
                    cur["g"] = gt
                gt = cur["g"]
                t1, t2, t3, yo = t1s(), t2s(), t3s(), ys()
                kb.op(kb.dve, lambda: nc.vector.tensor_tensor(out=t1[:, 0:tn], in0=pl[0][:, 0:tn], in1=gt[:, j, 0:tn], op=ALU.mult),
                      r=[pl[0], gt], w=[t1])
                kb.op(kb.dve, lambda: nc.vector.tensor_tensor(out=t2[:, 0:tn], in0=pl[1][:, 0:tn], in1=gt[:, 8 + j, 0:tn], op=ALU.mult),
                      r=[pl[1], gt], w=[t2])
                kb.op(kb.dve, lambda: nc.vector.tensor_tensor(out=t3[:, 0:tn], in0=pl[2][:, 0:tn], in1=gt[:, 16 + j, 0:tn], op=ALU.mult),
                      r=[pl[2], gt], w=[t3])
                kb.op(kb.dve, lambda: nc.vector.tensor_tensor(out=t1[:, 0:tn], in0=t1[:, 0:tn], in1=t2[:, 0:tn], op=ALU.add),
                      r=[t1, t2], w=[t1])
                kb.op(kb.dve, lambda: nc.vector.tensor_tensor(out=yo[:, 0:tn], in0=t1[:, 0:tn], in1=t3[:, 0:tn], op=ALU.add),
                      r=[t1, t3], w=[yo])
                kb.dma(kb.sp, S["YT"].t[j * 128:(j + 1) * 128, t0:t0 + tn], yo[:, 0:tn], r=[yo], w=[S["YT"]], sb=yo)

            groups = [dict(streams=[(0, I["w_branch_m"].t[l, :, :]), (1, I["w_branch_na"].t[l, :, :]),
                                    (2, I["w_branch_s5"].t[l, :, :])], nblk=8)]
            gemm_fm(kb, NT, [(S["YMT"], 4), (S["YNT"], 4), (S["YST"], 4)], groups, epi, wbufs=1, tag="mg",
                    t_lo=(CTX if l == self.depth - 1 else 0))
        kb.stk = old

    def make_residual(self, S, mods, gmk_x, prenorm=None, router=None, src_acc=None):
        kb, nc = self.kb, self.nc
        xts = self.stager("rxt", [128, 1024], F32, 3)
        tms = self.stager("rtm", [128, 1024], F32, 2)
        sq = kb.sb("rsq", [128, 1024], BF16)
        sts = self.stager("rst", [128, 8], F32, 3)
        gm = mods["gm"]
        state = {"i": 0, "hb": None, "first_t0": None}
        if prenorm is not None:
            pts = [kb.ps("rptr", [128, 512]) for _ in range(2)]
            hbl = [kb.sb("rhbl", [128, 8, 512], BF16) for _ in range(2)]
            xns = self.stager("rxn", [128, 1024], F32, 2)
            st2 = self.stager("rst2", [128, 4], F32, 3)

        def flush():
            if prenorm is not None and state["hb"] is not None:
                hb, t0f, n = state["hb"], state["first_t0"], state["n"]
                kb.dma(kb.sp, S["HXT"].t.rearrange("(kc p) n -> p kc n", p=128)[:, :, t0f:t0f + n], hb[:, :, 0:n],
                       r=[hb], w=[S["HXT"]], sb=hb)
                state["hb"] = None

        def epi(kb, pl, t0, src=None):
            seg = 1 if t0 < CTX else 0
            k = gmk_x + seg
            xt, tm, stt = xts(), tms(), sts()
            kb.dma(kb.sp, xt[:, :], S["XU"].t[t0:t0 + 128, :], r=[S["XU"]], w=[xt], sb=xt)
            srcs = [(pl[0][:, :], [pl[0]]), (pl[1][:, :], [pl[1]])] if src is None else src
            for h in range(2):
                ap, bufs = srcs[h]
                kb.op(kb.act, lambda ap=ap, h=h: nc.scalar.activation(out=sq[:, h * 512:(h + 1) * 512], in_=ap, func=AF.Square,
                                                                      accum_out=stt[:, 4 + h:5 + h]), r=bufs, w=[sq, stt])
            kb.op(kb.dve, lambda: nc.vector.tensor_tensor(out=stt[:, 0:1], in0=stt[:, 4:5], in1=stt[:, 5:6], op=ALU.add), r=[stt], w=[stt])
            kb.op(kb.dve, lambda: nc.vector.tensor_scalar(out=stt[:, 2:3], in0=stt[:, 0:1], scalar1=1.0 / D, scalar2=EPS,
                                                         op0=ALU.mult, op1=ALU.add), r=[stt], w=[stt])
            kb.op(kb.act, lambda: nc.scalar.activation(out=stt[:, 3:4], in_=stt[:, 2:3], func=AF.Sqrt), r=[stt], w=[stt])
            kb.op(kb.dve, lambda: nc.vector.reciprocal(out=stt[:, 1:2], in_=stt[:, 3:4]), r=[stt], w=[stt])
            for h in range(2):
                ap, bufs = srcs[h]
                kb.op(kb.dve, lambda ap=ap, h=h: nc.vector.scalar_tensor_tensor(
                    out=tm[:, h * 512:(h + 1) * 512], in0=ap, scalar=stt[:, 1:2], in1=gm[:, k, h * 512:(h + 1) * 512],
                    op0=ALU.mult, op1=ALU.mult), r=bufs + [stt, gm], w=[tm])
            kb.op(kb.dve, lambda: nc.vector.tensor_tensor(out=xt[:, :], in0=xt[:, :], in1=tm[:, :], op=ALU.add), r=[xt, tm], w=[xt])
            kb.dma(kb.sp, S["XU"].t[t0:t0 + 128, :], xt[:, :], r=[xt], w=[S["XU"]], sb=xt)
            if prenorm is not None:
                s2 = st2()
                xn = xns()
                self.rstd(xt[:, :], [xt], s2, sq)
                i = state["i"]
                if state["hb"] is None:
                    state["hb"] = hbl[(i // 4) % 2]
                    state["first_t0"] = t0
                    state["n"] = 0
                hb = state["hb"]
                kb.op(kb.dve, lambda: nc.vector.tensor_scalar(out=xn[:, :], in0=xt[:, :], scalar1=s2[:, 1:2], scalar2=None,
                                                             op0=ALU.mult), r=[xt, s2], w=[xn])
                hf = router["hf"]() if (router is not None and seg == 0) else None
                self.norm_T(xn, [xn], None, mods["ab"], 2, seg, pts, 0, hb, state["n"], hf=hf)
                import os as _os
                if hf is not None and _os.environ.get("ROUTER_MODE") != "hf":
                    router["fn"](hf, t0)
                state["n"] += 128
                state["i"] += 1
                if state["n"] == 512:
                    flush()
        return epi, flush

    def make_router(self, l, S):
        kb, nc, I = self.kb, self.nc, self.inp
        j = l // 2
        R = kb.sb("rtw", [128, 8, 8], F32)
        with nc.allow_non_contiguous_dma(reason="tiny router weight"):
            kb.dma(kb.sp, R[:, :, :], I["moe_router"].t[j, :, :].rearrange("(kc p) e -> p kc e", p=128), w=[R], sb=R)
        hfs = self.stager("rhf", [128, 8, 128], F32, 2)
        pr = kb.ps("rtp", [128, 512])
        lgs = self.stager("rlg", [128, 32], F32, 3)

        def fn(hf, t0):
            lg = lgs()
            import os as _os
            if _os.environ.get("ROUTER_OFF"):
                kb.op(kb.dve, lambda: nc.vector.tensor_copy(out=lg[:, 0:8], in_=hf[:, 0, 0:8]), r=[hf], w=[lg])
            else:
                for kc in range(8):
                    mm(kb, pr[:, 0:8], hf[:, kc, :], R[:, kc, :], kc == 0, kc == 7, r=[hf, R], w=[pr])
                kb.op(kb.dve, lambda: nc.vector.tensor_copy(out=lg[:, 0:8], in_=pr[:, 0:8]), r=[pr], w=[lg])
            if _os.environ.get("ROUTER_MODE") == "nomax":
                kb.op(kb.dve, lambda: nc.vector.tensor_copy(out=lg[:, 8:16], in_=lg[:, 0:8]), r=[lg], w=[lg])
            else:
                kb.op(kb.dve, lambda: nc.vector.max(out=lg[:, 8:16], in_=lg[:, 0:8]), r=[lg], w=[lg])
            kb.op(kb.dve, lambda: nc.vector.tensor_tensor(out=lg[:, 16:17], in0=lg[:, 8:9], in1=lg[:, 9:10], op=ALU.subtract), r=[lg], w=[lg])
            kb.op(kb.act, lambda: nc.scalar.activation(out=lg[:, 17:18], in_=lg[:, 16:17], func=AF.Sigmoid), r=[lg], w=[lg])
            kb.op(kb.act, lambda: nc.scalar.activation(out=lg[:, 18:19], in_=lg[:, 16:17], func=AF.Sigmoid, scale=-1.0), r=[lg], w=[lg])
            kb.op(kb.dve, lambda: nc.vector.tensor_scalar(out=lg[:, 24:32], in0=lg[:, 0:8], scalar1=lg[:, 8:9], scalar2=lg[:, 17:18],
                                                         op0=ALU.is_equal, op1=ALU.mult), r=[lg], w=[lg])
            kb.op(kb.dve, lambda: nc.vector.tensor_scalar(out=lg[:, 0:8], in0=lg[:, 0:8], scalar1=lg[:, 9:10], scalar2=lg[:, 18:19],
                                                         op0=ALU.is_equal, op1=ALU.mult), r=[lg], w=[lg])
            kb.op(kb.dve, lambda: nc.vector.tensor_tensor(out=self.cmbt[:, t0 // 128, :], in0=lg[:, 24:32], in1=lg[:, 0:8], op=ALU.add),
                  r=[lg], w=[self.cmbt])
        return {"hf": hfs, "fn": fn}

    def router_phase(self, l, S, mods):
        kb, nc, I = self.kb, self.nc, self.inp
        j = l // 2
        ab = mods["ab"]
        old = kb.stk
        with ExitStack() as st:
            kb.stk = st
            R = kb.sb("rtw", [128, 8, 8], F32)
            with nc.allow_non_contiguous_dma(reason="tiny router weight"):
                kb.dma(kb.sp, R[:, :, :], I["moe_router"].t[j, :, :].rearrange("(kc p) e -> p kc e", p=128), w=[R], sb=R)
            xts = self.stager("qxt", [128, 1024], F32, 3)
            sq = kb.sb("qsq", [128, 1024], BF16)
            sts = self.stager("qst", [128, 4], F32, 3)
            hfs = self.stager("qhf", [128, 8, 128], F32, 2)
            lgs = self.stager("qlg", [128, 32], F32, 3)
            pts = [kb.ps("qpt", [128, 512]) for _ in range(4)]
            prs = [kb.ps("qpr", [128, 512]) for _ in range(2)]
            for ti, t0 in enumerate(range(CTX, self.NT, 128)):
                xt, stt, hf, lg = xts(), sts(), hfs(), lgs()
                kb.dma(kb.sp, xt[:, :], S["XU"].t[t0:t0 + 128, :], r=[S["XU"]], w=[xt], sb=xt)
                self.rstd(xt[:, :], [xt], stt, sq)
                kb.op(kb.dve, lambda: nc.vector.tensor_scalar(out=xt[:, :], in0=xt[:, :], scalar1=stt[:, 1:2], scalar2=None, op0=ALU.mult),
                      r=[xt, stt], w=[xt])
                for half in range(2):
                    p = pts[(ti % 2) * 2 + half]
                    for q in range(4):
                        kc = half * 4 + q
                        kb.op(kb.pe, lambda p=p, q=q, kc=kc: nc.tensor.transpose(out=p[:, q * 128:(q + 1) * 128], in_=xt[:, kc * 128:(kc + 1) * 128],
                                                                                 identity=self.ident[:, :]), r=[xt, self.ident], w=[p], sig=(q == 3))
                    for q in range(4):
                        kc = half * 4 + q
                        if q % 2 == 0:
                            kb.op(kb.dve, lambda p=p, q=q, kc=kc: nc.vector.tensor_scalar(
                                out=hf[:, kc, :], in0=p[:, q * 128:(q + 1) * 128], scalar1=ab[:, 2, kc, 0:1], scalar2=ab[:, 3, kc, 0:1],
                                op0=ALU.mult, op1=ALU.add), r=[p, ab], w=[hf])
                        else:
                            kb.op(kb.act, lambda p=p, q=q, kc=kc: nc.scalar.activation(
                                out=hf[:, kc, :], in_=p[:, q * 128:(q + 1) * 128], func=AF.Identity,
                                scale=ab[:, 2, kc, 0:1], bias=ab[:, 3, kc, 0:1]), r=[p, ab], w=[hf])
                pr = prs[ti % 2]
                for kc in range(8):
                    mm(kb, pr[:, 0:8], hf[:, kc, :], R[:, kc, :], kc == 0, kc == 7, r=[hf, R], w=[pr])
                kb.op(kb.dve, lambda: nc.vector.tensor_copy(out=lg[:, 0:8], in_=pr[:, 0:8]), r=[pr], w=[lg])
                kb.op(kb.dve, lambda: nc.vector.max(out=lg[:, 8:16], in_=lg[:, 0:8]), r=[lg], w=[lg])
                kb.op(kb.dve, lambda: nc.vector.tensor_tensor(out=lg[:, 16:17], in0=lg[:, 8:9], in1=lg[:, 9:10], op=ALU.subtract), r=[lg], w=[lg])
                kb.op(kb.act, lambda: nc.scalar.activation(out=lg[:, 17:18], in_=lg[:, 16:17], func=AF.Sigmoid), r=[lg], w=[lg])
                kb.op(kb.act, lambda: nc.scalar.activation(out=lg[:, 18:19], in_=lg[:, 16:17], func=AF.Sigmoid, scale=-1.0), r=[lg], w=[lg])
                kb.op(kb.dve, lambda: nc.vector.tensor_scalar(out=lg[:, 24:32], in0=lg[:, 0:8], scalar1=lg[:, 8:9], scalar2=lg[:, 17:18],
                                                             op0=ALU.is_equal, op1=ALU.mult), r=[lg], w=[lg])
                kb.op(kb.dve, lambda: nc.vector.tensor_scalar(out=lg[:, 0:8], in0=lg[:, 0:8], scalar1=lg[:, 9:10], scalar2=lg[:, 18:19],
                                                             op0=ALU.is_equal, op1=ALU.mult), r=[lg], w=[lg])
                kb.op(kb.dve, lambda: nc.vector.tensor_tensor(out=self.cmbt[:, t0 // 128, :], in0=lg[:, 24:32], in1=lg[:, 0:8], op=ALU.add),
                      r=[lg], w=[self.cmbt])
        kb.stk = old

    def moe(self, l, S, mods):
        kb, nc, I = self.kb, self.nc, self.inp
        j = l // 2
        NT = self.NT
        xt_tiles = list(range(CTX, NT, 128))
        for e in range(NEXP):
            AT = S["AT2E"][e % 2]
            self.swiglu_fm(S["HXT"], I["moe_w_gate"].t[j, e, :, :], I["moe_w_up"].t[j, e, :, :], EFF, AT, f"me{e}",
                           t_lo=(CTX if l == self.depth - 1 else 0))
            old = kb.stk
            with ExitStack() as st:
                kb.stk = st
                cmb = self.cmbt
                accs = self.stager("acc", [128, 1024], F32, 3)
                if e == NEXP - 1:
                    res_epi, _ = self.make_residual(S, mods, 2)

                def epi(kb, pl, t0, e=e):
                    acc = accs()
                    ti = t0 // 128
                    if e == 0:
                        for h in range(2):
                            kb.op(kb.dve if h == 0 else kb.act,
                                  (lambda h=h: nc.vector.tensor_scalar(out=acc[:, h * 512:(h + 1) * 512], in0=pl[h][:, :],
                                                                       scalar1=cmb[:, ti, e:e + 1], scalar2=None, op0=ALU.mult)) if h == 0 else
                                  (lambda h=h: nc.scalar.activation(out=acc[:, h * 512:(h + 1) * 512], in_=pl[h][:, :], func=AF.Copy,
                                                                    scale=cmb[:, ti, e:e + 1])),
                                  r=[pl[h], cmb], w=[acc])
                    else:
                        kb.dma(kb.sp, acc[:, :], S["ACC"].t[t0:t0 + 128, :], r=[S["ACC"]], w=[acc], sb=acc)
                        for h in range(2):
                            kb.op(kb.dve, lambda h=h: nc.vector.scalar_tensor_tensor(
                                out=acc[:, h * 512:(h + 1) * 512], in0=pl[h][:, :], scalar=cmb[:, ti, e:e + 1],
                                in1=acc[:, h * 512:(h + 1) * 512], op0=ALU.mult, op1=ALU.add), r=[pl[h], cmb, acc], w=[acc])
                    if e < NEXP - 1:
                        kb.dma(kb.sp, S["ACC"].t[t0:t0 + 128, :], acc[:, :], r=[acc], w=[S["ACC"]], sb=acc)
                    else:
                        res_epi(kb, None, t0, src=[(acc[:, 0:512], [acc]), (acc[:, 512:1024], [acc])])

                gemm_tm(kb, xt_tiles, AT, EFF // 128, I["moe_w_down"].t[j, e, :, :], 1024, epi, tag=f"md{e}", npairs=3)
            kb.stk = old

    def outproj(self, l, S, mods, router=None):
        kb, nc, I = self.kb, self.nc, self.inp
        old = kb.stk
        with ExitStack() as st:
            kb.stk = st
            router = self.make_router(l, S) if router else None
            epi, flush = self.make_residual(S, mods, 0, prenorm=True, router=router)
            tiles = list(range(CTX if l == self.depth - 1 else 0, self.NT, 128))
            gemm_tm(kb, tiles, S["YT"], 8, I["w_out"].t[l, :, :], 1024, epi, tag="op", npairs=2)
            flush()
        kb.stk = old

    def swiglu_fm(self, A, Wg, Wu, ff, AT2, tag, t_lo=0):
        kb, nc = self.kb, self.nc
        old = kb.stk
        with ExitStack() as st:
            kb.stk = st
            sg = self.stager("sws", [128, 512], F32, 3)
            so = self.stager("swo", [128, 512], BF16, 3)

            def epi(kb, pl, g, j, t0, tn):
                s1, o1 = sg(), so()
                kb.op(kb.act, lambda: nc.scalar.activation(out=s1[:, 0:tn], in_=pl[0][:, 0:tn], func=AF.Silu), r=[pl[0]], w=[s1])
                kb.op(kb.dve, lambda: nc.vector.tensor_tensor(out=o1[:, 0:tn], in0=pl[1][:, 0:tn], in1=s1[:, 0:tn], op=ALU.mult),
                      r=[pl[1], s1], w=[o1])
                r0 = (g["b0"] + j) * 128
                kb.dma(kb.sp, AT2.t[r0:r0 + 128, t0:t0 + tn], o1[:, 0:tn], r=[o1], w=[AT2], sb=o1)

            nb = ff // 128
            groups = []
            b0 = 0
            while b0 < nb:
                n = min(4, nb - b0)
                groups.append(dict(streams=[(0, Wg[:, b0 * 128:(b0 + n) * 128]), (0, Wu[:, b0 * 128:(b0 + n) * 128])], nblk=n, b0=b0))
                b0 += n
            gemm_fm(kb, self.NT, [(A, 8)], groups, epi, tag=tag, t_lo=t_lo)
        kb.stk = old

    def ffn_dense(self, l, S, mods):
        kb, nc, I = self.kb, self.nc, self.inp
        j = l // 2
        self.swiglu_fm(S["HXT"], I["ffn_w_gate"].t[j, :, :], I["ffn_w_up"].t[j, :, :], FF, S["AT2"], "ff")
        old = kb.stk
        with ExitStack() as st:
            kb.stk = st
            epi, flush = self.make_residual(S, mods, 2)
            tiles = list(range(0, self.NT, 128))
            gemm_tm(kb, tiles, S["AT2"], FF // 128, I["ffn_w_down"].t[j, :, :], 1024, epi, tag="fd", npairs=3)
        kb.stk = old

    def declare_inputs(self):
        NT = self.NT
        L = self.depth
        self.din("xu", [NT, D])
        self.din("cc", [2, D])
        self.din("ada_w", [L, D, 6 * D])
        self.din("ada_b", [L, 6 * D])
        for n in ("norm_mix_pre", "norm_mix_post", "norm_ffn_pre", "norm_ffn_post"):
            self.din(n, [L, D])
        self.din("w_fm", [L, D, 6144])
        self.din("w_tm", [L, D, 1536])
        self.din("gate_b_sp", [L, 4, 128])
        for n in ("w_branch_m", "w_branch_na", "w_branch_s5"):
            self.din(n, [L, 512, D])
        self.din("w_out", [L, D, D])
        self.din("ffn_w_gate", [1, D, FF])
        self.din("ffn_w_up", [1, D, FF])
        self.din("ffn_w_down", [1, FF, D])
        self.din("moe_router", [1, D, NEXP])
        self.din("moe_w_gate", [1, NEXP, D, EFF])
        self.din("moe_w_up", [1, NEXP, D, EFF])
        self.din("moe_w_down", [1, NEXP, EFF, D])
        self.din("rope_cos", [128, self.N])
        self.din("rope_sin", [128, self.N])
        self.din("rope_rT", [128, 128])
        self.din("m_conv_w", [L, 3, 1024])
        self.din("m_conv_b", [L, 1024])
        self.din("m_norm", [L, 512])
        self.din("s5p", [L, 2, 128, 16, 4])
        self.din("s5B", [L, 2, 16, 128, 2, 16])
        self.din("s5CT", [L, 2, 16, 128, 2, 16])
        self.din("s5_d", [L, 512])
        self.din("s5_glu_w", [L, 512, 512])
        self.din("s5_glu_b", [L, 512])
        plan, cases, nkt, _ = self.na_plan()
        self.din("na_am", [L, len(cases), 8, 128, nkt * 128])
        for n in self.dbg_in:
            self.din("dbg_" + n, list(self.dbg_in[n][0]), self.dbg_in[n][1])

    def scratch(self):
        NT = self.NT
        S = {}
        S["XU"] = self.dscr("XU", [NT, D], F32)
        S["HXT"] = self.dscr("HXT", [D, NT], BF16)
        S["QP"] = self.dscr("QP", [1024, NT], BF16)
        S["NAQ"] = self.dscr("NAQ", [512, NT], BF16)
        S["NAK"] = self.dscr("NAK", [512, NT], BF16)
        S["S5U"] = self.dscr("S5U", [512, NT], BF16)
        S["GT"] = self.dscr("GT", [3072, NT], BF16)
        S["GR"] = self.dscr("GR", [512, NT], F32)
        S["V"] = self.dscr("V", [NT, 512], BF16)
        S["OS"] = self.dscr("OS", [NT, 512], BF16)
        S["NAV"] = self.dscr("NAV", [NT, 512], BF16)
        for n in ("YMT", "YNT", "YST"):
            S[n] = self.dscr(n, [512, NT], BF16)
        S["GS"] = self.dscr("GS", [512, NT], BF16)
        S["QK"] = self.dscr("QK", [1024, NT], BF16)
        S["HF"] = self.dscr("HF", [NT, 512], F32)
        S["YT"] = self.dscr("YT", [D, NT], BF16)
        S["AT2"] = self.dscr("AT2", [FF, NT], BF16)
        S["AT2E"] = [self.dscr(f"AT2E{i}", [EFF, NT], BF16) for i in range(2)]
        S["ACC"] = self.dscr("ACC", [NT, D], F32)
        S["CMB"] = self.dscr("CMB", [NT, NEXP], F32)
        return S

    def build(self, stop_after=None):
        kb, nc = self.kb, self.nc
        self.declare_inputs()
        S = self.scratch()
        self.S = S
        self.consts()
        NT = self.NT
        cp = kb.sb("cpsem", [1, 1], F32)
        for t0 in range(0, NT, 1024):
            n = min(1024, NT - t0)
            kb.dma(kb.sp, S["XU"].t[t0:t0 + n, :], self.inp["xu"].t[t0:t0 + n, :], w=[S["XU"]], sb=cp)
        for l in range(self.depth):
            old = kb.stk
            with ExitStack() as lst:
                kb.stk = lst
                mods = self.adaln(l)
                self.mods = mods
                self.prenorm_to_hxt(l, mods, S["XU"], S["HXT"], which=0)
                if "skip_mix" not in self.taps:
                    self.proj(l, S["HXT"], S)
                if stop_after == ("proj", l):
                    kb.stk = old
                    break
                if "YMT" not in self.dbg_in:
                    self.mlstm(l, S)
                if stop_after == ("mlstm", l):
                    kb.stk = old
                    break
                if "YST" not in self.dbg_in:
                    self.s5(l, S)
                if stop_after == ("s5", l):
                    kb.stk = old
                    break
                if "YNT" not in self.dbg_in:
                    self.na(l, S)
                if stop_after == ("na", l):
                    kb.stk = old
                    break
                for n in ("YMT", "YNT", "YST"):
                    if n in self.dbg_in:
                        dcp = kb.sb("dcp", [1, 1], F32)
                        kb.dma(kb.sp, S[n].t[:, :], self.inp["dbg_" + n].t[l, :, :], w=[S[n]], sb=dcp)
                self.merge(l, S)
                if stop_after == ("merge", l):
                    kb.stk = old
                    break
                is_moe = (l % 2 == 1)
                if is_moe:
                    self.cmbt = kb.sb("cmbt", [128, self.NT // 128, 8], F32)
                import os as _os
                self.outproj(l, S, mods, router=False)
                if is_moe:
                    self.router_phase(l, S, mods)
                if stop_after == ("outproj", l):
                    kb.stk = old
                    break
                if is_moe:
                    self.moe(l, S, mods)
                else:
                    self.ffn_dense(l, S, mods)
                if stop_after == ("ffn", l):
                    kb.stk = old
                    break
                if l == self.depth - 1:
                    yout = kb.dram("y", [self.N, D], F32, kind="ExternalOutput")
                    self.out["y"] = yout
                    dcp2 = kb.sb("dcp2", [1, 1], F32)
                    for t0 in range(0, self.N, 1024):
                        n = min(1024, self.N - t0)
                        kb.dma(kb.sp, yout.t[t0:t0 + n, :], S["XU"].t[CTX + t0:CTX + t0 + n, :], r=[S["XU"]], w=[yout], sb=dcp2)
            kb.stk = old
        self.finish()
        return nc

    def finish(self):
        kb = self.kb
        allb = list(self.out.values()) + [b for v in self.S.values() for b in (v if isinstance(v, list) else [v])]
        for b in allb:
            for s, v in list(b.w.items()):
                if kb.sp.known.get(s, 0) < v:
                    kb.sp.h.wait_ge(s.h, v)
                    kb.sp.known[s] = v
        for e in (kb.pe, kb.dve, kb.act, kb.pool):
            if e.sem.cnt > 0:
                kb.sp.h.wait_ge(e.sem.h, e.sem.cnt)
        for s in list(kb.dsems) + list(kb.retired):
            if s.cnt > 0:
                kb.sp.h.wait_ge(s.h, s.cnt)


def prep_shared(inp):
    L = inp["w_in"].shape[0]
    w_in = np.asarray(inp["w_in"], np.float32)
    w_fm = np.zeros((L, D, 6144), np.float32)
    w_fm[:, :, 0:1024] = w_in[:, :, 0:1024]
    w_fm[:, :, 1024:1536] = w_in[:, :, 2064:2576]
    w_fm[:, :, 1536:2048] = w_in[:, :, 2576:3088]
    w_fm[:, :, 2048:2560] = w_in[:, :, 3600:4112]
    w_fm[:, :, 2560:5632] = w_in[:, :, 4112:7184]
    gate_b_sp = np.zeros((L, 4, 128), np.float32)
    gate_b_sp[:, 1, :] = 30.0
    gate_b_sp[:, 3, :] = 30.0
    for k in range(4):
        for g in range(4):
            w_fm[:, :, 5632 + k * 128 + 32 * g] = w_in[:, :, 2048 + 4 * k + g]
            gate_b_sp[:, k, 32 * g] = np.asarray(inp["m_gate_b"], np.float32)[:, 4 * k + g]
    w_tm = np.concatenate([w_in[:, :, 1024:1536], w_in[:, :, 1536:2048], w_in[:, :, 3088:3600]], axis=2)
    sh = {"w_fm": w_fm, "w_tm": np.ascontiguousarray(w_tm), "gate_b_sp": gate_b_sp}
    sh["na_am"] = na_tables(inp["na_rpb"], inp["x"].shape[1])
    sh["rope_cos"], sh["rope_sin"], sh["rope_rT"] = rope_tables(inp["x"].shape[1])
    L = inp["w_in"].shape[0]
    f32 = lambda k: np.asarray(inp[k], np.float32)
    tostate = lambda a: a.reshape(L, 2, 16, 128).transpose(0, 1, 3, 2)
    logdt_rep = np.repeat(f32("s5_log_dt")[:, :, :, None], 64, axis=3)
    sh["s5p"] = np.ascontiguousarray(np.stack([tostate(f32("s5_lam_re")), tostate(f32("s5_lam_im")), tostate(logdt_rep),
                                               tostate(logdt_rep)], axis=-1))
    bcat = np.stack([f32("s5_b_re"), f32("s5_b_im")], axis=4)
    sh["s5B"] = np.ascontiguousarray(bcat.reshape(L, 2, 16, 128, 2, 16))
    ccat = np.stack([f32("s5_c_re"), f32("s5_c_im")], axis=3)
    ccat = np.stack([f32("s5_c_re").transpose(0, 1, 2, 4, 3), f32("s5_c_im").transpose(0, 1, 2, 4, 3)], axis=4)
    sh["s5CT"] = np.ascontiguousarray(ccat.reshape(L, 2, 16, 128, 2, 16))
    for n in ("s5_d", "s5_glu_w", "s5_glu_b"):
        sh[n] = np.ascontiguousarray(f32(n))
    for n in ("m_conv_w", "m_conv_b", "m_norm"):
        sh[n] = np.ascontiguousarray(np.asarray(inp[n], np.float32))
    for n in ("ada_w", "ada_b", "norm_mix_pre", "norm_mix_post", "norm_ffn_pre", "norm_ffn_post", "w_branch_m", "w_branch_na",
              "w_branch_s5", "w_out", "ffn_w_gate", "ffn_w_up", "ffn_w_down", "moe_router", "moe_w_gate", "moe_w_up", "moe_w_down"):
        sh[n] = np.ascontiguousarray(np.asarray(inp[n], np.float32))
    return sh


def na_tables(rpb, N):
    rpb = np.asarray(rpb, np.float32)
    L = rpb.shape[0]
    nq = N // 128
    rows = N // GRID_W
    win_r = min(8, rows)
    nkt = min(5, nq)
    cases = {}
    for qt in range(nq):
        kt0 = int(np.clip(qt - 2, 0, nq - nkt))
        r0s = tuple(int(np.clip(r - win_r // 2, 0, rows - win_r)) - 2 * kt0 for r in (2 * qt, 2 * qt + 1))
        key = (qt - kt0,) + r0s
        if key not in cases:
            cases[key] = len(cases)
    am = np.full((L, len(cases), 8, 128, nkt * 128), -30000.0, np.float32)
    q = np.arange(128)
    qr_par, qc = q // 64, q % 64
    k = np.arange(nkt * 128)
    krow_rel, kc = k // 64, k % 64
    col_start = np.clip(qc - 8, 0, 48)
    col_ok = (kc[None, :] >= col_start[:, None]) & (kc[None, :] < col_start[:, None] + 16)
    dc = np.clip(kc[None, :] - qc[:, None] + 15, 0, 30)
    for (dq, r0a, r0b), ci in cases.items():
        qrow_rel = 2 * dq + qr_par
        r0 = np.where(qr_par == 0, r0a, r0b)
        row_ok = (krow_rel[None, :] >= r0[:, None]) & (krow_rel[None, :] < r0[:, None] + win_r)
        dr = np.clip(krow_rel[None, :] - qrow_rel[:, None] + 7, 0, 14)
        ok = row_ok & col_ok
        g = rpb[:, :, dr, dc]
        am[:, ci] = np.where(ok[None, None], g, np.float32(-30000.0))
    return am


def rope_tables(N):
    pos = np.arange(N, dtype=np.int32)
    row = (pos // GRID_W).astype(np.float32)
    col = (pos % GRID_W).astype(np.float32)
    n_freq = 32
    inv = (np.float32(10000.0) ** (-np.arange(n_freq, dtype=np.float32) / np.float32(n_freq))).astype(np.float32)
    ar = (row[:, None] * inv).astype(np.float32)
    ac = (col[:, None] * inv).astype(np.float32)
    ang = np.concatenate([ar, ar, ac, ac], axis=1)
    cos = np.cos(ang).astype(np.float32).T
    sin = np.sin(ang).astype(np.float32).T
    R = np.zeros((128, 128), np.float32)
    for d in range(128):
        if d % 64 < 32:
            R[d, d + 32] = -1.0
        else:
            R[d, d - 32] = 1.0
    return np.ascontiguousarray(cos), np.ascontiguousarray(sin), np.ascontiguousarray(R.T)


def prep_core(inp, b):
    xu = np.concatenate([np.asarray(inp["ctx"][b], np.float32), np.asarray(inp["x"][b], np.float32)], axis=0)
    cc = np.stack([np.asarray(inp["c"][b], np.float32), np.asarray(inp["c_ctx"], np.float32)], axis=0)
    return {"xu": np.ascontiguousarray(xu), "cc": np.ascontiguousarray(cc)}


_CACHE = {}


def kernel(**inputs):
    x = np.asarray(inputs["x"])
    B, N, _ = x.shape
    depth = int(np.asarray(inputs["w_in"]).shape[0])
    key = (N, depth)
    if key not in _CACHE:
        P = Prog(N, depth=depth)
        P.build()
        _CACHE[key] = P
    P = _CACHE[key]
    sh = prep_shared(inputs)
    in_maps = []
    for b in range(B):
        core = prep_core(inputs, b)
        in_maps.append({k: (sh[k] if k in sh else core[k]) for k in P.inp})
    res = run_bass_kernel_spmd(P.nc, in_maps, core_ids=list(range(B)))
    out = np.stack([np.asarray(r["y"], np.float32) for r in res.results], axis=0)
    return out
```
